# Optimizing a Trainium2 kernel written in Bass

```python
import math
import jax, jax.numpy as jnp
from jax import lax
import numpy as np

D_MODEL = 2048
BATCH = 4
SEQ = 2048
DEPTH = 4
DEC_BATCH = 32
DEC_SEQ = 4
PAST_LEN = 16384
PAGE_SIZE = 128

N_MIXERS = 2
N_A_LAYERS = (DEPTH + 1) // 2
N_B_LAYERS = DEPTH // 2
HG_HEADS = 16
HG_DK = 128
HG_DV = D_MODEL // HG_HEADS
HG_HK = HG_HEADS * HG_DK
HG_HV = HG_HEADS * HG_DV
HG_IN = 2 * HG_HK + HG_HV + D_MODEL
HG_CHUNK = 64
N_Q_HEADS = 32
N_KV_HEADS = 8
HEAD_DIM = D_MODEL // N_Q_HEADS
GQA_GROUP = N_Q_HEADS // N_KV_HEADS
ATTN_IN = (N_Q_HEADS + 2 * N_KV_HEADS) * HEAD_DIM
WINDOW = 128
ROPE_THETA = 10000.0
D_FF = 5632
NORM_EPS = 1e-6
N_NORMS = 6

kernel_name = 'hgrn2_swa_sink_macaron_decode_step'

F32 = jnp.float32


def rms_norm(x, g):
    xf = x.astype(F32)
    y = xf * lax.rsqrt(jnp.mean(xf * xf, axis=-1, keepdims=True) + NORM_EPS)
    return (y * g.astype(F32)).astype(x.dtype)


def swiglu(h, w_in, w_out):
    gate, up = jnp.split(h @ w_in, 2, axis=-1)
    return (jax.nn.silu(gate) * up) @ w_out


def macaron_half(x, g_pre, g_post, w_in, w_out):
    return x + 0.5 * rms_norm(swiglu(rms_norm(x, g_pre), w_in, w_out), g_post)


def rope(x, pos):
    half = HEAD_DIM // 2
    inv = jnp.power(ROPE_THETA, -jnp.arange(half, dtype=F32) * (2.0 / HEAD_DIM))
    ang = pos.astype(F32)[:, None] * inv[None, :]
    cos = jnp.cos(ang)[:, None, :]
    sin = jnp.sin(ang)[:, None, :]
    xf = x.astype(F32)
    x1, x2 = xf[..., :half], xf[..., half:]
    return jnp.concatenate([x1 * cos - x2 * sin, x2 * cos + x1 * sin], axis=-1).astype(x.dtype)


def attn_project(h, w_in, pos):
    B, L, _ = h.shape
    q, k, v = jnp.split(h @ w_in, [N_Q_HEADS * HEAD_DIM, (N_Q_HEADS + N_KV_HEADS) * HEAD_DIM], axis=-1)
    q = rope(q.reshape(B, L, N_Q_HEADS, HEAD_DIM), pos)
    k = rope(k.reshape(B, L, N_KV_HEADS, HEAD_DIM), pos)
    v = v.reshape(B, L, N_KV_HEADS, HEAD_DIM)
    return q, k, v


def sink_attend(q, k, v, mask, sinks):
    s = jnp.einsum('bnqkgd,bnskd->bnkgqs', q, k).astype(F32) * (HEAD_DIM ** -0.5)
    s = jnp.where(mask[None, :, None, None], s, -jnp.inf)
    sink = sinks.astype(F32).reshape(1, 1, N_KV_HEADS, GQA_GROUP, 1, 1)
    m = jnp.maximum(jnp.max(s, axis=-1, keepdims=True), sink)
    p = jnp.exp(s - m)
    denom = jnp.sum(p, axis=-1, keepdims=True) + jnp.exp(sink - m)
    return jnp.einsum('bnkgqs,bnskd->bnqkgd', (p / denom).astype(v.dtype), v)


def swa_prompt(h, w_in, sinks, w_out):
    B, L, _ = h.shape
    buf = min(WINDOW, PAST_LEN)
    pos = jnp.arange(L, dtype=jnp.int32)
    q, k, v = attn_project(h, w_in, pos)
    nb = L // WINDOW
    qb = q.reshape(B, nb, WINDOW, N_KV_HEADS, GQA_GROUP, HEAD_DIM)
    zpad = jnp.zeros((B, WINDOW, N_KV_HEADS, HEAD_DIM), k.dtype)
    kb = jnp.concatenate([zpad, k], axis=1).reshape(B, nb + 1, WINDOW, N_KV_HEADS, HEAD_DIM)
    vb = jnp.concatenate([zpad, v], axis=1).reshape(B, nb + 1, WINDOW, N_KV_HEADS, HEAD_DIM)
    kk = jnp.concatenate([kb[:, :-1], kb[:, 1:]], axis=2)
    vv = jnp.concatenate([vb[:, :-1], vb[:, 1:]], axis=2)
    blk = jnp.arange(nb)[:, None, None]
    qpos = blk * WINDOW + jnp.arange(WINDOW)[None, :, None]
    kpos = (blk - 1) * WINDOW + jnp.arange(2 * WINDOW)[None, None, :]
    mask = (kpos >= 0) & (kpos <= qpos) & (kpos >= qpos - WINDOW)
    o = sink_attend(qb, kk, vv, mask, sinks).reshape(B, L, N_Q_HEADS * HEAD_DIM)
    return o @ w_out, k[:, L - buf:], v[:, L - buf:]


def swa_sample(h, k_buf, v_buf, w_in, sinks, w_out):
    B, L, _ = h.shape
    buf = k_buf.shape[1]
    pos = PAST_LEN + jnp.arange(L, dtype=jnp.int32)
    q, k, v = attn_project(h, w_in, pos)
    kk = jnp.concatenate([k_buf.astype(k.dtype), k], axis=1)
    vv = jnp.concatenate([v_buf.astype(v.dtype), v], axis=1)
    qpos = pos[:, None]
    kpos = (PAST_LEN - buf + jnp.arange(buf + L, dtype=jnp.int32))[None, :]
    mask = ((kpos <= qpos) & (kpos >= qpos - WINDOW))[None]
    qb = q.reshape(B, 1, L, N_KV_HEADS, GQA_GROUP, HEAD_DIM)
    o = sink_attend(qb, kk[:, None], vv[:, None], mask, sinks).reshape(B, L, N_Q_HEADS * HEAD_DIM)
    return o @ w_out, kk[:, -buf:], vv[:, -buf:]


def hgrn2_scan(q, log_f, k, v, S0):
    B, L, H, _ = q.shape
    C = min(HG_CHUNK, L)
    n = -(-L // C)
    pad = n * C - L

    def prep(a):
        a = jnp.pad(a.astype(F32), ((0, 0), (0, pad), (0, 0), (0, 0)))
        return a.reshape(B, n, C, H, a.shape[-1]).transpose(1, 0, 3, 2, 4)

    causal = jnp.tril(jnp.ones((C, C), dtype=bool))[None, None, :, :, None]

    def step(S, xs):
        qc, gc, kc, vc = xs
        b = jnp.cumsum(gc, axis=2)
        diff = b[:, :, :, None, :] - b[:, :, None, :, :]
        decay = jnp.exp(jnp.where(causal, diff, -jnp.inf))
        A = jnp.einsum('bhtk,bhtsk,bhsk->bhts', qc, decay, kc)
        o = jnp.einsum('bhts,bhsv->bhtv', A, vc) + jnp.einsum('bhtk,bhkv->bhtv', qc * jnp.exp(b), S)
        bl = b[:, :, -1:, :]
        S = jnp.exp(bl[:, :, 0, :])[..., None] * S + jnp.einsum('bhsk,bhsv->bhkv', kc * jnp.exp(bl - b), vc)
        return S, o

    S, o = lax.scan(step, S0.astype(F32), (prep(q), prep(log_f), prep(k), prep(v)))
    o = o.transpose(1, 0, 3, 2, 4).reshape(B, n * C, H, HG_DV)[:, :L]
    return o, S


def hgrn2_mixer(h, S0, w_in, lb, norm_g, w_out):
    B, L, _ = h.shape
    qz, fz, iz, gz = jnp.split(h @ w_in, [HG_HK, 2 * HG_HK, 2 * HG_HK + HG_HV], axis=-1)
    fzf = fz.astype(F32)
    lbf = lb.astype(F32)
    log_f = jnp.logaddexp(jnp.log(lbf), jnp.log1p(-lbf) + jax.nn.log_sigmoid(fzf))
    key = (1.0 - lbf) * jax.nn.sigmoid(-fzf)
    shp = (B, L, HG_HEADS, HG_DK)
    o, S = hgrn2_scan(qz.reshape(shp), log_f.reshape(shp), key.reshape(shp),
                      iz.reshape(B, L, HG_HEADS, HG_DV), S0)
    o = o * lax.rsqrt(jnp.mean(o * o, axis=-1, keepdims=True) + NORM_EPS) * norm_g.astype(F32).reshape(HG_HEADS, HG_DV)
    o = o.astype(h.dtype) * jax.nn.silu(gz).reshape(B, L, HG_HEADS, HG_DV)
    return o.reshape(B, L, HG_HV) @ w_out, S


def setup_inputs(seed: int = 0) -> dict:
    key = jax.random.key(seed)
    ks = jax.random.split(key, 16)
    buf = min(WINDOW, PAST_LEN)
    nrm = jax.random.normal
    return {
        'x_prompt': nrm(ks[0], (BATCH, SEQ, D_MODEL), F32),
        'x_sample': nrm(ks[1], (DEC_BATCH, DEC_SEQ, D_MODEL), F32),
        'state_hgrn': 0.5 * nrm(ks[2], (N_A_LAYERS, DEC_BATCH, HG_HEADS, HG_DK, HG_DV), F32),
        'cache_k_win': nrm(ks[3], (N_B_LAYERS, DEC_BATCH, buf, N_KV_HEADS, HEAD_DIM), F32),
        'cache_v_win': nrm(ks[4], (N_B_LAYERS, DEC_BATCH, buf, N_KV_HEADS, HEAD_DIM), F32),
        'norm_gains': 1.0 + 0.05 * nrm(ks[5], (DEPTH, N_NORMS, D_MODEL), F32),
        'w_ffn_in': nrm(ks[6], (DEPTH, 2, D_MODEL, 2 * D_FF), F32) * D_MODEL ** -0.5,
        'w_ffn_out': nrm(ks[7], (DEPTH, 2, D_FF, D_MODEL), F32) * D_FF ** -0.5,
        'w_hgrn_in': nrm(ks[8], (N_A_LAYERS, D_MODEL, HG_IN), F32) * D_MODEL ** -0.5,
        'hgrn_lb_logits': nrm(ks[9], (N_A_LAYERS, HG_HK), F32),
        'hgrn_norm_gain': 1.0 + 0.05 * nrm(ks[10], (N_A_LAYERS, HG_HV), F32),
        'w_hgrn_out': nrm(ks[11], (N_A_LAYERS, HG_HV, D_MODEL), F32) * HG_HV ** -0.5,
        'w_attn_in': nrm(ks[12], (N_B_LAYERS, D_MODEL, ATTN_IN), F32) * D_MODEL ** -0.5,
        'attn_sinks': nrm(ks[13], (N_B_LAYERS, N_Q_HEADS), F32),
        'w_attn_out': nrm(ks[14], (N_B_LAYERS, N_Q_HEADS * HEAD_DIM, D_MODEL), F32) * (N_Q_HEADS * HEAD_DIM) ** -0.5,
    }


def reference(x_prompt, x_sample, state_hgrn, cache_k_win, cache_v_win, norm_gains, w_ffn_in, w_ffn_out,
              w_hgrn_in, hgrn_lb_logits, hgrn_norm_gain, w_hgrn_out, w_attn_in, attn_sinks, w_attn_out):
    lb_all = jnp.cumsum(jax.nn.softmax(hgrn_lb_logits.astype(F32), axis=0), axis=0)
    lb_all = lb_all - lb_all[0:1]
    yp, ys = x_prompt, x_sample
    hg_p, hg_s, kp_l, vp_l, ks_l, vs_l = [], [], [], [], [], []
    for layer in range(DEPTH):
        g = norm_gains[layer]
        yp = macaron_half(yp, g[0], g[1], w_ffn_in[layer, 0], w_ffn_out[layer, 0])
        ys = macaron_half(ys, g[0], g[1], w_ffn_in[layer, 0], w_ffn_out[layer, 0])
        hp = rms_norm(yp, g[2])
        hs = rms_norm(ys, g[2])
        if layer % N_MIXERS == 0:
            a = layer // N_MIXERS
            S0p = jnp.zeros((hp.shape[0], HG_HEADS, HG_DK, HG_DV), F32)
            op, Sp = hgrn2_mixer(hp, S0p, w_hgrn_in[a], lb_all[a], hgrn_norm_gain[a], w_hgrn_out[a])
            os_, Ss = hgrn2_mixer(hs, state_hgrn[a], w_hgrn_in[a], lb_all[a], hgrn_norm_gain[a], w_hgrn_out[a])
            hg_p.append(Sp.astype(state_hgrn.dtype))
            hg_s.append(Ss.astype(state_hgrn.dtype))
        else:
            b = layer // N_MIXERS
            op, kp, vp = swa_prompt(hp, w_attn_in[b], attn_sinks[b], w_attn_out[b])
            os_, kn, vn = swa_sample(hs, cache_k_win[b], cache_v_win[b], w_attn_in[b], attn_sinks[b], w_attn_out[b])
            kp_l.append(kp.astype(cache_k_win.dtype))
            vp_l.append(vp.astype(cache_v_win.dtype))
            ks_l.append(kn.astype(cache_k_win.dtype))
            vs_l.append(vn.astype(cache_v_win.dtype))
        yp = yp + rms_norm(op, g[3])
        ys = ys + rms_norm(os_, g[3])
        yp = macaron_half(yp, g[4], g[5], w_ffn_in[layer, 1], w_ffn_out[layer, 1])
        ys = macaron_half(ys, g[4], g[5], w_ffn_in[layer, 1], w_ffn_out[layer, 1])
    return (yp, ys, jnp.stack(hg_p), jnp.stack(hg_s), jnp.stack(kp_l), jnp.stack(vp_l), jnp.stack(ks_l), jnp.stack(vs_l))
```

```python
import contextlib
import numpy as np
import concourse.bass as bass
import concourse.mybir as mybir
from concourse.bass_utils import run_bass_kernel_spmd

F32 = mybir.dt.float32
BF16 = mybir.dt.bfloat16
AF = mybir.ActivationFunctionType
ALU = mybir.AluOpType
AX = mybir.AxisListType

D = 2048
NCH = 16
TP = 512
NSEQ_S = 4
LS = 4
NS = NSEQ_S * LS
NTMAX = TP + NS
WIN = 128
PAST = 16384
EPS = 1e-6
HC = 64
import os
DBG_SKIP = os.environ.get('K_SKIP', '').split(',')
WORKW = 16512


class Cfg:
    def __init__(self, depth=4, seq=2048, dff=5632, batch=4, dec_batch=32):
        self.depth, self.seq, self.dff, self.batch, self.dec_batch = depth, seq, dff, batch, dec_batch
        self.npass = seq // TP
        self.nff = dff // 128
        self.na = (depth + 1) // 2
        self.nb = depth // 2


class Buf:
    __slots__ = ("w", "r")

    def __init__(self):
        self.w = None
        self.r = {}


class Eng:
    def __init__(self, h, sem, is_pe=False):
        self.h, self.sem, self.cnt, self.waited, self.is_pe = h, sem, 0, {}, is_pe

    def wait(self, dep):
        sem, val = dep
        if sem is self.sem and self.is_pe:
            return
        k = id(sem)
        if self.waited.get(k, 0) >= val:
            return
        self.waited[k] = val
        self.h.wait_ge(sem, val)


class Prog:
    def __init__(self, nc, es):
        self.nc, self.es = nc, es
        sm = lambda n: es.enter_context(nc.semaphore(n))
        self.pe = Eng(nc.tensor, sm("s_pe"), True)
        self.act = Eng(nc.scalar, sm("s_act"))
        self.dve = Eng(nc.vector, sm("s_dve"))
        self.pool = Eng(nc.gpsimd, sm("s_pool"))
        self.sp = Eng(nc.sync, sm("s_sp"))
        self.dsems = [[sm("s_d%d" % i), 0] for i in range(12)]
        self.dnext = 0
        self.psems = [[sm("s_q%d" % i), 0] for i in range(2)]
        self.pnext = 0

    def _deps(self, eng, reads, writes):
        for b in reads:
            if b.w is not None:
                eng.wait(b.w)
        for b in writes:
            if b.w is not None:
                eng.wait(b.w)
            for d in b.r.values():
                eng.wait(d)

    def _mark(self, dep, reads, writes):
        for b in reads:
            b.r[id(dep[0])] = dep
        for b in writes:
            b.w = dep
            b.r = {}

    def op(self, eng, name, reads, writes, *a, **k):
        self._deps(eng, reads, writes)
        ins = getattr(eng.h, name)(*a, **k)
        eng.cnt += 1
        ins.then_inc(eng.sem, 1)
        self._mark((eng.sem, eng.cnt), reads, writes)

    def mm(self, mms, reads, writes, transpose=False):
        eng = self.pe
        self._deps(eng, reads, writes)
        n = len(mms)
        ins = None
        for i, m in enumerate(mms):
            if transpose:
                ins = eng.h.transpose(m[0], m[1], m[2])
            else:
                st = m[3] if len(m) > 3 else (i == 0)
                sp = m[4] if len(m) > 4 else (i == n - 1)
                ins = eng.h.matmul(m[0], m[1], m[2], start=st, stop=sp)
        eng.cnt += 1
        ins.then_inc(eng.sem, 1)
        self._mark((eng.sem, eng.cnt), reads, writes)

    def dma(self, eng, out, in_, reads, writes, semslot=None, **k):
        if semslot is None and eng is self.pool:
            semslot = self.psems[self.pnext]
            self.pnext = (self.pnext + 1) % len(self.psems)
        elif semslot is None:
            semslot = self.dsems[self.dnext]
            self.dnext = (self.dnext + 1) % len(self.dsems)
        sem = semslot[0]
        if semslot[1] > 0:
            eng.wait((sem, semslot[1]))
        self._deps(eng, reads, writes)
        eng.h.dma_start(out=out, in_=in_, **k).then_inc(sem, 16)
        semslot[1] += 16
        self._mark((sem, semslot[1]), reads, writes)

    def barrier(self):
        engs = [self.pe, self.act, self.dve, self.pool, self.sp]
        for e in engs:
            for f in engs:
                if f is not e and f.cnt > 0:
                    e.wait((f.sem, f.cnt))
            for s in self.dsems + self.psems:
                if s[1] > 0:
                    e.wait((s[0], s[1]))


class Ring:
    def __init__(self, items):
        self.items, self.i = items, 0

    def next(self):
        it = self.items[self.i]
        self.i = (self.i + 1) % len(self.items)
        return it


def host_consts(cfg):
    c = {}
    c["ident32"] = np.eye(128, dtype=np.float32)
    c["ones32"] = np.ones((128, 128), np.float32)
    R = np.zeros((128, 128), np.float32)
    for blk in range(2):
        for j in range(32):
            R[blk * 64 + j + 32, blk * 64 + j] = -1.0
            R[blk * 64 + j, blk * 64 + j + 32] = 1.0
    c["rotm"] = R
    half = 32
    inv = np.power(np.float32(10000.0), -(np.arange(half, dtype=np.float32) * np.float32(2.0 / 64))).astype(np.float32)
    pos = np.concatenate([np.arange(cfg.seq, dtype=np.float32),
                          (PAST + np.arange(LS)).astype(np.float32)])
    ang = (pos[None, :] * inv[:, None]).astype(np.float32)
    cos = np.cos(ang.astype(np.float64)).astype(np.float32)
    sin = np.sin(ang.astype(np.float64)).astype(np.float32)
    cosT = np.tile(cos, (4, 1))
    sinT = np.tile(sin, (4, 1))
    c["cosT"] = np.concatenate([cosT[:, :cfg.seq]] + [cosT[:, cfg.seq:]] * NSEQ_S, axis=1).copy()
    c["sinT"] = np.concatenate([sinT[:, :cfg.seq]] + [sinT[:, cfg.seq:]] * NSEQ_S, axis=1).copy()
    rm = np.ones((128, NTMAX), np.float32)
    rm[:, 0:TP:HC] = 0.0
    rm[:, TP::LS] = 0.0
    c["rmask"] = rm
    c["cm64"] = np.triu(np.ones((HC, HC), np.float32))
    t = np.arange(128)[:, None]
    s = np.arange(256)[None, :]
    band = ((s >= t) & (s <= t + 128)).astype(np.float32)
    first = band * (s >= 128)
    c["mband"] = band
    c["mfirst"] = first.astype(np.float32)
    tt = (np.arange(16) % 4)[:, None]
    j = np.arange(132)[None, :]
    c["msamp"] = ((j >= tt) & (j <= 128 + tt)).astype(np.float32)
    return c


CONST_SHAPES = {
    "ident32": [128, 128], "ones32": [128, 128], "rotm": [128, 128],
    "rmask": [128, NTMAX], "cm64": [HC, HC], "mband": [128, 256], "mfirst": [128, 256],
    "msamp": [16, 132],
}


def build(cfg):
    nc = bass.Bass("TRN2", target_bir_lowering=False)
    NP, NFF, NA, NB, DFF = cfg.npass, cfg.nff, cfg.na, cfg.nb, cfg.dff
    L = cfg.depth

    def din(name, shape):
        return nc.dram_tensor(name, list(shape), F32, kind="ExternalInput").ap()

    def dout(name, shape):
        return nc.dram_tensor(name, list(shape), F32, kind="ExternalOutput").ap()

    xp = din("xp", [cfg.seq, D])
    xs = din("xs", [NS, D])
    st_in = din("st_in", [NA, NSEQ_S, 16, 128, 128])
    ck_in = din("ck_in", [max(NB, 1), NSEQ_S, WIN, 512])
    cv_in = din("cv_in", [max(NB, 1), NSEQ_S, WIN, 512])
    gains = din("gains", [128, L * 6 * NCH])
    w_fin = din("w_fin", [L, 2, D, 2 * DFF])
    w_fout = din("w_fout", [L, 2, DFF, D])
    w_hin = din("w_hin", [NA, D, 8192])
    lbl = din("lbl", [128, NA * 16])
    hgn = din("hgn", [128, NA * 16])
    w_hout = din("w_hout", [NA, D, D])
    w_ain = din("w_ain", [max(NB, 1), D, 3072])
    sinks_b = din("sinks_b", [max(NB, 1), 32])
    w_aout = din("w_aout", [max(NB, 1), D, D])
    sinks16_d = din("sinks16_d", [16, max(NB, 1) * 8])
    cst = {k: din("c_" + k, v) for k, v in CONST_SHAPES.items()}
    cosT_d = din("c_cosT", [128, cfg.seq + NS])
    sinT_d = din("c_sinT", [128, cfg.seq + NS])

    yp = dout("yp", [cfg.seq, D])
    ys = dout("ys", [NS, D])
    hg_p = dout("hg_p", [NA, 16, 128, 128])
    hg_s = dout("hg_s", [NA, NSEQ_S, 16, 128, 128])
    kw_p = dout("kw_p", [max(NB, 1), WIN, 512])
    vw_p = dout("vw_p", [max(NB, 1), WIN, 512])
    kw_s = dout("kw_s", [max(NB, 1), NSEQ_S, WIN, 512])
    vw_s = dout("vw_s", [max(NB, 1), NSEQ_S, WIN, 512])

    with contextlib.ExitStack() as es:
        P = Prog(nc, es)
        PE, ACT, DVE, POOL, SP = P.pe, P.act, P.dve, P.pool, P.sp

        def sb(name, shape, dt=F32):
            return es.enter_context(nc.sbuf_tensor(name, list(shape), dt))

        xT = sb("xT", [128, NCH, NTMAX])
        xb = [Buf() for _ in range(NCH)]
        yT = sb("yT", [128, NCH, NTMAX])
        yb = [Buf() for _ in range(NCH)]
        hT = yT[:, 0:NCH // 2, :].bitcast(BF16).rearrange("p a (b n) -> p (a b) n", b=2)
        hbufs = yb[0:NCH // 2]
        work = sb("work", [128, WORKW])
        ob = yT[:, NCH // 2:NCH, :].bitcast(BF16).rearrange("p a (b n) -> p (a b) n", b=2)
        obb = [yb[NCH // 2 + c // 2] for c in range(NCH)]
        ym = work[:, 0:NCH * NTMAX].rearrange("p (c n) -> p c n", n=NTMAX)
        ymb = [Buf() for _ in range(NCH)]
        rstd = sb("rstd", [128, NTMAX])
        rstdb = Buf()
        wr = [sb("wr%d" % i, [128, 8192], BF16) for i in range(2)]
        wrb = [Buf() for _ in range(2)]
        wsem = [[es.enter_context(nc.semaphore("s_w%d" % i)), 0] for i in range(2)]
        wring = Ring(list(range(2)))
        sqr = [sb("sq%d" % i, [128, NTMAX]) for i in range(2)]
        sqring = Ring([(sqr[i], Buf()) for i in range(2)])
        tmpr = [sb("tmp%d" % i, [128, 512]) for i in range(3)]
        tmpring = Ring([(tmpr[i], Buf()) for i in range(3)])
        g_sb = sb("g_sb", [128, L * 6 * NCH])
        gh_sb = sb("gh_sb", [128, L * 6 * NCH])
        gb = Buf()
        ident32 = sb("ident32", [128, 128]); ident16 = sb("ident16", [128, 128], BF16)
        ones32 = sb("ones32", [128, 128]); rotm = sb("rotm", [128, 128])
        rmask = sb("rmask", [128, NTMAX]); cm64 = sb("cm64", [HC, HC])
        mband = sb("mband", [128, 256]); mfirst = sb("mfirst", [128, 256]); msamp = sb("msamp", [16, 132])
        cb = Buf()
        lb_sb = sb("lb_sb", [128, NA * 16]); oml_sb = sb("oml_sb", [128, NA * 16]); hgn_sb = sb("hgn_sb", [128, NA * 16])
        sscr = nc.dram_tensor("sscr", [NA, 16, 128, 128], F32).ap()
        if NB:
            kprev = [sb("kprev%d" % b, [128, 4, WIN], BF16) for b in range(NB)]
            vprev = [sb("vprev%d" % b, [128, 512], BF16) for b in range(NB)]
            kvpb = [Buf() for _ in range(NB)]
            sink_sb = sb("sink_sb", [128, NB * 32]); nsink_sb = sb("nsink_sb", [128, NB * 32])
            sinks16 = sb("sinks16", [16, NB * 8]); nsinks16 = sb("nsinks16", [16, NB * 8])
            cosT = sb("cosT", [128, NTMAX]); sinT = sb("sinT", [128, NTMAX])
            ropeb = Buf()
        yflat = yT[:, :, :].rearrange("p c n -> p (c n)")
        xstring = Ring([(yflat[:, i * D:(i + 1) * D], Buf()) for i in range(2)])

        ps = es.enter_context(nc.psum_tensor("ps", [128, 8, 512], F32))
        psring = Ring([(ps[:, i, :], Buf()) for i in range(6)])
        accring = Ring([(ps[:, i, :], Buf()) for i in range(6, 8)])

        def wcols(off, n):
            return slice(off, off + n)

        def ld(dst, src, bufs):
            P.dma(SP, dst, src, [], bufs)

        ld(g_sb[:], gains, [gb])
        for nm, t_ in (("ident32", ident32), ("ones32", ones32), ("rotm", rotm), ("rmask", rmask),
                       ("cm64", cm64), ("mband", mband), ("mfirst", mfirst), ("msamp", msamp)):
            ld(t_[:], cst[nm], [cb])
        ld(lb_sb[:], lbl, [cb])
        ld(hgn_sb[:], hgn, [cb])
        P.op(DVE, "tensor_copy", [cb], [cb], ident16[:], ident32[:])
        P.op(DVE, "tensor_scalar", [gb], [gb], gh_sb[:], g_sb[:], 0.5, None, ALU.mult)
        P.op(DVE, "memset", [], [cb], oml_sb[:], 1.0)
        if NA > 1:
            assert NA == 2
            P.op(DVE, "tensor_tensor", [cb], [cb], oml_sb[:, 16:32], lb_sb[:, 16:32], lb_sb[:, 0:16], ALU.subtract)
            P.op(ACT, "activation", [cb], [cb], lb_sb[:, 16:32], oml_sb[:, 16:32], AF.Sigmoid)
            P.op(DVE, "tensor_scalar", [cb], [cb], oml_sb[:, 16:32], lb_sb[:, 16:32], -1.0, 1.0, ALU.mult, ALU.add)
        P.op(DVE, "memset", [cb], [cb], lb_sb[:, 0:16], 0.0)
        for b in range(NB):
            P.op(DVE, "memset", [], [kvpb[b]], kprev[b][:], 0.0)
            P.op(DVE, "memset", [], [kvpb[b]], vprev[b][:], 0.0)
            ld(sink_sb[:, b * 32:(b + 1) * 32], sinks_b[b:b + 1, :].partition_broadcast(128), [cb])
        if NB:
            ld(sinks16[:], sinks16_d, [cb])
            P.op(DVE, "tensor_scalar", [cb], [cb], nsink_sb[:], sink_sb[:], -1.0, None, ALU.mult)
            P.op(DVE, "tensor_scalar", [cb], [cb], nsinks16[:], sinks16[:], -1.0, None, ALU.mult)

        def gcol(tbl, l, n, c):
            i = (l * 6 + n) * NCH + c
            return tbl[:, i:i + 1]

        def wload(parts):
            s = wring.next()
            for (off, n, kc, src) in parts:
                dst = wr[s][:, off:off + kc * n].rearrange("p (k n) -> p k n", n=n)
                P.dma(POOL, dst, src, [], [wrb[s]], semslot=wsem[s])
            return s

        def slabs_of(p):
            return [(0, TP)] + ([(TP, NS)] if p == 0 else [])

        def compute_rstd(src, sbufs, slabs, nt):
            for (c0, n) in slabs:
                bank, bb = psring.next()
                for c in range(NCH):
                    sq, sqb = sqring.next()
                    P.op(ACT, "activation", [sbufs[c]], [sqb], sq[:, 0:n], src[:, c, c0:c0 + n], AF.Square)
                    P.mm([(bank[:, 0:n], ones32[:], sq[:, 0:n], c == 0, c == NCH - 1)], [sqb, cb], [bb])
                t_, tb = tmpring.next()
                P.op(ACT, "activation", [bb], [tb], t_[:, 0:n], bank[:, 0:n], AF.Sqrt, bias=EPS, scale=1.0 / D)
                P.op(DVE, "reciprocal", [tb], [rstdb], rstd[:, c0:c0 + n], t_[:, 0:n])

        def pre_norm(l, n_, slabs, nt):
            compute_rstd(xT, xb, slabs, nt)
            for c in range(NCH):
                P.op(DVE, "scalar_tensor_tensor", [xb[c], rstdb, gb], [hbufs[c // 2]],
                     hT[:, c, 0:nt], xT[:, c, 0:nt], gcol(g_sb, l, n_, c), rstd[:, 0:nt], ALU.mult, ALU.mult)

        def post_norm(l, n_, half, slabs, nt, src=None, srcb=None):
            if src is None:
                src, srcb = yT, yb
            compute_rstd(src, srcb, slabs, nt)
            tbl = gh_sb if half else g_sb
            for c in range(NCH):
                P.op(DVE, "scalar_tensor_tensor", [rstdb, gb], [srcb[c]],
                     src[:, c, 0:nt], src[:, c, 0:nt], gcol(tbl, l, n_, c), rstd[:, 0:nt], ALU.mult, ALU.mult)
                P.op(DVE, "tensor_tensor", [srcb[c]], [xb[c]], xT[:, c, 0:nt], xT[:, c, 0:nt], src[:, c, 0:nt], ALU.add)

        actT = work[:, 0:NFF * NTMAX // 2].bitcast(BF16).rearrange("p (j n) -> p j n", n=NTMAX)
        ab = [Buf() for _ in range(NFF)]

        def ffn(l, i, slabs, nt):
            pre_norm(l, 0 if i == 0 else 4, slabs, nt)
            wv = w_fin[l, i].rearrange("(c p) n -> p c n", p=128)
            for fb in range(NFF // 2):
                s = wload([(0, 256, 16, wv[:, :, fb * 256:(fb + 1) * 256]),
                           (4096, 256, 16, wv[:, :, DFF + fb * 256:DFF + (fb + 1) * 256])])
                ws = wr[s][:, :].rearrange("p (g c n) -> p g c n", g=2, c=16)
                for j in range(2):
                    J = 2 * fb + j
                    for (c0, n) in slabs:
                        bg, bgb = psring.next()
                        P.mm([(bg[:, 0:n], ws[:, 0, c, j * 128:(j + 1) * 128], hT[:, c, c0:c0 + n]) for c in range(NCH)],
                             [wrb[s]] + hbufs, [bgb])
                        bu, bub = psring.next()
                        P.mm([(bu[:, 0:n], ws[:, 1, c, j * 128:(j + 1) * 128], hT[:, c, c0:c0 + n]) for c in range(NCH)],
                             [wrb[s]] + hbufs, [bub])
                        t_, tb = tmpring.next()
                        P.op(ACT, "activation", [bgb], [tb], t_[:, 0:n], bg[:, 0:n], AF.Silu)
                        P.op(DVE, "tensor_tensor", [tb, bub], [ab[J]], actT[:, J, c0:c0 + n], t_[:, 0:n], bu[:, 0:n], ALU.mult)
            wo = w_fout[l, i].rearrange("(j p) n -> p j n", p=128)
            for dc in range(NCH):
                s = wload([(0, 128, NFF, wo[:, :, dc * 128:(dc + 1) * 128])])
                ws = wr[s][:, 0:NFF * 128].rearrange("p (j n) -> p j n", n=128)
                for (c0, n) in slabs:
                    bk, bkb = psring.next()
                    P.mm([(bk[:, 0:n], ws[:, j, :], actT[:, j, c0:c0 + n]) for j in range(NFF)], [wrb[s]] + ab, [bkb])
                    P.op(ACT, "activation", [bkb], [yb[dc]], yT[:, dc, c0:c0 + n], bk[:, 0:n], AF.Copy)
            post_norm(l, 1 if i == 0 else 5, True, slabs, nt)

        def out_proj(wmat, slabs, nt):
            P.barrier()
            wv = wmat.rearrange("(c p) n -> p c n", p=128)
            for dcb in range(4):
                s = wload([(0, 512, 16, wv[:, :, dcb * 512:(dcb + 1) * 512])])
                ws = wr[s][:, :].rearrange("p (c n) -> p c n", n=512)
                for k in range(4):
                    dc = dcb * 4 + k
                    for (c0, n) in slabs:
                        bk, bkb = psring.next()
                        P.mm([(bk[:, 0:n], ws[:, c, k * 128:(k + 1) * 128], ob[:, c, c0:c0 + n]) for c in range(NCH)],
                             [wrb[s]] + yb[NCH // 2:], [bkb])
                        P.op(ACT, "activation", [bkb], [ymb[dc]], ym[:, dc, c0:c0 + n], bk[:, 0:n], AF.Copy)

        def carver():
            off = [0]

            def carve(nwords, dt=F32):
                v = work[:, off[0]:off[0] + nwords]
                off[0] += nwords
                assert off[0] <= WORKW, off[0]
                if dt == BF16:
                    v = v.bitcast(BF16)
                return v
            return carve

        def hgrn(l, p, slabs, nt):
            a = l // 2
            last = (p == NP - 1)
            has_s = (p == 0)
            pre_norm(l, 2, slabs, nt)
            carve = carver()
            sets = []
            for _ in range(2):
                d_ = {}
                for nm in ("qf", "sg", "kk", "bb", "dd", "dl", "sgl"):
                    d_[nm] = (carve(NTMAX), Buf())
                for nm in ("vbf", "qt", "kt", "qh", "kh"):
                    d_[nm] = (carve(NTMAX // 2, BF16), Buf())
                d_["At"] = (carve(256, BF16), Buf())
                d_["Ats"] = (carve(8, BF16), Buf())
                d_["khT"] = (carve(512, BF16), Buf())
                d_["vT"] = (carve(512, BF16), Buf())
                d_["khTs"] = (carve(256, BF16), Buf())
                d_["vTs"] = (carve(256, BF16), Buf())
                d_["Sbf"] = (carve(512, BF16), Buf())
                d_["Ss"] = (carve(512), Buf())
                d_["Sbs"] = (carve(256, BF16), Buf())
                d_["Sw"] = (carve(128), Buf())
                sets.append(d_)
            wv = w_hin[a].rearrange("(c p) n -> p c n", p=128)
            npc = TP // HC

            def stage_P(h):
                S_ = sets[h % 2]
                s = wload([(k * 2048, 128, 16, wv[:, :, k * 2048 + h * 128:k * 2048 + (h + 1) * 128]) for k in range(4)])
                ws = wr[s][:, :].rearrange("p (k c n) -> p k c n", k=4, c=16)
                for (c0, n) in slabs:
                    banks = []
                    for k in range(4):
                        bk, bkb = psring.next()
                        P.mm([(bk[:, 0:n], ws[:, k, c, :], hT[:, c, c0:c0 + n]) for c in range(NCH)], [wrb[s]] + hbufs, [bkb])
                        banks.append((bk, bkb))
                    sl = slice(c0, c0 + n)
                    P.op(ACT, "activation", [banks[0][1]], [S_["qf"][1]], S_["qf"][0][:, sl], banks[0][0][:, 0:n], AF.Copy)
                    P.op(ACT, "activation", [banks[1][1]], [S_["sg"][1]], S_["sg"][0][:, sl], banks[1][0][:, 0:n], AF.Sigmoid)
                    P.op(DVE, "tensor_copy", [banks[2][1]], [S_["vbf"][1]], S_["vbf"][0][:, sl], banks[2][0][:, 0:n])
                    P.op(ACT, "activation", [banks[3][1]], [S_["sgl"][1]], S_["sgl"][0][:, sl], banks[3][0][:, 0:n], AF.Silu)
                if p == 0:
                    P.op(DVE, "memset", [], [S_["Sw"][1]], S_["Sw"][0], 0.0)
                else:
                    P.dma(SP, S_["Sw"][0], sscr[a, h], [], [S_["Sw"][1]])

            def cview(t_, n_chunks, csz, base=0):
                return t_[:, base:base + n_chunks * csz].rearrange("p (c t) -> p c t", t=csz)

            def stage_E(h):
                S_ = sets[h % 2]
                g = lambda nm: S_[nm][0]
                B = lambda nm: S_[nm][1]
                hi = a * 16 + h
                N_ = slice(0, nt)
                P.op(DVE, "tensor_scalar", [B("sg"), cb], [B("sg")], g("sg")[:, N_], g("sg")[:, N_],
                     oml_sb[:, hi:hi + 1], lb_sb[:, hi:hi + 1], ALU.mult, ALU.add)
                P.op(DVE, "tensor_scalar", [B("sg")], [B("kk")], g("kk")[:, N_], g("sg")[:, N_], -1.0, 1.0, ALU.mult, ALU.add)
                P.op(ACT, "activation", [B("sg")], [B("sg")], g("sg")[:, N_], g("sg")[:, N_], AF.Ln)
                P.op(DVE, "tensor_tensor_scan", [B("sg"), cb], [B("bb")], g("bb")[:, N_], rmask[:, N_], g("sg")[:, N_], 0.0, ALU.mult, ALU.add)
                parts = [(npc, HC, 0)] + ([(NSEQ_S, LS, TP)] if has_s else [])
                for (ncx, csz, base) in parts:
                    bv = cview(g("bb"), ncx, csz, base)
                    mid = csz // 2 - 1
                    P.op(DVE, "tensor_tensor", [B("bb")], [B("dd")], cview(g("dd"), ncx, csz, base), bv,
                         bv[:, :, mid:mid + 1].to_broadcast([128, ncx, csz]), ALU.subtract)
                    P.op(DVE, "tensor_tensor", [B("bb")], [B("dl")], cview(g("dl"), ncx, csz, base),
                         bv[:, :, csz - 1:csz].to_broadcast([128, ncx, csz]), bv, ALU.subtract)
                P.op(ACT, "activation", [B("dd")], [B("dd")], g("dd")[:, N_], g("dd")[:, N_], AF.Exp)
                P.op(DVE, "tensor_tensor", [B("qf"), B("dd")], [B("qt")], g("qt")[:, N_], g("qf")[:, N_], g("dd")[:, N_], ALU.mult)
                P.op(DVE, "reciprocal", [B("dd")], [B("dd")], g("dd")[:, N_], g("dd")[:, N_])
                P.op(DVE, "tensor_tensor", [B("kk"), B("dd")], [B("kt")], g("kt")[:, N_], g("kk")[:, N_], g("dd")[:, N_], ALU.mult)
                P.op(ACT, "activation", [B("bb")], [B("bb")], g("bb")[:, N_], g("bb")[:, N_], AF.Exp)
                P.op(DVE, "tensor_tensor", [B("qf"), B("bb")], [B("qh")], g("qh")[:, N_], g("qf")[:, N_], g("bb")[:, N_], ALU.mult)
                P.op(ACT, "activation", [B("dl")], [B("dl")], g("dl")[:, N_], g("dl")[:, N_], AF.Exp)
                P.op(DVE, "tensor_tensor", [B("kk"), B("dl")], [B("kh")], g("kh")[:, N_], g("kk")[:, N_], g("dl")[:, N_], ALU.mult)

            def stage_T(h):
                S_ = sets[h % 2]
                g = lambda nm: S_[nm][0]
                B = lambda nm: S_[nm][1]
                bk, bkb = psring.next()
                P.mm([(bk[0:HC, ci * HC:(ci + 1) * HC], g("kt")[:, ci * HC:(ci + 1) * HC], g("qt")[:, ci * HC:(ci + 1) * HC], True, True)
                      for ci in range(npc)], [B("kt"), B("qt")], [bkb])
                At = g("At")[0:HC, :].rearrange("p (c t) -> p c t", t=HC)
                P.op(DVE, "tensor_tensor", [bkb, cb], [B("At")], At, bk[0:HC, :].rearrange("p (c t) -> p c t", t=HC),
                     cm64[:, :].unsqueeze(1).to_broadcast([HC, npc, HC]), ALU.mult)
                bk2, bk2b = psring.next()
                k16 = bk2.bitcast(BF16)
                P.mm([(k16[0:HC, ci * 128:(ci + 1) * 128], g("kh")[:, ci * HC:(ci + 1) * HC], ident16[:]) for ci in range(npc)],
                     [B("kh"), cb], [bk2b], transpose=True)
                P.op(ACT, "activation", [bk2b], [B("khT")], g("khT")[0:HC, :], k16[0:HC, :], AF.Copy)
                bk3, bk3b = psring.next()
                v16 = bk3.bitcast(BF16)
                P.mm([(v16[0:HC, ci * 128:(ci + 1) * 128], g("vbf")[:, ci * HC:(ci + 1) * HC], ident16[:]) for ci in range(npc)],
                     [B("vbf"), cb], [bk3b], transpose=True)
                P.op(DVE, "tensor_copy", [bk3b], [B("vT")], g("vT")[0:HC, :], v16[0:HC, :])
                if has_s:
                    bk4, bk4b = psring.next()
                    P.mm([(bk4[0:LS, i * LS:(i + 1) * LS], g("kt")[:, TP + i * LS:TP + (i + 1) * LS], g("qt")[:, TP + i * LS:TP + (i + 1) * LS], True, True)
                          for i in range(NSEQ_S)], [B("kt"), B("qt")], [bk4b])
                    P.op(DVE, "tensor_tensor", [bk4b, cb], [B("Ats")], g("Ats")[0:LS, :].rearrange("p (c t) -> p c t", t=LS),
                         bk4[0:LS, 0:NS].rearrange("p (c t) -> p c t", t=LS),
                         cm64[0:LS, 0:LS].unsqueeze(1).to_broadcast([LS, NSEQ_S, LS]), ALU.mult)
                    bk5, bk5b = psring.next()
                    s16 = bk5.bitcast(BF16)
                    P.mm([(s16[0:LS, i * 128:(i + 1) * 128], g("kh")[:, TP + i * LS:TP + (i + 1) * LS], ident16[:]) for i in range(NSEQ_S)]
                         + [(s16[0:LS, 512 + i * 128:512 + (i + 1) * 128], g("vbf")[:, TP + i * LS:TP + (i + 1) * LS], ident16[:]) for i in range(NSEQ_S)],
                         [B("kh"), B("vbf"), cb], [bk5b], transpose=True)
                    P.op(ACT, "activation", [bk5b], [B("khTs")], g("khTs")[0:LS, :], s16[0:LS, 0:512], AF.Copy)
                    P.op(ACT, "activation", [bk5b], [B("vTs")], g("vTs")[0:LS, :], s16[0:LS, 512:1024], AF.Copy)

            def stage_U(h):
                S_ = sets[h % 2]
                g = lambda nm: S_[nm][0]
                B = lambda nm: S_[nm][1]
                khT = g("khT")[0:HC, :].rearrange("p (c k) -> p c k", k=128)
                vT = g("vT")[0:HC, :].rearrange("p (c k) -> p c k", k=128)
                Sbf = g("Sbf").rearrange("p (c k) -> p c k", k=128)
                S = g("Sw")
                for half in range(npc // 4):
                    bk, bkb = psring.next()
                    P.mm([(bk[:, k * 128:(k + 1) * 128], khT[:, half * 4 + k, :], vT[:, half * 4 + k, :], True, True) for k in range(4)],
                         [B("khT"), B("vT")], [bkb])
                    for k in range(4):
                        ci = half * 4 + k
                        P.op(DVE, "tensor_copy", [B("Sw")], [B("Sbf")], Sbf[:, ci, :], S)
                        P.op(DVE, "scalar_tensor_tensor", [B("bb"), bkb], [B("Sw")], S, S,
                             g("bb")[:, ci * HC + HC - 1:ci * HC + HC], bk[:, k * 128:(k + 1) * 128], ALU.mult, ALU.add)
                if last:
                    P.dma(SP, hg_p[a, h], S, [B("Sw")], [])
                else:
                    P.dma(SP, sscr[a, h], S, [B("Sw")], [])
                if has_s:
                    Ss = g("Ss").rearrange("p (i k) -> p i k", k=128)
                    P.dma(SP, Ss, st_in[a, :, h].rearrange("i k v -> k i v"), [], [B("Ss")])
                    P.op(DVE, "tensor_copy", [B("Ss")], [B("Sbs")], g("Sbs"), g("Ss"))
                    bk, bkb = psring.next()
                    khTs = g("khTs")[0:LS, :].rearrange("p (i k) -> p i k", k=128)
                    vTs = g("vTs")[0:LS, :].rearrange("p (i k) -> p i k", k=128)
                    P.mm([(bk[:, i * 128:(i + 1) * 128], khTs[:, i, :], vTs[:, i, :], True, True) for i in range(NSEQ_S)],
                         [B("khTs"), B("vTs")], [bkb])
                    dec = g("bb")[:, TP:TP + NS].rearrange("p (i t) -> p i t", t=LS)[:, :, LS - 1:LS]
                    P.op(DVE, "tensor_tensor", [B("bb")], [B("Ss")], Ss, Ss, dec.to_broadcast([128, NSEQ_S, 128]), ALU.mult)
                    P.op(DVE, "tensor_tensor", [bkb], [B("Ss")], Ss, Ss, bk[:, :].rearrange("p (i k) -> p i k", k=128), ALU.add)
                    P.dma(SP, hg_s[a, :, h].rearrange("i k v -> k i v"), Ss, [B("Ss")], [])

            def stage_O(h):
                S_ = sets[h % 2]
                g = lambda nm: S_[nm][0]
                B = lambda nm: S_[nm][1]
                At = g("At")[0:HC, :].rearrange("p (c t) -> p c t", t=HC)
                vT = g("vT")[0:HC, :].rearrange("p (c k) -> p c k", k=128)
                Sbf = g("Sbf").rearrange("p (c k) -> p c k", k=128)
                obank = []
                bk, bkb = psring.next()
                mms = []
                for ci in range(npc):
                    o_ = bk[:, ci * HC:(ci + 1) * HC]
                    mms.append((o_, vT[:, ci, :], At[:, ci, :], True, False))
                    mms.append((o_, Sbf[:, ci, :], g("qh")[:, ci * HC:(ci + 1) * HC], False, True))
                P.mm(mms, [B("vT"), B("At"), B("Sbf"), B("qh")], [bkb])
                obank.append((bk, bkb, 0, TP))
                if has_s:
                    bk2, bk2b = psring.next()
                    Ats = g("Ats")[0:LS, :].rearrange("p (c t) -> p c t", t=LS)
                    vTs = g("vTs")[0:LS, :].rearrange("p (i k) -> p i k", k=128)
                    Sbs = g("Sbs").rearrange("p (i k) -> p i k", k=128)
                    mms = []
                    for i in range(NSEQ_S):
                        o_ = bk2[:, i * LS:(i + 1) * LS]
                        mms.append((o_, vTs[:, i, :], Ats[:, i, :], True, False))
                        mms.append((o_, Sbs[:, i, :], g("qh")[:, TP + i * LS:TP + (i + 1) * LS], False, True))
                    P.mm(mms, [B("vTs"), B("Ats"), B("Sbs"), B("qh")], [bk2b])
                    obank.append((bk2, bk2b, TP, NS))
                hi = a * 16 + h
                for (bk_, bkb_, c0, n) in obank:
                    sl = slice(c0, c0 + n)
                    P.op(ACT, "activation", [bkb_], [B("dd")], g("dd")[:, sl], bk_[:, 0:n], AF.Square)
                    bn, bnb = psring.next()
                    P.mm([(bn[:, 0:n], ones32[:], g("dd")[:, sl], True, True)], [B("dd"), cb], [bnb])
                    P.op(ACT, "activation", [bnb], [B("dl")], g("dl")[:, sl], bn[:, 0:n], AF.Sqrt, bias=EPS, scale=1.0 / 128)
                    P.op(DVE, "reciprocal", [B("dl")], [B("dl")], g("dl")[:, sl], g("dl")[:, sl])
                    P.op(DVE, "scalar_tensor_tensor", [bkb_, B("dl"), cb], [B("dd")], g("dd")[:, sl], bk_[:, 0:n],
                         hgn_sb[:, hi:hi + 1], g("dl")[:, sl], ALU.mult, ALU.mult)
                    P.op(DVE, "tensor_tensor", [B("dd"), B("sgl")], [obb[h]], ob[:, h, sl], g("dd")[:, sl],
                         g("sgl")[:, sl], ALU.mult)

            stage_P(0)
            stage_E(0)
            for h in range(16):
                if h + 1 < 16:
                    stage_P(h + 1)
                stage_T(h)
                stage_U(h)
                stage_O(h)
                if h + 1 < 16:
                    stage_E(h + 1)
            out_proj(w_hout[a], slabs, nt)
            post_norm(l, 3, False, slabs, nt, ym, ymb)

        def swa(l, p, slabs, nt):
            b = l // 2
            last = (p == NP - 1)
            has_s = (p == 0)
            pre_norm(l, 2, slabs, nt)
            carve = carver()
            k32 = carve(4 * (WIN + NS)).rearrange("p (j n) -> p j n", n=WIN + NS); k32b = Buf()
            vl32 = carve(512); vl32b = Buf()
            vsst_r = Ring([(carve(128), Buf()) for _ in range(2)])
            krp = carve(4 * (WIN + TP) // 2, BF16).rearrange("p (j n) -> p j n", n=WIN + TP); krpb = [Buf() for _ in range(4)]
            vtm = carve(5 * 512 // 2, BF16).rearrange("p (t n) -> p t n", n=512); vtmb = [Buf() for _ in range(5)]
            qsets = []
            for _ in range(2):
                qsets.append((carve(4 * NTMAX // 2, BF16).rearrange("p (c n) -> p c n", n=NTMAX), [Buf() for _ in range(4)]))
            vbf = carve(NTMAX // 2, BF16); vbfb = Buf()
            qf_r = Ring([(carve(NTMAX), Buf()) for _ in range(2)])
            t2_r = Ring([(carve(NTMAX), Buf()) for _ in range(2)])
            r32_r = Ring([(carve(NTMAX), Buf()) for _ in range(2)])
            pe_r = Ring([(carve(256), Buf()) for _ in range(3)])
            pm_r = Ring([(carve(256), Buf()) for _ in range(3)])
            pn_r = Ring([(carve(256), Buf()) for _ in range(3)])
            pT_r = Ring([(carve(128, BF16), Buf()) for _ in range(3)])
            sm_r = Ring([(carve(8), Buf()) for _ in range(4)])
            stg = carve(512); stgb = Buf()
            if has_s:
                qs = carve(4 * 64 // 2, BF16).rearrange("p (j i c t) -> p j i c t", j=4, i=4, c=4); qsb = Buf()
                kcs = carve(16 * 132 // 2, BF16).rearrange("p (j i n) -> p j i n", j=4, i=4); kcsb = Buf()
                vcs = carve(4 * 512 // 2, BF16).rearrange("p (i n) -> p i n", n=512); vcsb = Buf()
                vns = carve(4 * 512 // 2, BF16).rearrange("p (i n) -> p i n", n=512); vnsb = Buf()
                kc32 = carve(512); kc32b = Buf()

            P.op(DVE, "tensor_copy", [kvpb[b]], krpb, krp[:, :, 0:WIN], kprev[b][:, :, :])
            P.op(DVE, "tensor_copy", [kvpb[b]], [vtmb[0]], vtm[:, 0, :], vprev[b][:, :])
            P.dma(SP, cosT[:, 0:TP], cosT_d[:, p * TP:(p + 1) * TP], [], [ropeb])
            P.dma(SP, sinT[:, 0:TP], sinT_d[:, p * TP:(p + 1) * TP], [], [ropeb])
            if has_s:
                P.dma(SP, cosT[:, TP:TP + NS], cosT_d[:, cfg.seq:cfg.seq + NS], [], [ropeb])
                P.dma(SP, sinT[:, TP:TP + NS], sinT_d[:, cfg.seq:cfg.seq + NS], [], [ropeb])

            wv = w_ain[b].rearrange("(c p) n -> p c n", p=128)

            def rope(bank, bankb, n, c0, outs):
                qf, qfb = qf_r.next()
                P.op(ACT, "activation", [bankb], [qfb], qf[:, 0:n], bank[:, 0:n], AF.Copy)
                if "sR" in DBG_SKIP:
                    for (en, out_ap, bufs, cs, v3) in outs:
                        src = qf[:, cs]
                        if v3:
                            src = src.rearrange("p (i t) -> p i t", t=LS)
                        P.op(DVE, "tensor_copy", [qfb], bufs, out_ap, src)
                    return
                br, brb = psring.next()
                P.mm([(br[:, 0:n], rotm[:], qf[:, 0:n], True, True)], [qfb, cb], [brb])
                t2, t2b = t2_r.next()
                P.op(DVE, "tensor_tensor", [brb, ropeb], [t2b], t2[:, 0:n], br[:, 0:n], sinT[:, c0:c0 + n], ALU.mult)
                P.op(DVE, "tensor_tensor", [qfb, ropeb], [qfb], qf[:, 0:n], qf[:, 0:n], cosT[:, c0:c0 + n], ALU.mult)
                r32, r32b = r32_r.next()
                P.op(DVE, "tensor_tensor", [qfb, t2b], [r32b], r32[:, 0:n], qf[:, 0:n], t2[:, 0:n], ALU.add)
                for (en, out_ap, bufs, cs, v3) in outs:
                    src = r32[:, cs]
                    if v3:
                        src = src.rearrange("p (i t) -> p i t", t=LS)
                    if en == "act":
                        P.op(ACT, "activation", [r32b], bufs, out_ap, src, AF.Copy)
                    else:
                        P.op(DVE, "tensor_copy", [r32b], bufs, out_ap, src)

            def stage_P(jp):
                qr, qrb = qsets[jp % 2]
                s1 = wload([(0, 512, 16, wv[:, :, jp * 512:(jp + 1) * 512])])
                ws1 = wr[s1][:, :].rearrange("p (c n) -> p c n", n=512)
                for (c0, n) in slabs:
                    for c in range(4):
                        bk, bkb = psring.next()
                        P.mm([(bk[:, 0:n], ws1[:, k, c * 128:(c + 1) * 128], hT[:, k, c0:c0 + n]) for k in range(NCH)],
                             [wrb[s1]] + hbufs, [bkb])
                        rope(bk, bkb, n, c0, [("act", qr[:, c, c0:c0 + n], [qrb[c]], slice(0, n), False)])
                s2 = wload([(0, 128, 16, wv[:, :, 2048 + jp * 128:2048 + (jp + 1) * 128]),
                            (2048, 128, 16, wv[:, :, 2560 + jp * 128:2560 + (jp + 1) * 128])])
                ws2 = wr[s2][:, 0:4096].rearrange("p (g c n) -> p g c n", g=2, c=16)
                for (c0, n) in slabs:
                    bk, bkb = psring.next()
                    P.mm([(bk[:, 0:n], ws2[:, 0, k, :], hT[:, k, c0:c0 + n]) for k in range(NCH)], [wrb[s2]] + hbufs, [bkb])
                    if "sK" in DBG_SKIP:
                        pass
                    elif c0 == 0:
                        rope(bk, bkb, n, c0, [("act", krp[:, jp, WIN:WIN + TP], [krpb[jp]], slice(0, n), False),
                                              ("dve", k32[:, jp, 0:WIN], [k32b], slice(TP - WIN, TP), False)])
                    else:
                        rope(bk, bkb, n, c0, [("act", kcs[:, jp, :, 128:132], [kcsb], slice(0, n), True),
                                              ("dve", k32[:, jp, WIN:WIN + NS], [k32b], slice(0, n), False)])
                    if c0 == 0:
                        for tt in range(4):
                            bt, btb = psring.next()
                            P.mm([(bt[:, 0:128], hT[:, k, tt * 128:(tt + 1) * 128], ws2[:, 1, k, :]) for k in range(NCH)],
                                 [wrb[s2]] + hbufs, [btb])
                            if tt == 3 and last:
                                P.op(ACT, "activation", [btb], [vl32b], vl32[:, jp * 128:(jp + 1) * 128], bt[:, 0:128], AF.Copy)
                                P.op(DVE, "tensor_copy", [vl32b], [vtmb[1 + tt]], vtm[:, 1 + tt, jp * 128:(jp + 1) * 128],
                                     vl32[:, jp * 128:(jp + 1) * 128])
                            else:
                                P.op(ACT, "activation", [btb], [vtmb[1 + tt]], vtm[:, 1 + tt, jp * 128:(jp + 1) * 128], bt[:, 0:128], AF.Copy)
                    else:
                        for i in range(NSEQ_S):
                            bt, btb = psring.next()
                            P.mm([(bt[0:LS, 0:128], hT[:, k, TP + i * LS:TP + (i + 1) * LS], ws2[:, 1, k, :]) for k in range(NCH)],
                                 [wrb[s2]] + hbufs, [btb])
                            vs_, vsb_ = vsst_r.next()
                            P.op(ACT, "activation", [btb], [vsb_], vs_[0:LS, :], bt[0:LS, 0:128], AF.Copy)
                            P.op(DVE, "tensor_copy", [vsb_], [vnsb], vns[0:LS, i, jp * 128:(jp + 1) * 128], vs_[0:LS, :])
                            P.dma(SP, vw_s[b, i, WIN - LS:WIN, jp * 128:(jp + 1) * 128], vs_[0:LS, :], [vsb_], [])

            def softmax_pv(S_ap, sbuf_b, rows, ncols, nsink_col, sink_col, mask_ap):
                sm, smb = sm_r.next()
                P.op(DVE, "reduce_max", [sbuf_b], [smb], sm[0:rows, 0:1], S_ap, AX.X)
                P.op(DVE, "tensor_scalar", [smb, cb], [smb], sm[0:rows, 1:2], sm[0:rows, 0:1], -0.125, nsink_col, ALU.mult, ALU.min)
                pe_, peb = pe_r.next()
                P.op(ACT, "activation", [sbuf_b, smb], [peb], pe_[0:rows, 0:ncols], S_ap, AF.Exp, bias=sm[0:rows, 1:2], scale=0.125)
                P.op(ACT, "activation", [smb, cb], [smb], sm[0:rows, 2:3], sink_col, AF.Exp, bias=sm[0:rows, 1:2], scale=1.0)
                pm, pmb = pm_r.next()
                P.op(DVE, "tensor_tensor", [peb, cb], [pmb], pm[0:rows, 0:ncols], pe_[0:rows, 0:ncols], mask_ap, ALU.mult)
                P.op(DVE, "reduce_sum", [pmb], [smb], sm[0:rows, 3:4], pm[0:rows, 0:ncols], AX.X)
                P.op(DVE, "tensor_tensor", [smb], [smb], sm[0:rows, 4:5], sm[0:rows, 3:4], sm[0:rows, 2:3], ALU.add)
                P.op(DVE, "reciprocal", [smb], [smb], sm[0:rows, 5:6], sm[0:rows, 4:5])
                pn, pnb = pn_r.next()
                P.op(ACT, "activation", [pmb, smb], [pnb], pn[0:rows, 0:ncols], pm[0:rows, 0:ncols], AF.Copy, scale=sm[0:rows, 5:6])
                return pn, pnb

            def stage_A(jp):
                qr, qrb = qsets[jp % 2]
                for c in range(4):
                    bo, bob = accring.next()
                    for offi in range(2):
                        kv = 2 * jp + offi
                        hq = 4 * kv + c
                        psl = slice(offi * 64, offi * 64 + 64)
                        for qb in range(TP // 128):
                            bs, bsb = psring.next()
                            P.mm([(bs[:, 0:256], qr[psl, c, qb * 128:(qb + 1) * 128], krp[psl, jp, qb * 128:qb * 128 + 256], True, True)],
                                 [qrb[c], krpb[jp]], [bsb])
                            mask = mfirst if (p == 0 and qb == 0 and "nomf" not in DBG_SKIP) else mband
                            pn, pnb = softmax_pv(bs[:, 0:256], bsb, 128, 256, nsink_sb[:, b * 32 + hq:b * 32 + hq + 1],
                                                 sink_sb[:, b * 32 + hq:b * 32 + hq + 1], mask[:, :])
                            bt, btb = psring.next()
                            P.mm([(bt[:, kb * 128:(kb + 1) * 128], pn[:, kb * 128:(kb + 1) * 128], ident32[:]) for kb in range(2)],
                                 [pnb, cb], [btb], transpose=True)
                            pT, pTb = pT_r.next()
                            P.op(DVE, "tensor_copy", [btb], [pTb], pT[:, 0:256], bt[:, 0:256])
                            P.mm([(bo[psl, qb * 128:(qb + 1) * 128], vtm[:, qb + kb, jp * 128 + offi * 64:jp * 128 + offi * 64 + 64],
                                   pT[:, kb * 128:(kb + 1) * 128]) for kb in range(2)],
                                 [vtmb[qb], vtmb[qb + 1], pTb], [bob])
                    P.op(ACT, "activation", [bob], [obb[jp * 4 + c]], ob[:, jp * 4 + c, 0:TP], bo[:, 0:TP], AF.Copy)

            def sample_attn():
                for i in range(NSEQ_S):
                    P.dma(SP, kc32[:, :], ck_in[b, i], [], [kc32b])
                    bt, btb = psring.next()
                    P.mm([(bt[:, j * 128:(j + 1) * 128], kc32[:, j * 128:(j + 1) * 128], ident32[:]) for j in range(4)],
                         [kc32b, cb], [btb], transpose=True)
                    P.op(DVE, "tensor_copy", [btb], [kcsb], kcs[:, :, i, 0:128], bt[:, :].rearrange("p (j n) -> p j n", n=128))
                    P.dma(POOL, vcs[:, i, :], cv_in[b, i], [], [vcsb])
                for jp in range(4):
                    for offi in range(2):
                        kv = 2 * jp + offi
                        psl = slice(offi * 64, offi * 64 + 64)
                        for i in range(NSEQ_S):
                            bs, bsb = psring.next()
                            P.mm([(bs[0:16, 0:132], qs[psl, jp, i, :, :], kcs[psl, jp, i, :], True, True)], [qsb, kcsb], [bsb])
                            pn, pnb = softmax_pv(bs[0:16, 0:132], bsb, 16, 132, nsinks16[:, b * 8 + kv:b * 8 + kv + 1],
                                                 sinks16[:, b * 8 + kv:b * 8 + kv + 1], msamp[:, :])
                            bt, btb = psring.next()
                            P.mm([(bt[:, 0:16], pn[0:16, 0:128], ident32[0:16, 0:16]),
                                  (bt[0:LS, 16:32], pn[0:16, 128:132], ident32[0:16, 0:16])], [pnb, cb], [btb], transpose=True)
                            pT, pTb = pT_r.next()
                            P.op(DVE, "tensor_copy", [btb], [pTb], pT[:, 0:16], bt[:, 0:16])
                            P.op(DVE, "tensor_copy", [btb], [pTb], pT[0:LS, 16:32], bt[0:LS, 16:32])
                            bo, bob = psring.next()
                            P.mm([(bo[psl, 0:16], vcs[:, i, kv * 64:(kv + 1) * 64], pT[:, 0:16]),
                                  (bo[psl, 0:16], vns[0:LS, i, kv * 64:(kv + 1) * 64], pT[0:LS, 16:32])],
                                 [vcsb, vnsb, pTb], [bob])
                            P.op(ACT, "activation", [bob], obb[jp * 4:jp * 4 + 4],
                                 ob[psl, jp * 4:jp * 4 + 4, TP + i * LS:TP + (i + 1) * LS],
                                 bo[psl, 0:16].rearrange("p (c t) -> p c t", t=LS), AF.Copy)
                for i in range(NSEQ_S):
                    P.dma(SP, kw_s[b, i, 0:WIN - LS], ck_in[b, i, LS:WIN], [], [])
                    P.dma(SP, vw_s[b, i, 0:WIN - LS], cv_in[b, i, LS:WIN], [], [])
                    for (src32, srcb, dst) in ((k32, k32b, kw_s),):
                        bt, btb = psring.next()
                        P.mm([(bt[0:LS, j * 128:(j + 1) * 128], src32[:, j, WIN + i * LS:WIN + (i + 1) * LS], ident32[:]) for j in range(4)],
                             [srcb, cb], [btb], transpose=True)
                        P.op(ACT, "activation", [btb], [stgb], stg[0:LS, :], bt[0:LS, :], AF.Copy)
                        P.dma(SP, dst[b, i, WIN - LS:WIN], stg[0:LS, :], [stgb], [])

            def fill_qs(jp):
                qr, qrb = qsets[jp % 2]
                P.op(DVE, "tensor_copy", qrb, [qsb], qs[:, jp].rearrange("p i c t -> p c i t"),
                     qr[:, :, TP:TP + NS].rearrange("p c (i t) -> p c i t", t=LS))

            for i in range(5):
                if i < 4 and "sP" not in DBG_SKIP:
                    stage_P(i)
                    if has_s and "sQ" not in DBG_SKIP:
                        fill_qs(i)
                if i >= 1 and "sA" not in DBG_SKIP:
                    stage_A(i - 1)
            if has_s and "sS" not in DBG_SKIP:
                sample_attn()
            if last and "sC" not in DBG_SKIP:
                P.dma(SP, vw_p[b], vl32[:, :], [vl32b], [])
                for (src32, srcb, dst) in ((k32, k32b, kw_p),):
                    bt, btb = psring.next()
                    P.mm([(bt[:, j * 128:(j + 1) * 128], src32[:, j, 0:WIN], ident32[:]) for j in range(4)],
                         [srcb, cb], [btb], transpose=True)
                    P.op(ACT, "activation", [btb], [stgb], stg[:, :], bt[:, :], AF.Copy)
                    P.dma(SP, dst[b], stg[:, :], [stgb], [])
            if not last:
                P.op(DVE, "tensor_copy", krpb, [kvpb[b]], kprev[b][:, :, :], krp[:, :, TP:TP + WIN])
                P.op(DVE, "tensor_copy", [vtmb[4]], [kvpb[b]], vprev[b][:, :], vtm[:, 4, :])
            out_proj(w_aout[b], slabs, nt)
            post_norm(l, 3, False, slabs, nt, ym, ymb)

        def load_x(p, slabs):
            for (c0, n) in slabs:
                for t0 in range(0, n, 128):
                    r = min(128, n - t0)
                    xs_, xsb = xstring.next()
                    src = xp[p * TP + t0:p * TP + t0 + r, :] if c0 == 0 else xs[0:r, :]
                    P.dma(SP, xs_[0:r, :], src, [], [xsb])
                    for q in range(4):
                        bk, bkb = psring.next()
                        P.mm([(bk[:, k * 128:k * 128 + r], xs_[0:r, (4 * q + k) * 128:(4 * q + k + 1) * 128], ident32[0:r, 0:r]) for k in range(4)],
                             [xsb, cb], [bkb], transpose=True)
                        P.op(ACT, "activation", [bkb], xb[4 * q:4 * q + 4], xT[:, 4 * q:4 * q + 4, c0 + t0:c0 + t0 + r],
                             bk[:, :].rearrange("p (k t) -> p k t", t=128)[:, :, 0:r], AF.Copy)

        def store_x(p, slabs):
            for (c0, n) in slabs:
                for t0 in range(0, n, 128):
                    r = min(128, n - t0)
                    xs_, xsb = xstring.next()
                    for q in range(4):
                        bk, bkb = psring.next()
                        P.mm([(bk[0:r, k * 128:(k + 1) * 128], xT[:, 4 * q + k, c0 + t0:c0 + t0 + r], ident32[:]) for k in range(4)],
                             xb[4 * q:4 * q + 4] + [cb], [bkb], transpose=True)
                        P.op(ACT if q % 2 else DVE, "activation" if q % 2 else "tensor_copy", [bkb], [xsb],
                             xs_[0:r, q * 512:(q + 1) * 512], bk[0:r, :], *([AF.Copy] if q % 2 else []))
                    dst = yp[p * TP + t0:p * TP + t0 + r, :] if c0 == 0 else ys[0:r, :]
                    P.dma(SP, dst, xs_[0:r, :], [xsb], [])

        for p in range(NP):
            slabs = slabs_of(p)
            nt = sum(n for _, n in slabs)
            P.barrier()
            load_x(p, slabs)
            P.barrier()
            for l in range(L):
                if "ffn" not in DBG_SKIP:
                    ffn(l, 0, slabs, nt)
                P.barrier()
                if l % 2 == 0:
                    if "hgrn" not in DBG_SKIP:
                        hgrn(l, p, slabs, nt)
                else:
                    if "swa" not in DBG_SKIP:
                        swa(l, p, slabs, nt)
                P.barrier()
                if "ffn" not in DBG_SKIP:
                    ffn(l, 1, slabs, nt)
            P.barrier()
            store_x(p, slabs)
        P.barrier()
    return nc


def _attn_perm():
    idx = np.empty(2048, np.int64)
    for jp in range(4):
        for c in range(4):
            for off in range(2):
                h = 4 * (2 * jp + off) + c
                base = jp * 512 + c * 128 + off * 64
                idx[base:base + 64] = h * 64 + np.arange(64)
    return idx


_CACHE = {}


def run(cfg, inputs):
    ncores = int(os.environ.get('K_NCORES', '8'))
    key = (cfg.depth, cfg.seq, cfg.dff)
    if key not in _CACHE:
        _CACHE[key] = build(cfg)
    nc = _CACHE[key]
    f = lambda a: np.ascontiguousarray(np.asarray(a, dtype=np.float32))
    L, NA, NB = cfg.depth, cfg.na, cfg.nb
    perm = _attn_perm()
    gains = f(np.asarray(inputs["norm_gains"]).reshape(L, 6, NCH, 128).transpose(3, 0, 1, 2).reshape(128, L * 6 * NCH))
    lbl = f(np.asarray(inputs["hgrn_lb_logits"]).reshape(NA, 16, 128).transpose(2, 0, 1).reshape(128, NA * 16))
    hgn = f(np.asarray(inputs["hgrn_norm_gain"]).reshape(NA, 16, 128).transpose(2, 0, 1).reshape(128, NA * 16))
    if NB:
        wa = np.asarray(inputs["w_attn_in"])
        w_ain = f(np.concatenate([wa[:, :, :2048][:, :, perm], wa[:, :, 2048:]], axis=2))
        w_aout = f(np.asarray(inputs["w_attn_out"])[:, perm, :])
        sinks = f(inputs["attn_sinks"])
        ck = np.asarray(inputs["cache_k_win"]).reshape(NB, cfg.dec_batch, WIN, 512)
        cv = np.asarray(inputs["cache_v_win"]).reshape(NB, cfg.dec_batch, WIN, 512)
    else:
        w_ain = np.zeros((1, D, 3072), np.float32); w_aout = np.zeros((1, D, D), np.float32)
        sinks = np.zeros((1, 32), np.float32)
        ck = np.zeros((1, cfg.dec_batch, WIN, 512), np.float32); cv = ck
    consts = host_consts(cfg)
    shared = {
        "gains": gains, "w_fin": f(inputs["w_ffn_in"]), "w_fout": f(inputs["w_ffn_out"]),
        "w_hin": f(inputs["w_hgrn_in"]), "lbl": lbl, "hgn": hgn, "w_hout": f(inputs["w_hgrn_out"]),
        "w_ain": w_ain, "sinks_b": sinks, "w_aout": w_aout,
        "sinks16_d": f(np.repeat(sinks.reshape(max(NB, 1), 8, 4).transpose(2, 0, 1).reshape(4, max(NB, 1) * 8), 4, axis=0)),
    }
    for k, v in consts.items():
        shared["c_" + k] = f(v)
    xp_all = np.asarray(inputs["x_prompt"]); xs_all = np.asarray(inputs["x_sample"])
    st_all = np.asarray(inputs["state_hgrn"])
    in_maps = []
    for c in range(ncores):
        m = dict(shared)
        m["xp"] = f(xp_all[c % cfg.batch])
        sl = slice(c * NSEQ_S, (c + 1) * NSEQ_S)
        m["xs"] = f(xs_all[sl].reshape(NS, D))
        m["st_in"] = f(st_all[:, sl])
        m["ck_in"] = f(ck[:, sl]); m["cv_in"] = f(cv[:, sl])
        in_maps.append(m)
    res = run_bass_kernel_spmd(nc, in_maps, core_ids=list(range(ncores)))
    R = res.results
    yp = np.stack([R[b_ % ncores]["yp"] for b_ in range(cfg.batch)])
    ys = np.concatenate([R[c]["ys"].reshape(NSEQ_S, LS, D) for c in range(ncores)], axis=0)
    hgp = np.stack([R[b_ % ncores]["hg_p"] for b_ in range(cfg.batch)], axis=1)
    hgs = np.concatenate([R[c]["hg_s"] for c in range(ncores)], axis=1)
    out = [yp, ys, hgp, hgs]
    kp = np.stack([R[b_ % ncores]["kw_p"][:NB] for b_ in range(cfg.batch)], axis=1).reshape(NB, cfg.batch, WIN, 8, 64)
    vp = np.stack([R[b_ % ncores]["vw_p"][:NB] for b_ in range(cfg.batch)], axis=1).reshape(NB, cfg.batch, WIN, 8, 64)
    ks = np.concatenate([R[c]["kw_s"][:NB] for c in range(ncores)], axis=1).reshape(NB, ncores * NSEQ_S, WIN, 8, 64)
    vs = np.concatenate([R[c]["vw_s"][:NB] for c in range(ncores)], axis=1).reshape(NB, ncores * NSEQ_S, WIN, 8, 64)
    out += [kp, vp, ks, vs]
    return tuple(np.ascontiguousarray(o, dtype=np.float32) for o in out)


def kernel(**inputs):
    return run(Cfg(), inputs)
```

```python
import contextlib
import numpy as np
import concourse.bass as bass
import concourse.mybir as mybir
from concourse.bass_utils import run_bass_kernel_spmd

F32 = mybir.dt.float32
BF16 = mybir.dt.bfloat16
AF = mybir.ActivationFunctionType
ALU = mybir.AluOpType
AX = mybir.AxisListType

D = 2048
NCH = 16
TP = 512
NSEQ_S = 4
LS = 4
NS = NSEQ_S * LS
NTMAX = TP + NS
WIN = 128
PAST = 16384
EPS = 1e-6
HC = 64
import os
DBG_SKIP = os.environ.get('K_SKIP', '').split(',')
WORKW = 16512


class Cfg:
    def __init__(self, depth=4, seq=2048, dff=5632, batch=4, dec_batch=32):
        self.depth, self.seq, self.dff, self.batch, self.dec_batch = depth, seq, dff, batch, dec_batch
        self.npass = seq // TP
        self.nff = dff // 128
        self.na = (depth + 1) // 2
        self.nb = depth // 2


class Buf:
    __slots__ = ("w", "r")

    def __init__(self):
        self.w = None
        self.r = {}


class Eng:
    def __init__(self, h, sem, is_pe=False):
        self.h, self.sem, self.cnt, self.waited, self.is_pe = h, sem, 0, {}, is_pe

    def wait(self, dep):
        sem, val = dep
        if sem is self.sem and self.is_pe:
            return
        k = id(sem)
        if self.waited.get(k, 0) >= val:
            return
        self.waited[k] = val
        self.h.wait_ge(sem, val)


class Prog:
    def __init__(self, nc, es):
        self.nc, self.es = nc, es
        sm = lambda n: es.enter_context(nc.semaphore(n))
        self.pe = Eng(nc.tensor, sm("s_pe"), True)
        self.act = Eng(nc.scalar, sm("s_act"))
        self.dve = Eng(nc.vector, sm("s_dve"))
        self.pool = Eng(nc.gpsimd, sm("s_pool"))
        self.sp = Eng(nc.sync, sm("s_sp"))
        self.dsems = [[sm("s_d%d" % i), 0] for i in range(12)]
        self.dnext = 0
        self.psems = [[sm("s_q%d" % i), 0] for i in range(2)]
        self.pnext = 0

    def _deps(self, eng, reads, writes):
        for b in reads:
            if b.w is not None:
                eng.wait(b.w)
        for b in writes:
            if b.w is not None:
                eng.wait(b.w)
            for d in b.r.values():
                eng.wait(d)

    def _mark(self, dep, reads, writes):
        for b in reads:
            b.r[id(dep[0])] = dep
        for b in writes:
            b.w = dep
            b.r = {}

    def op(self, eng, name, reads, writes, *a, **k):
        self._deps(eng, reads, writes)
        ins = getattr(eng.h, name)(*a, **k)
        eng.cnt += 1
        ins.then_inc(eng.sem, 1)
        self._mark((eng.sem, eng.cnt), reads, writes)

    def mm(self, mms, reads, writes, transpose=False):
        eng = self.pe
        self._deps(eng, reads, writes)
        n = len(mms)
        ins = None
        for i, m in enumerate(mms):
            if transpose:
                ins = eng.h.transpose(m[0], m[1], m[2])
            else:
                st = m[3] if len(m) > 3 else (i == 0)
                sp = m[4] if len(m) > 4 else (i == n - 1)
                ins = eng.h.matmul(m[0], m[1], m[2], start=st, stop=sp)
        eng.cnt += 1
        ins.then_inc(eng.sem, 1)
        self._mark((eng.sem, eng.cnt), reads, writes)

    def dma(self, eng, out, in_, reads, writes, semslot=None, **k):
        if semslot is None and eng is self.pool:
            semslot = self.psems[self.pnext]
            self.pnext = (self.pnext + 1) % len(self.psems)
        elif semslot is None:
            semslot = self.dsems[self.dnext]
            self.dnext = (self.dnext + 1) % len(self.dsems)
        sem = semslot[0]
        if semslot[1] > 0:
            eng.wait((sem, semslot[1]))
        self._deps(eng, reads, writes)
        eng.h.dma_start(out=out, in_=in_, **k).then_inc(sem, 16)
        semslot[1] += 16
        self._mark((sem, semslot[1]), reads, writes)

    def barrier(self):
        engs = [self.pe, self.act, self.dve, self.pool, self.sp]
        for e in engs:
            for f in engs:
                if f is not e and f.cnt > 0:
                    e.wait((f.sem, f.cnt))
            for s in self.dsems + self.psems:
                if s[1] > 0:
                    e.wait((s[0], s[1]))


class Ring:
    def __init__(self, items):
        self.items, self.i = items, 0

    def next(self):
        it = self.items[self.i]
        self.i = (self.i + 1) % len(self.items)
        return it


def host_consts(cfg):
    c = {}
    c["ident32"] = np.eye(128, dtype=np.float32)
    c["ones32"] = np.ones((128, 128), np.float32)
    R = np.zeros((128, 128), np.float32)
    for blk in range(2):
        for j in range(32):
            R[blk * 64 + j + 32, blk * 64 + j] = -1.0
            R[blk * 64 + j, blk * 64 + j + 32] = 1.0
    c["rotm"] = R
    half = 32
    inv = np.power(np.float32(10000.0), -(np.arange(half, dtype=np.float32) * np.float32(2.0 / 64))).astype(np.float32)
    pos = np.concatenate([np.arange(cfg.seq, dtype=np.float32),
                          (PAST + np.arange(LS)).astype(np.float32)])
    ang = (pos[None, :] * inv[:, None]).astype(np.float32)
    cos = np.cos(ang.astype(np.float64)).astype(np.float32)
    sin = np.sin(ang.astype(np.float64)).astype(np.float32)
    cosT = np.tile(cos, (4, 1))
    sinT = np.tile(sin, (4, 1))
    c["cosT"] = np.concatenate([cosT[:, :cfg.seq]] + [cosT[:, cfg.seq:]] * NSEQ_S, axis=1).copy()
    c["sinT"] = np.concatenate([sinT[:, :cfg.seq]] + [sinT[:, cfg.seq:]] * NSEQ_S, axis=1).copy()
    rm = np.ones((128, NTMAX), np.float32)
    rm[:, 0:TP:HC] = 0.0
    rm[:, TP::LS] = 0.0
    c["rmask"] = rm
    c["cm64"] = np.triu(np.ones((HC, HC), np.float32))
    t = np.arange(128)[:, None]
    s = np.arange(256)[None, :]
    band = ((s >= t) & (s <= t + 128)).astype(np.float32)
    first = band * (s >= 128)
    c["mband"] = band
    c["mfirst"] = first.astype(np.float32)
    tt = (np.arange(16) % 4)[:, None]
    j = np.arange(132)[None, :]
    c["msamp"] = ((j >= tt) & (j <= 128 + tt)).astype(np.float32)
    return c


CONST_SHAPES = {
    "ident32": [128, 128], "ones32": [128, 128], "rotm": [128, 128],
    "rmask": [128, NTMAX], "cm64": [HC, HC], "mband": [128, 256], "mfirst": [128, 256],
    "msamp": [16, 132],
}


def build(cfg):
    nc = bass.Bass("TRN2", target_bir_lowering=False)
    NP, NFF, NA, NB, DFF = cfg.npass, cfg.nff, cfg.na, cfg.nb, cfg.dff
    L = cfg.depth

    def din(name, shape):
        return nc.dram_tensor(name, list(shape), F32, kind="ExternalInput").ap()

    def dout(name, shape):
        return nc.dram_tensor(name, list(shape), F32, kind="ExternalOutput").ap()

    xp = din("xp", [cfg.seq, D])
    xs = din("xs", [NS, D])
    st_in = din("st_in", [NA, NSEQ_S, 16, 128, 128])
    ck_in = din("ck_in", [max(NB, 1), NSEQ_S, WIN, 512])
    cv_in = din("cv_in", [max(NB, 1), NSEQ_S, WIN, 512])
    gains = din("gains", [128, L * 6 * NCH])
    w_fin = din("w_fin", [L, 2, D, 2 * DFF])
    w_fout = din("w_fout", [L, 2, DFF, D])
    w_hin = din("w_hin", [NA, D, 8192])
    lbl = din("lbl", [128, NA * 16])
    hgn = din("hgn", [128, NA * 16])
    w_hout = din("w_hout", [NA, D, D])
    w_ain = din("w_ain", [max(NB, 1), D, 3072])
    sinks_b = din("sinks_b", [max(NB, 1), 32])
    w_aout = din("w_aout", [max(NB, 1), D, D])
    sinks16_d = din("sinks16_d", [16, max(NB, 1) * 8])
    cst = {k: din("c_" + k, v) for k, v in CONST_SHAPES.items()}
    cosT_d = din("c_cosT", [128, cfg.seq + NS])
    sinT_d = din("c_sinT", [128, cfg.seq + NS])

    yp = dout("yp", [cfg.seq, D])
    ys = dout("ys", [NS, D])
    hg_p = dout("hg_p", [NA, 16, 128, 128])
    hg_s = dout("hg_s", [NA, NSEQ_S, 16, 128, 128])
    kw_p = dout("kw_p", [max(NB, 1), WIN, 512])
    vw_p = dout("vw_p", [max(NB, 1), WIN, 512])
    kw_s = dout("kw_s", [max(NB, 1), NSEQ_S, WIN, 512])
    vw_s = dout("vw_s", [max(NB, 1), NSEQ_S, WIN, 512])

    with contextlib.ExitStack() as es:
        P = Prog(nc, es)
        PE, ACT, DVE, POOL, SP = P.pe, P.act, P.dve, P.pool, P.sp

        def sb(name, shape, dt=F32):
            return es.enter_context(nc.sbuf_tensor(name, list(shape), dt))

        xT = sb("xT", [128, NCH, NTMAX])
        xb = [Buf() for _ in range(NCH)]
        yT = sb("yT", [128, NCH, NTMAX])
        yb = [Buf() for _ in range(NCH)]
        hT = yT[:, 0:NCH // 2, :].bitcast(BF16).rearrange("p a (b n) -> p (a b) n", b=2)
        hbufs = yb[0:NCH // 2]
        work = sb("work", [128, WORKW])
        ob = yT[:, NCH // 2:NCH, :].bitcast(BF16).rearrange("p a (b n) -> p (a b) n", b=2)
        obb = [yb[NCH // 2 + c // 2] for c in range(NCH)]
        ym = work[:, 0:NCH * NTMAX].rearrange("p (c n) -> p c n", n=NTMAX)
        ymb = [Buf() for _ in range(NCH)]
        rstd = sb("rstd", [128, NTMAX])
        rstdb = Buf()
        wr = [sb("wr%d" % i, [128, 8192], BF16) for i in range(2)]
        wrb = [Buf() for _ in range(2)]
        wsem = [[es.enter_context(nc.semaphore("s_w%d" % i)), 0] for i in range(2)]
        wring = Ring(list(range(2)))
        sqr = [sb("sq%d" % i, [128, NTMAX]) for i in range(2)]
        sqring = Ring([(sqr[i], Buf()) for i in range(2)])
        tmpr = [sb("tmp%d" % i, [128, 512]) for i in range(3)]
        tmpring = Ring([(tmpr[i], Buf()) for i in range(3)])
        g_sb = sb("g_sb", [128, L * 6 * NCH])
        gh_sb = sb("gh_sb", [128, L * 6 * NCH])
        gb = Buf()
        ident32 = sb("ident32", [128, 128]); ident16 = sb("ident16", [128, 128], BF16)
        ones32 = sb("ones32", [128, 128]); rotm = sb("rotm", [128, 128])
        rmask = sb("rmask", [128, NTMAX]); cm64 = sb("cm64", [HC, HC])
        mband = sb("mband", [128, 256]); mfirst = sb("mfirst", [128, 256]); msamp = sb("msamp", [16, 132])
        cb = Buf()
        lb_sb = sb("lb_sb", [128, NA * 16]); oml_sb = sb("oml_sb", [128, NA * 16]); hgn_sb = sb("hgn_sb", [128, NA * 16])
        sscr = nc.dram_tensor("sscr", [NA, 16, 128, 128], F32).ap()
        if NB:
            kprev = [sb("kprev%d" % b, [128, 4, WIN], BF16) for b in range(NB)]
            vprev = [sb("vprev%d" % b, [128, 512], BF16) for b in range(NB)]
            kvpb = [Buf() for _ in range(NB)]
            sink_sb = sb("sink_sb", [128, NB * 32]); nsink_sb = sb("nsink_sb", [128, NB * 32])
            sinks16 = sb("sinks16", [16, NB * 8]); nsinks16 = sb("nsinks16", [16, NB * 8])
            cosT = sb("cosT", [128, NTMAX]); sinT = sb("sinT", [128, NTMAX])
            ropeb = Buf()
        xstg = sb("xstg", [128, D])
        xstring = Ring([(xstg[:, :], Buf())])

        ps = es.enter_context(nc.psum_tensor("ps", [128, 8, 512], F32))
        psring = Ring([(ps[:, i, :], Buf()) for i in range(6)])
        accring = Ring([(ps[:, i, :], Buf()) for i in range(6, 8)])

        def wcols(off, n):
            return slice(off, off + n)

        def ld(dst, src, bufs):
            P.dma(SP, dst, src, [], bufs)

        ld(g_sb[:], gains, [gb])
        for nm, t_ in (("ident32", ident32), ("ones32", ones32), ("rotm", rotm), ("rmask", rmask),
                       ("cm64", cm64), ("mband", mband), ("mfirst", mfirst), ("msamp", msamp)):
            ld(t_[:], cst[nm], [cb])
        ld(lb_sb[:], lbl, [cb])
        ld(hgn_sb[:], hgn, [cb])
        P.op(DVE, "tensor_copy", [cb], [cb], ident16[:], ident32[:])
        P.op(DVE, "tensor_scalar", [gb], [gb], gh_sb[:], g_sb[:], 0.5, None, ALU.mult)
        P.op(DVE, "memset", [], [cb], oml_sb[:], 1.0)
        if NA > 1:
            assert NA == 2
            P.op(DVE, "tensor_tensor", [cb], [cb], oml_sb[:, 16:32], lb_sb[:, 16:32], lb_sb[:, 0:16], ALU.subtract)
            P.op(ACT, "activation", [cb], [cb], lb_sb[:, 16:32], oml_sb[:, 16:32], AF.Sigmoid)
            P.op(DVE, "tensor_scalar", [cb], [cb], oml_sb[:, 16:32], lb_sb[:, 16:32], -1.0, 1.0, ALU.mult, ALU.add)
        P.op(DVE, "memset", [cb], [cb], lb_sb[:, 0:16], 0.0)
        for b in range(NB):
            P.op(DVE, "memset", [], [kvpb[b]], kprev[b][:], 0.0)
            P.op(DVE, "memset", [], [kvpb[b]], vprev[b][:], 0.0)
            ld(sink_sb[:, b * 32:(b + 1) * 32], sinks_b[b:b + 1, :].partition_broadcast(128), [cb])
        if NB:
            ld(sinks16[:], sinks16_d, [cb])
            P.op(DVE, "tensor_scalar", [cb], [cb], nsink_sb[:], sink_sb[:], -1.0, None, ALU.mult)
            P.op(DVE, "tensor_scalar", [cb], [cb], nsinks16[:], sinks16[:], -1.0, None, ALU.mult)

        def gcol(tbl, l, n, c):
            i = (l * 6 + n) * NCH + c
            return tbl[:, i:i + 1]

        def wload(parts):
            s = wring.next()
            for (off, n, kc, src) in parts:
                dst = wr[s][:, off:off + kc * n].rearrange("p (k n) -> p k n", n=n)
                P.dma(POOL, dst, src, [], [wrb[s]], semslot=wsem[s])
            return s

        def slabs_of(p):
            return [(0, TP)] + ([(TP, NS)] if p == 0 else [])

        def compute_rstd(src, sbufs, slabs, nt):
            for (c0, n) in slabs:
                bank, bb = psring.next()
                for c in range(NCH):
                    sq, sqb = sqring.next()
                    P.op(ACT, "activation", [sbufs[c]], [sqb], sq[:, 0:n], src[:, c, c0:c0 + n], AF.Square)
                    P.mm([(bank[:, 0:n], ones32[:], sq[:, 0:n], c == 0, c == NCH - 1)], [sqb, cb], [bb])
                t_, tb = tmpring.next()
                P.op(ACT, "activation", [bb], [tb], t_[:, 0:n], bank[:, 0:n], AF.Sqrt, bias=EPS, scale=1.0 / D)
                P.op(DVE, "reciprocal", [tb], [rstdb], rstd[:, c0:c0 + n], t_[:, 0:n])

        def pre_norm(l, n_, slabs, nt):
            compute_rstd(xT, xb, slabs, nt)
            for c in range(NCH):
                P.op(DVE, "scalar_tensor_tensor", [xb[c], rstdb, gb], [hbufs[c // 2]],
                     hT[:, c, 0:nt], xT[:, c, 0:nt], gcol(g_sb, l, n_, c), rstd[:, 0:nt], ALU.mult, ALU.mult)

        def post_norm(l, n_, half, slabs, nt, src=None, srcb=None):
            if src is None:
                src, srcb = yT, yb
            compute_rstd(src, srcb, slabs, nt)
            tbl = gh_sb if half else g_sb
            for c in range(NCH):
                P.op(DVE, "scalar_tensor_tensor", [rstdb, gb], [srcb[c]],
                     src[:, c, 0:nt], src[:, c, 0:nt], gcol(tbl, l, n_, c), rstd[:, 0:nt], ALU.mult, ALU.mult)
                P.op(DVE, "tensor_tensor", [srcb[c]], [xb[c]], xT[:, c, 0:nt], xT[:, c, 0:nt], src[:, c, 0:nt], ALU.add)

        actT = work[:, 0:NFF * NTMAX // 2].bitcast(BF16).rearrange("p (j n) -> p j n", n=NTMAX)
        ab = [Buf() for _ in range(NFF)]

        def ffn(l, i, slabs, nt):
            pre_norm(l, 0 if i == 0 else 4, slabs, nt)
            wv = w_fin[l, i].rearrange("(c p) n -> p c n", p=128)
            for fb in range(NFF // 2):
                s = wload([(0, 256, 16, wv[:, :, fb * 256:(fb + 1) * 256]),
                           (4096, 256, 16, wv[:, :, DFF + fb * 256:DFF + (fb + 1) * 256])])
                ws = wr[s][:, :].rearrange("p (g c n) -> p g c n", g=2, c=16)
                for j in range(2):
                    J = 2 * fb + j
                    for (c0, n) in slabs:
                        bg, bgb = psring.next()
                        P.mm([(bg[:, 0:n], ws[:, 0, c, j * 128:(j + 1) * 128], hT[:, c, c0:c0 + n]) for c in range(NCH)],
                             [wrb[s]] + hbufs, [bgb])
                        bu, bub = psring.next()
                        P.mm([(bu[:, 0:n], ws[:, 1, c, j * 128:(j + 1) * 128], hT[:, c, c0:c0 + n]) for c in range(NCH)],
                             [wrb[s]] + hbufs, [bub])
                        t_, tb = tmpring.next()
                        P.op(ACT, "activation", [bgb], [tb], t_[:, 0:n], bg[:, 0:n], AF.Silu)
                        P.op(DVE, "tensor_tensor", [tb, bub], [ab[J]], actT[:, J, c0:c0 + n], t_[:, 0:n], bu[:, 0:n], ALU.mult)
            wo = w_fout[l, i].rearrange("(j p) n -> p j n", p=128)
            for dc in range(NCH):
                s = wload([(0, 128, NFF, wo[:, :, dc * 128:(dc + 1) * 128])])
                ws = wr[s][:, 0:NFF * 128].rearrange("p (j n) -> p j n", n=128)
                for (c0, n) in slabs:
                    bk, bkb = psring.next()
                    P.mm([(bk[:, 0:n], ws[:, j, :], actT[:, j, c0:c0 + n]) for j in range(NFF)], [wrb[s]] + ab, [bkb])
                    P.op(ACT, "activation", [bkb], [yb[dc]], yT[:, dc, c0:c0 + n], bk[:, 0:n], AF.Copy)
            post_norm(l, 1 if i == 0 else 5, True, slabs, nt)

        def out_proj(wmat, slabs, nt):
            P.barrier()
            wv = wmat.rearrange("(c p) n -> p c n", p=128)
            for dcb in range(4):
                s = wload([(0, 512, 16, wv[:, :, dcb * 512:(dcb + 1) * 512])])
                ws = wr[s][:, :].rearrange("p (c n) -> p c n", n=512)
                for k in range(4):
                    dc = dcb * 4 + k
                    for (c0, n) in slabs:
                        bk, bkb = psring.next()
                        P.mm([(bk[:, 0:n], ws[:, c, k * 128:(k + 1) * 128], ob[:, c, c0:c0 + n]) for c in range(NCH)],
                             [wrb[s]] + yb[NCH // 2:], [bkb])
                        P.op(ACT, "activation", [bkb], [ymb[dc]], ym[:, dc, c0:c0 + n], bk[:, 0:n], AF.Copy)

        def carver():
            off = [0]

            def carve(nwords, dt=F32):
                v = work[:, off[0]:off[0] + nwords]
                off[0] += nwords
                assert off[0] <= WORKW, off[0]
                if dt == BF16:
                    v = v.bitcast(BF16)
                return v
            return carve

        def hgrn(l, p, slabs, nt):
            a = l // 2
            last = (p == NP - 1)
            has_s = (p == 0)
            pre_norm(l, 2, slabs, nt)
            carve = carver()
            sets = []
            for _ in range(2):
                d_ = {}
                for nm in ("qf", "sg", "kk", "bb", "dd", "dl", "sgl"):
                    d_[nm] = (carve(NTMAX), Buf())
                for nm in ("vbf", "qt", "kt", "qh", "kh"):
                    d_[nm] = (carve(NTMAX // 2, BF16), Buf())
                d_["At"] = (carve(256, BF16), Buf())
                d_["Ats"] = (carve(8, BF16), Buf())
                d_["khT"] = (carve(512, BF16), Buf())
                d_["vT"] = (carve(512, BF16), Buf())
                d_["khTs"] = (carve(256, BF16), Buf())
                d_["vTs"] = (carve(256, BF16), Buf())
                d_["Sbf"] = (carve(512, BF16), Buf())
                d_["Ss"] = (carve(512), Buf())
                d_["Sbs"] = (carve(256, BF16), Buf())
                d_["Sw"] = (carve(128), Buf())
                sets.append(d_)
            wv = w_hin[a].rearrange("(c p) n -> p c n", p=128)
            npc = TP // HC

            def stage_P(h):
                S_ = sets[h % 2]
                s = wload([(k * 2048, 128, 16, wv[:, :, k * 2048 + h * 128:k * 2048 + (h + 1) * 128]) for k in range(4)])
                ws = wr[s][:, :].rearrange("p (k c n) -> p k c n", k=4, c=16)
                for (c0, n) in slabs:
                    banks = []
                    for k in range(4):
                        bk, bkb = psring.next()
                        P.mm([(bk[:, 0:n], ws[:, k, c, :], hT[:, c, c0:c0 + n]) for c in range(NCH)], [wrb[s]] + hbufs, [bkb])
                        banks.append((bk, bkb))
                    sl = slice(c0, c0 + n)
                    P.op(ACT, "activation", [banks[0][1]], [S_["qf"][1]], S_["qf"][0][:, sl], banks[0][0][:, 0:n], AF.Copy)
                    P.op(ACT, "activation", [banks[1][1]], [S_["sg"][1]], S_["sg"][0][:, sl], banks[1][0][:, 0:n], AF.Sigmoid)
                    P.op(DVE, "tensor_copy", [banks[2][1]], [S_["vbf"][1]], S_["vbf"][0][:, sl], banks[2][0][:, 0:n])
                    P.op(ACT, "activation", [banks[3][1]], [S_["sgl"][1]], S_["sgl"][0][:, sl], banks[3][0][:, 0:n], AF.Silu)
                if p == 0:
                    P.op(DVE, "memset", [], [S_["Sw"][1]], S_["Sw"][0], 0.0)
                else:
                    P.dma(SP, S_["Sw"][0], sscr[a, h], [], [S_["Sw"][1]])

            def cview(t_, n_chunks, csz, base=0):
                return t_[:, base:base + n_chunks * csz].rearrange("p (c t) -> p c t", t=csz)

            def stage_E(h):
                S_ = sets[h % 2]
                g = lambda nm: S_[nm][0]
                B = lambda nm: S_[nm][1]
                hi = a * 16 + h
                N_ = slice(0, nt)
                P.op(DVE, "tensor_scalar", [B("sg"), cb], [B("sg")], g("sg")[:, N_], g("sg")[:, N_],
                     oml_sb[:, hi:hi + 1], lb_sb[:, hi:hi + 1], ALU.mult, ALU.add)
                P.op(DVE, "tensor_scalar", [B("sg")], [B("kk")], g("kk")[:, N_], g("sg")[:, N_], -1.0, 1.0, ALU.mult, ALU.add)
                P.op(ACT, "activation", [B("sg")], [B("sg")], g("sg")[:, N_], g("sg")[:, N_], AF.Ln)
                P.op(DVE, "tensor_tensor_scan", [B("sg"), cb], [B("bb")], g("bb")[:, N_], rmask[:, N_], g("sg")[:, N_], 0.0, ALU.mult, ALU.add)
                parts = [(npc, HC, 0)] + ([(NSEQ_S, LS, TP)] if has_s else [])
                for (ncx, csz, base) in parts:
                    bv = cview(g("bb"), ncx, csz, base)
                    mid = csz // 2 - 1
                    P.op(DVE, "tensor_tensor", [B("bb")], [B("dd")], cview(g("dd"), ncx, csz, base), bv,
                         bv[:, :, mid:mid + 1].to_broadcast([128, ncx, csz]), ALU.subtract)
                    P.op(DVE, "tensor_tensor", [B("bb")], [B("dl")], cview(g("dl"), ncx, csz, base),
                         bv[:, :, csz - 1:csz].to_broadcast([128, ncx, csz]), bv, ALU.subtract)
                P.op(ACT, "activation", [B("dd")], [B("dd")], g("dd")[:, N_], g("dd")[:, N_], AF.Exp)
                P.op(DVE, "tensor_tensor", [B("qf"), B("dd")], [B("qt")], g("qt")[:, N_], g("qf")[:, N_], g("dd")[:, N_], ALU.mult)
                P.op(DVE, "reciprocal", [B("dd")], [B("dd")], g("dd")[:, N_], g("dd")[:, N_])
                P.op(DVE, "tensor_tensor", [B("kk"), B("dd")], [B("kt")], g("kt")[:, N_], g("kk")[:, N_], g("dd")[:, N_], ALU.mult)
                P.op(ACT, "activation", [B("bb")], [B("bb")], g("bb")[:, N_], g("bb")[:, N_], AF.Exp)
                P.op(DVE, "tensor_tensor", [B("qf"), B("bb")], [B("qh")], g("qh")[:, N_], g("qf")[:, N_], g("bb")[:, N_], ALU.mult)
                P.op(ACT, "activation", [B("dl")], [B("dl")], g("dl")[:, N_], g("dl")[:, N_], AF.Exp)
                P.op(DVE, "tensor_tensor", [B("kk"), B("dl")], [B("kh")], g("kh")[:, N_], g("kk")[:, N_], g("dl")[:, N_], ALU.mult)

            def stage_T(h):
                S_ = sets[h % 2]
                g = lambda nm: S_[nm][0]
                B = lambda nm: S_[nm][1]
                bk, bkb = psring.next()
                P.mm([(bk[0:HC, ci * HC:(ci + 1) * HC], g("kt")[:, ci * HC:(ci + 1) * HC], g("qt")[:, ci * HC:(ci + 1) * HC], True, True)
                      for ci in range(npc)], [B("kt"), B("qt")], [bkb])
                At = g("At")[0:HC, :].rearrange("p (c t) -> p c t", t=HC)
                P.op(DVE, "tensor_tensor", [bkb, cb], [B("At")], At, bk[0:HC, :].rearrange("p (c t) -> p c t", t=HC),
                     cm64[:, :].unsqueeze(1).to_broadcast([HC, npc, HC]), ALU.mult)
                bk2, bk2b = psring.next()
                k16 = bk2.bitcast(BF16)
                P.mm([(k16[0:HC, ci * 128:(ci + 1) * 128], g("kh")[:, ci * HC:(ci + 1) * HC], ident16[:]) for ci in range(npc)],
                     [B("kh"), cb], [bk2b], transpose=True)
                P.op(ACT, "activation", [bk2b], [B("khT")], g("khT")[0:HC, :], k16[0:HC, :], AF.Copy)
                bk3, bk3b = psring.next()
                v16 = bk3.bitcast(BF16)
                P.mm([(v16[0:HC, ci * 128:(ci + 1) * 128], g("vbf")[:, ci * HC:(ci + 1) * HC], ident16[:]) for ci in range(npc)],
                     [B("vbf"), cb], [bk3b], transpose=True)
                P.op(DVE, "tensor_copy", [bk3b], [B("vT")], g("vT")[0:HC, :], v16[0:HC, :])
                if has_s:
                    bk4, bk4b = psring.next()
                    P.mm([(bk4[0:LS, i * LS:(i + 1) * LS], g("kt")[:, TP + i * LS:TP + (i + 1) * LS], g("qt")[:, TP + i * LS:TP + (i + 1) * LS], True, True)
                          for i in range(NSEQ_S)], [B("kt"), B("qt")], [bk4b])
                    P.op(DVE, "tensor_tensor", [bk4b, cb], [B("Ats")], g("Ats")[0:LS, :].rearrange("p (c t) -> p c t", t=LS),
                         bk4[0:LS, 0:NS].rearrange("p (c t) -> p c t", t=LS),
                         cm64[0:LS, 0:LS].unsqueeze(1).to_broadcast([LS, NSEQ_S, LS]), ALU.mult)
                    bk5, bk5b = psring.next()
                    s16 = bk5.bitcast(BF16)
                    P.mm([(s16[0:LS, i * 128:(i + 1) * 128], g("kh")[:, TP + i * LS:TP + (i + 1) * LS], ident16[:]) for i in range(NSEQ_S)]
                         + [(s16[0:LS, 512 + i * 128:512 + (i + 1) * 128], g("vbf")[:, TP + i * LS:TP + (i + 1) * LS], ident16[:]) for i in range(NSEQ_S)],
                         [B("kh"), B("vbf"), cb], [bk5b], transpose=True)
                    P.op(ACT, "activation", [bk5b], [B("khTs")], g("khTs")[0:LS, :], s16[0:LS, 0:512], AF.Copy)
                    P.op(ACT, "activation", [bk5b], [B("vTs")], g("vTs")[0:LS, :], s16[0:LS, 512:1024], AF.Copy)

            def stage_U(h):
                S_ = sets[h % 2]
                g = lambda nm: S_[nm][0]
                B = lambda nm: S_[nm][1]
                khT = g("khT")[0:HC, :].rearrange("p (c k) -> p c k", k=128)
                vT = g("vT")[0:HC, :].rearrange("p (c k) -> p c k", k=128)
                Sbf = g("Sbf").rearrange("p (c k) -> p c k", k=128)
                S = g("Sw")
                for half in range(npc // 4):
                    bk, bkb = psring.next()
                    P.mm([(bk[:, k * 128:(k + 1) * 128], khT[:, half * 4 + k, :], vT[:, half * 4 + k, :], True, True) for k in range(4)],
                         [B("khT"), B("vT")], [bkb])
                    for k in range(4):
                        ci = half * 4 + k
                        P.op(DVE, "tensor_copy", [B("Sw")], [B("Sbf")], Sbf[:, ci, :], S)
                        P.op(DVE, "scalar_tensor_tensor", [B("bb"), bkb], [B("Sw")], S, S,
                             g("bb")[:, ci * HC + HC - 1:ci * HC + HC], bk[:, k * 128:(k + 1) * 128], ALU.mult, ALU.add)
                if last:
                    P.dma(SP, hg_p[a, h], S, [B("Sw")], [])
                else:
                    P.dma(SP, sscr[a, h], S, [B("Sw")], [])
                if has_s:
                    Ss = g("Ss").rearrange("p (i k) -> p i k", k=128)
                    P.dma(SP, Ss, st_in[a, :, h].rearrange("i k v -> k i v"), [], [B("Ss")])
                    P.op(DVE, "tensor_copy", [B("Ss")], [B("Sbs")], g("Sbs"), g("Ss"))
                    bk, bkb = psring.next()
                    khTs = g("khTs")[0:LS, :].rearrange("p (i k) -> p i k", k=128)
                    vTs = g("vTs")[0:LS, :].rearrange("p (i k) -> p i k", k=128)
                    P.mm([(bk[:, i * 128:(i + 1) * 128], khTs[:, i, :], vTs[:, i, :], True, True) for i in range(NSEQ_S)],
                         [B("khTs"), B("vTs")], [bkb])
                    dec = g("bb")[:, TP:TP + NS].rearrange("p (i t) -> p i t", t=LS)[:, :, LS - 1:LS]
                    P.op(DVE, "tensor_tensor", [B("bb")], [B("Ss")], Ss, Ss, dec.to_broadcast([128, NSEQ_S, 128]), ALU.mult)
                    P.op(DVE, "tensor_tensor", [bkb], [B("Ss")], Ss, Ss, bk[:, :].rearrange("p (i k) -> p i k", k=128), ALU.add)
                    P.dma(SP, hg_s[a, :, h].rearrange("i k v -> k i v"), Ss, [B("Ss")], [])

            def stage_O(h):
                S_ = sets[h % 2]
                g = lambda nm: S_[nm][0]
                B = lambda nm: S_[nm][1]
                At = g("At")[0:HC, :].rearrange("p (c t) -> p c t", t=HC)
                vT = g("vT")[0:HC, :].rearrange("p (c k) -> p c k", k=128)
                Sbf = g("Sbf").rearrange("p (c k) -> p c k", k=128)
                obank = []
                bk, bkb = psring.next()
                mms = []
                for ci in range(npc):
                    o_ = bk[:, ci * HC:(ci + 1) * HC]
                    mms.append((o_, vT[:, ci, :], At[:, ci, :], True, False))
                    mms.append((o_, Sbf[:, ci, :], g("qh")[:, ci * HC:(ci + 1) * HC], False, True))
                P.mm(mms, [B("vT"), B("At"), B("Sbf"), B("qh")], [bkb])
                obank.append((bk, bkb, 0, TP))
                if has_s:
                    bk2, bk2b = psring.next()
                    Ats = g("Ats")[0:LS, :].rearrange("p (c t) -> p c t", t=LS)
                    vTs = g("vTs")[0:LS, :].rearrange("p (i k) -> p i k", k=128)
                    Sbs = g("Sbs").rearrange("p (i k) -> p i k", k=128)
                    mms = []
                    for i in range(NSEQ_S):
                        o_ = bk2[:, i * LS:(i + 1) * LS]
                        mms.append((o_, vTs[:, i, :], Ats[:, i, :], True, False))
                        mms.append((o_, Sbs[:, i, :], g("qh")[:, TP + i * LS:TP + (i + 1) * LS], False, True))
                    P.mm(mms, [B("vTs"), B("Ats"), B("Sbs"), B("qh")], [bk2b])
                    obank.append((bk2, bk2b, TP, NS))
                hi = a * 16 + h
                for (bk_, bkb_, c0, n) in obank:
                    sl = slice(c0, c0 + n)
                    P.op(ACT, "activation", [bkb_], [B("dd")], g("dd")[:, sl], bk_[:, 0:n], AF.Square)
                    bn, bnb = psring.next()
                    P.mm([(bn[:, 0:n], ones32[:], g("dd")[:, sl], True, True)], [B("dd"), cb], [bnb])
                    P.op(ACT, "activation", [bnb], [B("dl")], g("dl")[:, sl], bn[:, 0:n], AF.Sqrt, bias=EPS, scale=1.0 / 128)
                    P.op(DVE, "reciprocal", [B("dl")], [B("dl")], g("dl")[:, sl], g("dl")[:, sl])
                    P.op(DVE, "scalar_tensor_tensor", [bkb_, B("dl"), cb], [B("dd")], g("dd")[:, sl], bk_[:, 0:n],
                         hgn_sb[:, hi:hi + 1], g("dl")[:, sl], ALU.mult, ALU.mult)
                    P.op(DVE, "tensor_tensor", [B("dd"), B("sgl")], [obb[h]], ob[:, h, sl], g("dd")[:, sl],
                         g("sgl")[:, sl], ALU.mult)

            stage_P(0)
            stage_E(0)
            for h in range(16):
                if h + 1 < 16:
                    stage_P(h + 1)
                stage_T(h)
                stage_U(h)
                stage_O(h)
                if h + 1 < 16:
                    stage_E(h + 1)
            out_proj(w_hout[a], slabs, nt)
            post_norm(l, 3, False, slabs, nt, ym, ymb)

        def swa(l, p, slabs, nt):
            b = l // 2
            last = (p == NP - 1)
            has_s = (p == 0)
            pre_norm(l, 2, slabs, nt)
            carve = carver()
            k32 = carve(4 * (WIN + NS)).rearrange("p (j n) -> p j n", n=WIN + NS); k32b = Buf()
            vl32 = carve(512); vl32b = Buf()
            vsst_r = Ring([(carve(128), Buf()) for _ in range(2)])
            krp = carve(4 * (WIN + TP) // 2, BF16).rearrange("p (j n) -> p j n", n=WIN + TP); krpb = [Buf() for _ in range(4)]
            vtm = carve(5 * 512 // 2, BF16).rearrange("p (t n) -> p t n", n=512); vtmb = [Buf() for _ in range(5)]
            qsets = []
            for _ in range(2):
                qsets.append((carve(4 * NTMAX // 2, BF16).rearrange("p (c n) -> p c n", n=NTMAX), [Buf() for _ in range(4)]))
            vbf = carve(NTMAX // 2, BF16); vbfb = Buf()
            qf_r = Ring([(carve(NTMAX), Buf()) for _ in range(2)])
            t2_r = Ring([(carve(NTMAX), Buf()) for _ in range(2)])
            r32_r = Ring([(carve(NTMAX), Buf()) for _ in range(2)])
            pe_r = Ring([(carve(256), Buf()) for _ in range(3)])
            pm_r = Ring([(carve(256), Buf()) for _ in range(3)])
            pn_r = Ring([(carve(256), Buf()) for _ in range(3)])
            pT_r = Ring([(carve(128, BF16), Buf()) for _ in range(3)])
            sm_r = Ring([(carve(8), Buf()) for _ in range(4)])
            stg = carve(512); stgb = Buf()
            if has_s:
                qs = carve(4 * 64 // 2, BF16).rearrange("p (j i c t) -> p j i c t", j=4, i=4, c=4); qsb = Buf()
                kcs = carve(16 * 132 // 2, BF16).rearrange("p (j i n) -> p j i n", j=4, i=4); kcsb = Buf()
                vcs = carve(4 * 512 // 2, BF16).rearrange("p (i n) -> p i n", n=512); vcsb = Buf()
                vns = carve(4 * 512 // 2, BF16).rearrange("p (i n) -> p i n", n=512); vnsb = Buf()
                kc32 = carve(512); kc32b = Buf()

            P.op(DVE, "tensor_copy", [kvpb[b]], krpb, krp[:, :, 0:WIN], kprev[b][:, :, :])
            P.op(DVE, "tensor_copy", [kvpb[b]], [vtmb[0]], vtm[:, 0, :], vprev[b][:, :])
            P.dma(SP, cosT[:, 0:TP], cosT_d[:, p * TP:(p + 1) * TP], [], [ropeb])
            P.dma(SP, sinT[:, 0:TP], sinT_d[:, p * TP:(p + 1) * TP], [], [ropeb])
            if has_s:
                P.dma(SP, cosT[:, TP:TP + NS], cosT_d[:, cfg.seq:cfg.seq + NS], [], [ropeb])
                P.dma(SP, sinT[:, TP:TP + NS], sinT_d[:, cfg.seq:cfg.seq + NS], [], [ropeb])

            wv = w_ain[b].rearrange("(c p) n -> p c n", p=128)

            def rope(bank, bankb, n, c0, outs):
                qf, qfb = qf_r.next()
                P.op(ACT, "activation", [bankb], [qfb], qf[:, 0:n], bank[:, 0:n], AF.Copy)
                if "sR" in DBG_SKIP:
                    for (en, out_ap, bufs, cs, v3) in outs:
                        src = qf[:, cs]
                        if v3:
                            src = src.rearrange("p (i t) -> p i t", t=LS)
                        P.op(DVE, "tensor_copy", [qfb], bufs, out_ap, src)
                    return
                br, brb = psring.next()
                P.mm([(br[:, 0:n], rotm[:], qf[:, 0:n], True, True)], [qfb, cb], [brb])
                t2, t2b = t2_r.next()
                P.op(DVE, "tensor_tensor", [brb, ropeb], [t2b], t2[:, 0:n], br[:, 0:n], sinT[:, c0:c0 + n], ALU.mult)
                P.op(DVE, "tensor_tensor", [qfb, ropeb], [qfb], qf[:, 0:n], qf[:, 0:n], cosT[:, c0:c0 + n], ALU.mult)
                r32, r32b = r32_r.next()
                P.op(DVE, "tensor_tensor", [qfb, t2b], [r32b], r32[:, 0:n], qf[:, 0:n], t2[:, 0:n], ALU.add)
                for (en, out_ap, bufs, cs, v3) in outs:
                    src = r32[:, cs]
                    if v3:
                        src = src.rearrange("p (i t) -> p i t", t=LS)
                    if en == "act":
                        P.op(ACT, "activation", [r32b], bufs, out_ap, src, AF.Copy)
                    else:
                        P.op(DVE, "tensor_copy", [r32b], bufs, out_ap, src)

            def stage_P(jp):
                qr, qrb = qsets[jp % 2]
                s1 = wload([(0, 512, 16, wv[:, :, jp * 512:(jp + 1) * 512])])
                ws1 = wr[s1][:, :].rearrange("p (c n) -> p c n", n=512)
                for (c0, n) in slabs:
                    for c in range(4):
                        bk, bkb = psring.next()
                        P.mm([(bk[:, 0:n], ws1[:, k, c * 128:(c + 1) * 128], hT[:, k, c0:c0 + n]) for k in range(NCH)],
                             [wrb[s1]] + hbufs, [bkb])
                        rope(bk, bkb, n, c0, [("act", qr[:, c, c0:c0 + n], [qrb[c]], slice(0, n), False)])
                s2 = wload([(0, 128, 16, wv[:, :, 2048 + jp * 128:2048 + (jp + 1) * 128]),
                            (2048, 128, 16, wv[:, :, 2560 + jp * 128:2560 + (jp + 1) * 128])])
                ws2 = wr[s2][:, 0:4096].rearrange("p (g c n) -> p g c n", g=2, c=16)
                for (c0, n) in slabs:
                    bk, bkb = psring.next()
                    P.mm([(bk[:, 0:n], ws2[:, 0, k, :], hT[:, k, c0:c0 + n]) for k in range(NCH)], [wrb[s2]] + hbufs, [bkb])
                    if "sK" in DBG_SKIP:
                        pass
                    elif c0 == 0:
                        rope(bk, bkb, n, c0, [("act", krp[:, jp, WIN:WIN + TP], [krpb[jp]], slice(0, n), False),
                                              ("dve", k32[:, jp, 0:WIN], [k32b], slice(TP - WIN, TP), False)])
                    else:
                        rope(bk, bkb, n, c0, [("act", kcs[:, jp, :, 128:132], [kcsb], slice(0, n), True),
                                              ("dve", k32[:, jp, WIN:WIN + NS], [k32b], slice(0, n), False)])
                    if c0 == 0:
                        for tt in range(4):
                            bt, btb = psring.next()
                            P.mm([(bt[:, 0:128], hT[:, k, tt * 128:(tt + 1) * 128], ws2[:, 1, k, :]) for k in range(NCH)],
                                 [wrb[s2]] + hbufs, [btb])
                            if tt == 3 and last:
                                P.op(ACT, "activation", [btb], [vl32b], vl32[:, jp * 128:(jp + 1) * 128], bt[:, 0:128], AF.Copy)
                                P.op(DVE, "tensor_copy", [vl32b], [vtmb[1 + tt]], vtm[:, 1 + tt, jp * 128:(jp + 1) * 128],
                                     vl32[:, jp * 128:(jp + 1) * 128])
                            else:
                                P.op(ACT, "activation", [btb], [vtmb[1 + tt]], vtm[:, 1 + tt, jp * 128:(jp + 1) * 128], bt[:, 0:128], AF.Copy)
                    else:
                        for i in range(NSEQ_S):
                            bt, btb = psring.next()
                            P.mm([(bt[0:LS, 0:128], hT[:, k, TP + i * LS:TP + (i + 1) * LS], ws2[:, 1, k, :]) for k in range(NCH)],
                                 [wrb[s2]] + hbufs, [btb])
                            vs_, vsb_ = vsst_r.next()
                            P.op(ACT, "activation", [btb], [vsb_], vs_[0:LS, :], bt[0:LS, 0:128], AF.Copy)
                            P.op(DVE, "tensor_copy", [vsb_], [vnsb], vns[0:LS, i, jp * 128:(jp + 1) * 128], vs_[0:LS, :])
                            P.dma(SP, vw_s[b, i, WIN - LS:WIN, jp * 128:(jp + 1) * 128], vs_[0:LS, :], [vsb_], [])

            def softmax_pv(S_ap, sbuf_b, rows, ncols, nsink_col, sink_col, mask_ap):
                sm, smb = sm_r.next()
                P.op(DVE, "reduce_max", [sbuf_b], [smb], sm[0:rows, 0:1], S_ap, AX.X)
                P.op(DVE, "tensor_scalar", [smb, cb], [smb], sm[0:rows, 1:2], sm[0:rows, 0:1], -0.125, nsink_col, ALU.mult, ALU.min)
                pe_, peb = pe_r.next()
                P.op(ACT, "activation", [sbuf_b, smb], [peb], pe_[0:rows, 0:ncols], S_ap, AF.Exp, bias=sm[0:rows, 1:2], scale=0.125)
                P.op(ACT, "activation", [smb, cb], [smb], sm[0:rows, 2:3], sink_col, AF.Exp, bias=sm[0:rows, 1:2], scale=1.0)
                pm, pmb = pm_r.next()
                P.op(DVE, "tensor_tensor", [peb, cb], [pmb], pm[0:rows, 0:ncols], pe_[0:rows, 0:ncols], mask_ap, ALU.mult)
                P.op(DVE, "reduce_sum", [pmb], [smb], sm[0:rows, 3:4], pm[0:rows, 0:ncols], AX.X)
                P.op(DVE, "tensor_tensor", [smb], [smb], sm[0:rows, 4:5], sm[0:rows, 3:4], sm[0:rows, 2:3], ALU.add)
                P.op(DVE, "reciprocal", [smb], [smb], sm[0:rows, 5:6], sm[0:rows, 4:5])
                pn, pnb = pn_r.next()
                P.op(ACT, "activation", [pmb, smb], [pnb], pn[0:rows, 0:ncols], pm[0:rows, 0:ncols], AF.Copy, scale=sm[0:rows, 5:6])
                return pn, pnb

            def stage_A(jp):
                qr, qrb = qsets[jp % 2]
                for c in range(4):
                    bo, bob = accring.next()
                    for offi in range(2):
                        kv = 2 * jp + offi
                        hq = 4 * kv + c
                        psl = slice(offi * 64, offi * 64 + 64)
                        for qb in range(TP // 128):
                            bs, bsb = psring.next()
                            P.mm([(bs[:, 0:256], qr[psl, c, qb * 128:(qb + 1) * 128], krp[psl, jp, qb * 128:qb * 128 + 256], True, True)],
                                 [qrb[c], krpb[jp]], [bsb])
                            mask = mfirst if (p == 0 and qb == 0 and "nomf" not in DBG_SKIP) else mband
                            pn, pnb = softmax_pv(bs[:, 0:256], bsb, 128, 256, nsink_sb[:, b * 32 + hq:b * 32 + hq + 1],
                                                 sink_sb[:, b * 32 + hq:b * 32 + hq + 1], mask[:, :])
                            bt, btb = psring.next()
                            P.mm([(bt[:, kb * 128:(kb + 1) * 128], pn[:, kb * 128:(kb + 1) * 128], ident32[:]) for kb in range(2)],
                                 [pnb, cb], [btb], transpose=True)
                            pT, pTb = pT_r.next()
                            P.op(DVE, "tensor_copy", [btb], [pTb], pT[:, 0:256], bt[:, 0:256])
                            P.mm([(bo[psl, qb * 128:(qb + 1) * 128], vtm[:, qb + kb, jp * 128 + offi * 64:jp * 128 + offi * 64 + 64],
                                   pT[:, kb * 128:(kb + 1) * 128]) for kb in range(2)],
                                 [vtmb[qb], vtmb[qb + 1], pTb], [bob])
                    P.op(ACT, "activation", [bob], [obb[jp * 4 + c]], ob[:, jp * 4 + c, 0:TP], bo[:, 0:TP], AF.Copy)

            def sample_attn():
                for i in range(NSEQ_S):
                    P.dma(SP, kc32[:, :], ck_in[b, i], [], [kc32b])
                    bt, btb = psring.next()
                    P.mm([(bt[:, j * 128:(j + 1) * 128], kc32[:, j * 128:(j + 1) * 128], ident32[:]) for j in range(4)],
                         [kc32b, cb], [btb], transpose=True)
                    P.op(DVE, "tensor_copy", [btb], [kcsb], kcs[:, :, i, 0:128], bt[:, :].rearrange("p (j n) -> p j n", n=128))
                    P.dma(POOL, vcs[:, i, :], cv_in[b, i], [], [vcsb])
                for jp in range(4):
                    for offi in range(2):
                        kv = 2 * jp + offi
                        psl = slice(offi * 64, offi * 64 + 64)
                        for i in range(NSEQ_S):
                            bs, bsb = psring.next()
                            P.mm([(bs[0:16, 0:132], qs[psl, jp, i, :, :], kcs[psl, jp, i, :], True, True)], [qsb, kcsb], [bsb])
                            pn, pnb = softmax_pv(bs[0:16, 0:132], bsb, 16, 132, nsinks16[:, b * 8 + kv:b * 8 + kv + 1],
                                                 sinks16[:, b * 8 + kv:b * 8 + kv + 1], msamp[:, :])
                            bt, btb = psring.next()
                            P.mm([(bt[:, 0:16], pn[0:16, 0:128], ident32[0:16, 0:16]),
                                  (bt[0:LS, 16:32], pn[0:16, 128:132], ident32[0:16, 0:16])], [pnb, cb], [btb], transpose=True)
                            pT, pTb = pT_r.next()
                            P.op(DVE, "tensor_copy", [btb], [pTb], pT[:, 0:16], bt[:, 0:16])
                            P.op(DVE, "tensor_copy", [btb], [pTb], pT[0:LS, 16:32], bt[0:LS, 16:32])
                            bo, bob = psring.next()
                            P.mm([(bo[psl, 0:16], vcs[:, i, kv * 64:(kv + 1) * 64], pT[:, 0:16]),
                                  (bo[psl, 0:16], vns[0:LS, i, kv * 64:(kv + 1) * 64], pT[0:LS, 16:32])],
                                 [vcsb, vnsb, pTb], [bob])
                            P.op(ACT, "activation", [bob], obb[jp * 4:jp * 4 + 4],
                                 ob[psl, jp * 4:jp * 4 + 4, TP + i * LS:TP + (i + 1) * LS],
                                 bo[psl, 0:16].rearrange("p (c t) -> p c t", t=LS), AF.Copy)
                for i in range(NSEQ_S):
                    P.dma(SP, kw_s[b, i, 0:WIN - LS], ck_in[b, i, LS:WIN], [], [])
                    P.dma(SP, vw_s[b, i, 0:WIN - LS], cv_in[b, i, LS:WIN], [], [])
                    for (src32, srcb, dst) in ((k32, k32b, kw_s),):
                        bt, btb = psring.next()
                        P.mm([(bt[0:LS, j * 128:(j + 1) * 128], src32[:, j, WIN + i * LS:WIN + (i + 1) * LS], ident32[:]) for j in range(4)],
                             [srcb, cb], [btb], transpose=True)
                        P.op(ACT, "activation", [btb], [stgb], stg[0:LS, :], bt[0:LS, :], AF.Copy)
                        P.dma(SP, dst[b, i, WIN - LS:WIN], stg[0:LS, :], [stgb], [])

            def fill_qs(jp):
                qr, qrb = qsets[jp % 2]
                P.op(DVE, "tensor_copy", qrb, [qsb], qs[:, jp].rearrange("p i c t -> p c i t"),
                     qr[:, :, TP:TP + NS].rearrange("p c (i t) -> p c i t", t=LS))

            for i in range(5):
                if i < 4 and "sP" not in DBG_SKIP:
                    stage_P(i)
                    if has_s and "sQ" not in DBG_SKIP:
                        fill_qs(i)
                if i >= 1 and "sA" not in DBG_SKIP:
                    stage_A(i - 1)
            if has_s and "sS" not in DBG_SKIP:
                sample_attn()
            if last and "sC" not in DBG_SKIP:
                P.dma(SP, vw_p[b], vl32[:, :], [vl32b], [])
                for (src32, srcb, dst) in ((k32, k32b, kw_p),):
                    bt, btb = psring.next()
                    P.mm([(bt[:, j * 128:(j + 1) * 128], src32[:, j, 0:WIN], ident32[:]) for j in range(4)],
                         [srcb, cb], [btb], transpose=True)
                    P.op(ACT, "activation", [btb], [stgb], stg[:, :], bt[:, :], AF.Copy)
                    P.dma(SP, dst[b], stg[:, :], [stgb], [])
            if not last:
                P.op(DVE, "tensor_copy", krpb, [kvpb[b]], kprev[b][:, :, :], krp[:, :, TP:TP + WIN])
                P.op(DVE, "tensor_copy", [vtmb[4]], [kvpb[b]], vprev[b][:, :], vtm[:, 4, :])
            out_proj(w_aout[b], slabs, nt)
            post_norm(l, 3, False, slabs, nt, ym, ymb)

        def load_x(p, slabs):
            for (c0, n) in slabs:
                for t0 in range(0, n, 128):
                    r = min(128, n - t0)
                    xs_, xsb = xstring.next()
                    src = xp[p * TP + t0:p * TP + t0 + r, :] if c0 == 0 else xs[0:r, :]
                    P.dma(SP, xs_[0:r, :], src, [], [xsb])
                    for q in range(4):
                        bk, bkb = psring.next()
                        P.mm([(bk[:, k * 128:k * 128 + r], xs_[0:r, (4 * q + k) * 128:(4 * q + k + 1) * 128], ident32[0:r, 0:r]) for k in range(4)],
                             [xsb, cb], [bkb], transpose=True)
                        P.op(ACT, "activation", [bkb], xb[4 * q:4 * q + 4], xT[:, 4 * q:4 * q + 4, c0 + t0:c0 + t0 + r],
                             bk[:, :].rearrange("p (k t) -> p k t", t=128)[:, :, 0:r], AF.Copy)

        def store_x(p, slabs):
            for (c0, n) in slabs:
                for t0 in range(0, n, 128):
                    r = min(128, n - t0)
                    xs_, xsb = xstring.next()
                    for q in range(4):
                        bk, bkb = psring.next()
                        P.mm([(bk[0:r, k * 128:(k + 1) * 128], xT[:, 4 * q + k, c0 + t0:c0 + t0 + r], ident32[:]) for k in range(4)],
                             xb[4 * q:4 * q + 4] + [cb], [bkb], transpose=True)
                        P.op(ACT if q % 2 else DVE, "activation" if q % 2 else "tensor_copy", [bkb], [xsb],
                             xs_[0:r, q * 512:(q + 1) * 512], bk[0:r, :], *([AF.Copy] if q % 2 else []))
                    dst = yp[p * TP + t0:p * TP + t0 + r, :] if c0 == 0 else ys[0:r, :]
                    P.dma(SP, dst, xs_[0:r, :], [xsb], [])

        for p in range(NP):
            slabs = slabs_of(p)
            nt = sum(n for _, n in slabs)
            P.barrier()
            load_x(p, slabs)
            P.barrier()
            for l in range(L):
                if "ffn" not in DBG_SKIP:
                    ffn(l, 0, slabs, nt)
                P.barrier()
                if l % 2 == 0:
                    if "hgrn" not in DBG_SKIP:
                        hgrn(l, p, slabs, nt)
                else:
                    if "swa" not in DBG_SKIP:
                        swa(l, p, slabs, nt)
                P.barrier()
                if "ffn" not in DBG_SKIP:
                    ffn(l, 1, slabs, nt)
            P.barrier()
            store_x(p, slabs)
        P.barrier()
    return nc


def _attn_perm():
    idx = np.empty(2048, np.int64)
    for jp in range(4):
        for c in range(4):
            for off in range(2):
                h = 4 * (2 * jp + off) + c
                base = jp * 512 + c * 128 + off * 64
                idx[base:base + 64] = h * 64 + np.arange(64)
    return idx


_CACHE = {}


def run(cfg, inputs):
    ncores = int(os.environ.get('K_NCORES', '8'))
    key = (cfg.depth, cfg.seq, cfg.dff)
    if key not in _CACHE:
        _CACHE[key] = build(cfg)
    nc = _CACHE[key]
    f = lambda a: np.ascontiguousarray(np.asarray(a, dtype=np.float32))
    L, NA, NB = cfg.depth, cfg.na, cfg.nb
    perm = _attn_perm()
    gains = f(np.asarray(inputs["norm_gains"]).reshape(L, 6, NCH, 128).transpose(3, 0, 1, 2).reshape(128, L * 6 * NCH))
    lbl = f(np.asarray(inputs["hgrn_lb_logits"]).reshape(NA, 16, 128).transpose(2, 0, 1).reshape(128, NA * 16))
    hgn = f(np.asarray(inputs["hgrn_norm_gain"]).reshape(NA, 16, 128).transpose(2, 0, 1).reshape(128, NA * 16))
    if NB:
        wa = np.asarray(inputs["w_attn_in"])
        w_ain = f(np.concatenate([wa[:, :, :2048][:, :, perm], wa[:, :, 2048:]], axis=2))
        w_aout = f(np.asarray(inputs["w_attn_out"])[:, perm, :])
        sinks = f(inputs["attn_sinks"])
        ck = np.asarray(inputs["cache_k_win"]).reshape(NB, cfg.dec_batch, WIN, 512)
        cv = np.asarray(inputs["cache_v_win"]).reshape(NB, cfg.dec_batch, WIN, 512)
    else:
        w_ain = np.zeros((1, D, 3072), np.float32); w_aout = np.zeros((1, D, D), np.float32)
        sinks = np.zeros((1, 32), np.float32)
        ck = np.zeros((1, cfg.dec_batch, WIN, 512), np.float32); cv = ck
    consts = host_consts(cfg)
    shared = {
        "gains": gains, "w_fin": f(inputs["w_ffn_in"]), "w_fout": f(inputs["w_ffn_out"]),
        "w_hin": f(inputs["w_hgrn_in"]), "lbl": lbl, "hgn": hgn, "w_hout": f(inputs["w_hgrn_out"]),
        "w_ain": w_ain, "sinks_b": sinks, "w_aout": w_aout,
        "sinks16_d": f(np.repeat(sinks.reshape(max(NB, 1), 8, 4).transpose(2, 0, 1).reshape(4, max(NB, 1) * 8), 4, axis=0)),
    }
    for k, v in consts.items():
        shared["c_" + k] = f(v)
    xp_all = np.asarray(inputs["x_prompt"]); xs_all = np.asarray(inputs["x_sample"])
    st_all = np.asarray(inputs["state_hgrn"])
    in_maps = []
    for c in range(ncores):
        m = dict(shared)
        m["xp"] = f(xp_all[c % cfg.batch])
        sl = slice(c * NSEQ_S, (c + 1) * NSEQ_S)
        m["xs"] = f(xs_all[sl].reshape(NS, D))
        m["st_in"] = f(st_all[:, sl])
        m["ck_in"] = f(ck[:, sl]); m["cv_in"] = f(cv[:, sl])
        in_maps.append(m)
    res = run_bass_kernel_spmd(nc, in_maps, core_ids=list(range(ncores)))
    R = res.results
    yp = np.stack([R[b_ % ncores]["yp"] for b_ in range(cfg.batch)])
    ys = np.concatenate([R[c]["ys"].reshape(NSEQ_S, LS, D) for c in range(ncores)], axis=0)
    hgp = np.stack([R[b_ % ncores]["hg_p"] for b_ in range(cfg.batch)], axis=1)
    hgs = np.concatenate([R[c]["hg_s"] for c in range(ncores)], axis=1)
    out = [yp, ys, hgp, hgs]
    kp = np.stack([R[b_ % ncores]["kw_p"][:NB] for b_ in range(cfg.batch)], axis=1).reshape(NB, cfg.batch, WIN, 8, 64)
    vp = np.stack([R[b_ % ncores]["vw_p"][:NB] for b_ in range(cfg.batch)], axis=1).reshape(NB, cfg.batch, WIN, 8, 64)
    ks = np.concatenate([R[c]["kw_s"][:NB] for c in range(ncores)], axis=1).reshape(NB, ncores * NSEQ_S, WIN, 8, 64)
    vs = np.concatenate([R[c]["vw_s"][:NB] for c in range(ncores)], axis=1).reshape(NB, ncores * NSEQ_S, WIN, 8, 64)
    out += [kp, vp, ks, vs]
    return tuple(np.ascontiguousarray(o, dtype=np.float32) for o in out)


def kernel(**inputs):
    return run(Cfg(), inputs)
```

```python
import contextlib
import numpy as np
import concourse.bass as bass
import concourse.mybir as mybir
from concourse.bass_utils import run_bass_kernel_spmd

F32 = mybir.dt.float32
BF16 = mybir.dt.bfloat16
AF = mybir.ActivationFunctionType
ALU = mybir.AluOpType
AX = mybir.AxisListType

D = 2048
NCH = 16
TP = 512
NSEQ_S = 4
LS = 4
NS = NSEQ_S * LS
NTMAX = TP + NS
WIN = 128
PAST = 16384
EPS = 1e-6
HC = 64
import os
DBG_SKIP = os.environ.get('K_SKIP', '').split(',')
WORKW = 17600


class Cfg:
    def __init__(self, depth=4, seq=2048, dff=5632, batch=4, dec_batch=32):
        self.depth, self.seq, self.dff, self.batch, self.dec_batch = depth, seq, dff, batch, dec_batch
        self.npass = seq // TP
        self.nff = dff // 128
        self.na = (depth + 1) // 2
        self.nb = depth // 2


class Buf:
    __slots__ = ("w", "r")

    def __init__(self):
        self.w = None
        self.r = {}


class Eng:
    def __init__(self, h, sem, is_pe=False):
        self.h, self.sem, self.cnt, self.waited, self.is_pe = h, sem, 0, {}, is_pe

    def wait(self, dep):
        sem, val = dep
        if sem is self.sem and self.is_pe:
            return
        k = id(sem)
        if self.waited.get(k, 0) >= val:
            return
        self.waited[k] = val
        self.h.wait_ge(sem, val)


class Prog:
    def __init__(self, nc, es):
        self.nc, self.es = nc, es
        sm = lambda n: es.enter_context(nc.semaphore(n))
        self.pe = Eng(nc.tensor, sm("s_pe"), True)
        self.act = Eng(nc.scalar, sm("s_act"))
        self.dve = Eng(nc.vector, sm("s_dve"))
        self.pool = Eng(nc.gpsimd, sm("s_pool"))
        self.sp = Eng(nc.sync, sm("s_sp"))
        self.dsems = [[sm("s_d%d" % i), 0] for i in range(12)]
        self.dnext = 0
        self.psems = [[sm("s_q%d" % i), 0] for i in range(2)]
        self.pnext = 0

    def _deps(self, eng, reads, writes):
        for b in reads:
            if b.w is not None:
                eng.wait(b.w)
        for b in writes:
            if b.w is not None:
                eng.wait(b.w)
            for d in b.r.values():
                eng.wait(d)

    def _mark(self, dep, reads, writes):
        for b in reads:
            b.r[id(dep[0])] = dep
        for b in writes:
            b.w = dep
            b.r = {}

    def op(self, eng, name, reads, writes, *a, **k):
        self._deps(eng, reads, writes)
        ins = getattr(eng.h, name)(*a, **k)
        eng.cnt += 1
        ins.then_inc(eng.sem, 1)
        self._mark((eng.sem, eng.cnt), reads, writes)

    def mm(self, mms, reads, writes, transpose=False):
        eng = self.pe
        self._deps(eng, reads, writes)
        n = len(mms)
        ins = None
        for i, m in enumerate(mms):
            if transpose:
                ins = eng.h.transpose(m[0], m[1], m[2])
            else:
                st = m[3] if len(m) > 3 else (i == 0)
                sp = m[4] if len(m) > 4 else (i == n - 1)
                ins = eng.h.matmul(m[0], m[1], m[2], start=st, stop=sp)
        eng.cnt += 1
        ins.then_inc(eng.sem, 1)
        self._mark((eng.sem, eng.cnt), reads, writes)

    def dma(self, eng, out, in_, reads, writes, semslot=None, **k):
        if semslot is None and eng is self.pool:
            semslot = self.psems[self.pnext]
            self.pnext = (self.pnext + 1) % len(self.psems)
        elif semslot is None:
            semslot = self.dsems[self.dnext]
            self.dnext = (self.dnext + 1) % len(self.dsems)
        sem = semslot[0]
        if semslot[1] > 0:
            eng.wait((sem, semslot[1]))
        self._deps(eng, reads, writes)
        eng.h.dma_start(out=out, in_=in_, **k).then_inc(sem, 16)
        semslot[1] += 16
        self._mark((sem, semslot[1]), reads, writes)

    def barrier(self, waiters=None):
        engs = [self.pe, self.act, self.dve, self.pool, self.sp]
        for e in (waiters or [self.pe, self.act, self.dve, self.sp]):
            for f in engs:
                if f is not e and f.cnt > 0:
                    e.wait((f.sem, f.cnt))
            for s in self.dsems + self.psems:
                if s[1] > 0:
                    e.wait((s[0], s[1]))


def pool_sync(P):
    P.barrier(waiters=[P.pool])


class Ring:
    def __init__(self, items):
        self.items, self.i = items, 0

    def next(self):
        it = self.items[self.i]
        self.i = (self.i + 1) % len(self.items)
        return it


def host_consts(cfg):
    c = {}
    c["ident32"] = np.eye(128, dtype=np.float32)
    c["ones32"] = np.ones((128, 128), np.float32)
    R = np.zeros((128, 128), np.float32)
    for blk in range(2):
        for j in range(32):
            R[blk * 64 + j + 32, blk * 64 + j] = -1.0
            R[blk * 64 + j, blk * 64 + j + 32] = 1.0
    c["rotm"] = R
    half = 32
    inv = np.power(np.float32(10000.0), -(np.arange(half, dtype=np.float32) * np.float32(2.0 / 64))).astype(np.float32)
    pos = np.concatenate([np.arange(cfg.seq, dtype=np.float32),
                          (PAST + np.arange(LS)).astype(np.float32)])
    ang = (pos[None, :] * inv[:, None]).astype(np.float32)
    cos = np.cos(ang.astype(np.float64)).astype(np.float32)
    sin = np.sin(ang.astype(np.float64)).astype(np.float32)
    cosT = np.tile(cos, (4, 1))
    sinT = np.tile(sin, (4, 1))
    c["cosT"] = np.concatenate([cosT[:, :cfg.seq]] + [cosT[:, cfg.seq:]] * NSEQ_S, axis=1).copy()
    c["sinT"] = np.concatenate([sinT[:, :cfg.seq]] + [sinT[:, cfg.seq:]] * NSEQ_S, axis=1).copy()
    rm = np.ones((128, NTMAX), np.float32)
    rm[:, 0:TP:HC] = 0.0
    rm[:, TP::LS] = 0.0
    c["rmask"] = rm
    c["cm64"] = np.triu(np.ones((HC, HC), np.float32))
    cmd = np.triu(np.ones((HC, HC), np.float32)); cmd[0:HC // 2, HC // 2:] = 0.0
    c["cmd64"] = cmd
    t = np.arange(128)[:, None]
    s = np.arange(256)[None, :]
    band = ((s >= t) & (s <= t + 128)).astype(np.float32)
    first = band * (s >= 128)
    c["mband"] = band
    c["mfirst"] = first.astype(np.float32)
    tt = (np.arange(16) % 4)[:, None]
    j = np.arange(132)[None, :]
    c["msamp"] = ((j >= tt) & (j <= 128 + tt)).astype(np.float32)
    return c


CONST_SHAPES = {
    "ident32": [128, 128], "ones32": [128, 128], "rotm": [128, 128],
    "rmask": [128, NTMAX], "cm64": [HC, HC], "cmd64": [HC, HC], "mband": [128, 256], "mfirst": [128, 256],
    "msamp": [16, 132],
}


def build(cfg):
    nc = bass.Bass("TRN2", target_bir_lowering=False)
    NP, NFF, NA, NB, DFF = cfg.npass, cfg.nff, cfg.na, cfg.nb, cfg.dff
    L = cfg.depth

    def din(name, shape):
        return nc.dram_tensor(name, list(shape), F32, kind="ExternalInput").ap()

    def dout(name, shape):
        return nc.dram_tensor(name, list(shape), F32, kind="ExternalOutput").ap()

    xp = din("xp", [cfg.seq, D])
    xs = din("xs", [NS, D])
    st_in = din("st_in", [NA, NSEQ_S, 16, 128, 128])
    ck_in = din("ck_in", [max(NB, 1), NSEQ_S, WIN, 512])
    cv_in = din("cv_in", [max(NB, 1), NSEQ_S, WIN, 512])
    gains = din("gains", [128, L * 6 * NCH])
    w_fin = din("w_fin", [L, 2, NFF // 2, 128, 8192])
    w_fout = din("w_fout", [L, 2, NCH, 128, NFF * 128])
    w_hin = din("w_hin", [NA, D, 8192])
    lbl = din("lbl", [128, NA * 16])
    hgn = din("hgn", [128, NA * 16])
    w_hout = din("w_hout", [NA, D, D])
    w_ain = din("w_ain", [max(NB, 1), D, 3072])
    sinks_b = din("sinks_b", [max(NB, 1), 32])
    w_aout = din("w_aout", [max(NB, 1), D, D])
    sinks16_d = din("sinks16_d", [16, max(NB, 1) * 8])
    cst = {k: din("c_" + k, v) for k, v in CONST_SHAPES.items()}
    cosT_d = din("c_cosT", [128, cfg.seq + NS])
    sinT_d = din("c_sinT", [128, cfg.seq + NS])

    yp = dout("yp", [cfg.seq, D])
    ys = dout("ys", [NS, D])
    hg_p = dout("hg_p", [NA, 16, 128, 128])
    hg_s = dout("hg_s", [NA, NSEQ_S, 16, 128, 128])
    kw_p = dout("kw_p", [max(NB, 1), WIN, 512])
    vw_p = dout("vw_p", [max(NB, 1), WIN, 512])
    kw_s = dout("kw_s", [max(NB, 1), NSEQ_S, WIN, 512])
    vw_s = dout("vw_s", [max(NB, 1), NSEQ_S, WIN, 512])

    with contextlib.ExitStack() as es:
        P = Prog(nc, es)
        PE, ACT, DVE, POOL, SP = P.pe, P.act, P.dve, P.pool, P.sp

        def sb(name, shape, dt=F32):
            return es.enter_context(nc.sbuf_tensor(name, list(shape), dt))

        xT = sb("xT", [128, NCH, NTMAX])
        xb = [Buf() for _ in range(NCH)]
        yT = sb("yT", [128, NCH, NTMAX])
        yb = [Buf() for _ in range(NCH)]
        hT = yT[:, 0:NCH // 2, :].bitcast(BF16).rearrange("p a (b n) -> p (a b) n", b=2)
        hbufs = yb[0:NCH // 2]
        work = sb("work", [128, WORKW])
        ob = yT[:, NCH // 2:NCH, :].bitcast(BF16).rearrange("p a (b n) -> p (a b) n", b=2)
        obb = [yb[NCH // 2 + c // 2] for c in range(NCH)]
        ym = work[:, 0:NCH * NTMAX].rearrange("p (c n) -> p c n", n=NTMAX)
        ymb = [Buf() for _ in range(NCH)]
        rstd = sb("rstd", [128, NTMAX])
        rstdb = Buf()
        wr = [sb("wr%d" % i, [128, 8192], BF16) for i in range(2)]
        wrb = [Buf() for _ in range(2)]
        wsem = [[es.enter_context(nc.semaphore("s_w%d" % i)), 0] for i in range(2)]
        wring = Ring(list(range(2)))
        sqr = [sb("sq%d" % i, [128, NTMAX]) for i in range(2)]
        sqring = Ring([(sqr[i], Buf()) for i in range(2)])
        tmpr = [sb("tmp%d" % i, [128, 512]) for i in range(3)]
        tmpring = Ring([(tmpr[i], Buf()) for i in range(3)])
        g_sb = sb("g_sb", [128, L * 6 * NCH])
        gh_sb = sb("gh_sb", [128, L * 6 * NCH])
        gb = Buf()
        ident32 = sb("ident32", [128, 128]); ident16 = sb("ident16", [128, 128], BF16)
        ones32 = sb("ones32", [128, 128]); rotm = sb("rotm", [128, 128])
        rmask = sb("rmask", [128, NTMAX]); cm64 = sb("cm64", [HC, HC]); cmd64 = sb("cmd64", [HC, HC])
        mband = sb("mband", [128, 256]); mfirst = sb("mfirst", [128, 256]); msamp = sb("msamp", [16, 132])
        cb = Buf()
        lb_sb = sb("lb_sb", [128, NA * 16]); oml_sb = sb("oml_sb", [128, NA * 16]); hgn_sb = sb("hgn_sb", [128, NA * 16])
        sscr = nc.dram_tensor("sscr", [NA, 16, 128, 128], F32).ap()
        if NB:
            kprev = [sb("kprev%d" % b, [128, 4, WIN], BF16) for b in range(NB)]
            vprev = [sb("vprev%d" % b, [128, 512], BF16) for b in range(NB)]
            kvpb = [Buf() for _ in range(NB)]
            sink_sb = sb("sink_sb", [128, NB * 32]); nsink_sb = sb("nsink_sb", [128, NB * 32])
            sinks16 = sb("sinks16", [16, NB * 8]); nsinks16 = sb("nsinks16", [16, NB * 8])
            cosT = sb("cosT", [128, NTMAX]); sinT = sb("sinT", [128, NTMAX])
            ropeb = Buf()
        yflat = yT[:, :, :].rearrange("p c n -> p (c n)")
        xstring = Ring([(yflat[:, i * D:(i + 1) * D], Buf()) for i in range(2)])

        ps = es.enter_context(nc.psum_tensor("ps", [128, 8, 512], F32))
        psring = Ring([(ps[:, i, :], Buf()) for i in range(6)])
        accring = Ring([(ps[:, i, :], Buf()) for i in range(6, 8)])

        def wcols(off, n):
            return slice(off, off + n)

        def ld(dst, src, bufs):
            P.dma(SP, dst, src, [], bufs)

        ld(g_sb[:], gains, [gb])
        for nm, t_ in (("ident32", ident32), ("ones32", ones32), ("rotm", rotm), ("rmask", rmask),
                       ("cm64", cm64), ("cmd64", cmd64), ("mband", mband), ("mfirst", mfirst), ("msamp", msamp)):
            ld(t_[:], cst[nm], [cb])
        ld(lb_sb[:], lbl, [cb])
        ld(hgn_sb[:], hgn, [cb])
        P.op(DVE, "tensor_copy", [cb], [cb], ident16[:], ident32[:])
        P.op(DVE, "tensor_scalar", [gb], [gb], gh_sb[:], g_sb[:], 0.5, None, ALU.mult)
        P.op(DVE, "memset", [], [cb], oml_sb[:], 1.0)
        if NA > 1:
            assert NA == 2
            P.op(DVE, "tensor_tensor", [cb], [cb], oml_sb[:, 16:32], lb_sb[:, 16:32], lb_sb[:, 0:16], ALU.subtract)
            P.op(ACT, "activation", [cb], [cb], lb_sb[:, 16:32], oml_sb[:, 16:32], AF.Sigmoid)
            P.op(DVE, "tensor_scalar", [cb], [cb], oml_sb[:, 16:32], lb_sb[:, 16:32], -1.0, 1.0, ALU.mult, ALU.add)
        P.op(DVE, "memset", [cb], [cb], lb_sb[:, 0:16], 0.0)
        for b in range(NB):
            P.op(DVE, "memset", [], [kvpb[b]], kprev[b][:], 0.0)
            P.op(DVE, "memset", [], [kvpb[b]], vprev[b][:], 0.0)
            ld(sink_sb[:, b * 32:(b + 1) * 32], sinks_b[b:b + 1, :].partition_broadcast(128), [cb])
        if NB:
            ld(sinks16[:], sinks16_d, [cb])
            P.op(DVE, "tensor_scalar", [cb], [cb], nsink_sb[:], sink_sb[:], -1.0, None, ALU.mult)
            P.op(DVE, "tensor_scalar", [cb], [cb], nsinks16[:], sinks16[:], -1.0, None, ALU.mult)

        def gcol(tbl, l, n, c):
            i = (l * 6 + n) * NCH + c
            return tbl[:, i:i + 1]

        def wload(parts):
            s = wring.next()
            for (off, n, kc, src) in parts:
                dst = wr[s][:, off:off + kc * n].rearrange("p (k n) -> p k n", n=n)
                P.dma(POOL, dst, src, [], [wrb[s]], semslot=wsem[s])
            return s

        def wload_flat(src2d, ncols):
            s = wring.next()
            b = max(d for d in range(1, 2049) if ncols % d == 0)
            P.dma(POOL, wr[s][:, 0:ncols].rearrange("p (a b) -> p a b", b=b), src2d.rearrange("p (a b) -> p a b", b=b),
                  [], [wrb[s]], semslot=wsem[s])
            return s

        def slabs_of(p):
            return [(0, TP)] + ([(TP, NS)] if p == 0 else [])

        def compute_rstd(src, sbufs, slabs, nt):
            for (c0, n) in slabs:
                bank, bb = psring.next()
                for c in range(NCH):
                    sq, sqb = sqring.next()
                    P.op(ACT, "activation", [sbufs[c]], [sqb], sq[:, 0:n], src[:, c, c0:c0 + n], AF.Square)
                    P.mm([(bank[:, 0:n], ones32[:], sq[:, 0:n], c == 0, c == NCH - 1)], [sqb, cb], [bb])
                t_, tb = tmpring.next()
                P.op(ACT, "activation", [bb], [tb], t_[:, 0:n], bank[:, 0:n], AF.Sqrt, bias=EPS, scale=1.0 / D)
                P.op(DVE, "reciprocal", [tb], [rstdb], rstd[:, c0:c0 + n], t_[:, 0:n])

        def pre_norm(l, n_, slabs, nt):
            compute_rstd(xT, xb, slabs, nt)
            for c in range(NCH):
                P.op(DVE, "scalar_tensor_tensor", [xb[c], rstdb, gb], [hbufs[c // 2]],
                     hT[:, c, 0:nt], xT[:, c, 0:nt], gcol(g_sb, l, n_, c), rstd[:, 0:nt], ALU.mult, ALU.mult)

        def post_norm(l, n_, half, slabs, nt, src=None, srcb=None):
            if src is None:
                src, srcb = yT, yb
            compute_rstd(src, srcb, slabs, nt)
            tbl = gh_sb if half else g_sb
            for c in range(NCH):
                P.op(DVE, "scalar_tensor_tensor", [rstdb, gb], [srcb[c]],
                     src[:, c, 0:nt], src[:, c, 0:nt], gcol(tbl, l, n_, c), rstd[:, 0:nt], ALU.mult, ALU.mult)
                P.op(DVE, "tensor_tensor", [srcb[c]], [xb[c]], xT[:, c, 0:nt], xT[:, c, 0:nt], src[:, c, 0:nt], ALU.add)

        actT = work[:, 0:NFF * NTMAX // 2].bitcast(BF16).rearrange("p (j n) -> p j n", n=NTMAX)
        ab = [Buf() for _ in range(NFF)]

        def ffn(l, i, slabs, nt):
            pre_norm(l, 0 if i == 0 else 4, slabs, nt)
            for fb in range(NFF // 2):
                s = wload_flat(w_fin[l, i, fb], 8192)
                ws = wr[s][:, :].rearrange("p (g c n) -> p g c n", g=2, c=16)
                for j in range(2):
                    J = 2 * fb + j
                    for (c0, n) in slabs:
                        bg, bgb = psring.next()
                        P.mm([(bg[:, 0:n], ws[:, 0, c, j * 128:(j + 1) * 128], hT[:, c, c0:c0 + n]) for c in range(NCH)],
                             [wrb[s]] + hbufs, [bgb])
                        bu, bub = psring.next()
                        P.mm([(bu[:, 0:n], ws[:, 1, c, j * 128:(j + 1) * 128], hT[:, c, c0:c0 + n]) for c in range(NCH)],
                             [wrb[s]] + hbufs, [bub])
                        t_, tb = tmpring.next()
                        P.op(ACT, "activation", [bgb], [tb], t_[:, 0:n], bg[:, 0:n], AF.Silu)
                        P.op(DVE, "tensor_tensor", [tb, bub], [ab[J]], actT[:, J, c0:c0 + n], t_[:, 0:n], bu[:, 0:n], ALU.mult)
            for dc in range(NCH):
                s = wload_flat(w_fout[l, i, dc], NFF * 128)
                ws = wr[s][:, 0:NFF * 128].rearrange("p (j n) -> p j n", n=128)
                for (c0, n) in slabs:
                    bk, bkb = psring.next()
                    P.mm([(bk[:, 0:n], ws[:, j, :], actT[:, j, c0:c0 + n]) for j in range(NFF)], [wrb[s]] + ab, [bkb])
                    P.op(ACT, "activation", [bkb], [yb[dc]], yT[:, dc, c0:c0 + n], bk[:, 0:n], AF.Copy)
            post_norm(l, 1 if i == 0 else 5, True, slabs, nt)

        def out_proj(wmat, slabs, nt):
            P.barrier()
            wv = wmat.rearrange("(c p) n -> p c n", p=128)
            for dcb in range(4):
                s = wload([(0, 512, 16, wv[:, :, dcb * 512:(dcb + 1) * 512])])
                ws = wr[s][:, :].rearrange("p (c n) -> p c n", n=512)
                for k in range(4):
                    dc = dcb * 4 + k
                    for (c0, n) in slabs:
                        bk, bkb = psring.next()
                        P.mm([(bk[:, 0:n], ws[:, c, k * 128:(k + 1) * 128], ob[:, c, c0:c0 + n]) for c in range(NCH)],
                             [wrb[s]] + yb[NCH // 2:], [bkb])
                        P.op(ACT, "activation", [bkb], [ymb[dc]], ym[:, dc, c0:c0 + n], bk[:, 0:n], AF.Copy)

        def carver():
            off = [0]

            def carve(nwords, dt=F32):
                v = work[:, off[0]:off[0] + nwords]
                off[0] += nwords
                assert off[0] <= WORKW, off[0]
                if dt == BF16:
                    v = v.bitcast(BF16)
                return v
            return carve

        def hgrn(l, p, slabs, nt):
            a = l // 2
            last = (p == NP - 1)
            has_s = (p == 0)
            pre_norm(l, 2, slabs, nt)
            carve = carver()
            sets = []
            for _ in range(2):
                d_ = {}
                for nm in ("qf", "sg", "kk", "bb", "dd", "dl", "sgl"):
                    d_[nm] = (carve(NTMAX), Buf())
                for nm in ("vbf", "qt", "kt", "qh", "kh", "pxk", "pxq"):
                    d_[nm] = (carve(NTMAX // 2, BF16), Buf())
                d_["At"] = (carve(256, BF16), Buf())
                d_["Ats"] = (carve(8, BF16), Buf())
                d_["khT"] = (carve(512, BF16), Buf())
                d_["vT"] = (carve(512, BF16), Buf())
                d_["khTs"] = (carve(256, BF16), Buf())
                d_["vTs"] = (carve(256, BF16), Buf())
                d_["Sbf"] = (carve(512, BF16), Buf())
                d_["Ss"] = (carve(512), Buf())
                d_["Sbs"] = (carve(256, BF16), Buf())
                d_["Sw"] = (carve(128), Buf())
                P.op(DVE, "memset", [], [d_["pxk"][1]], d_["pxk"][0][:, :], 0.0)
                P.op(DVE, "memset", [], [d_["pxq"][1]], d_["pxq"][0][:, :], 0.0)
                sets.append(d_)
            wv = w_hin[a].rearrange("(c p) n -> p c n", p=128)
            npc = TP // HC

            def stage_P(h):
                S_ = sets[h % 2]
                s = wload([(k * 2048, 128, 16, wv[:, :, k * 2048 + h * 128:k * 2048 + (h + 1) * 128]) for k in range(4)])
                ws = wr[s][:, :].rearrange("p (k c n) -> p k c n", k=4, c=16)
                for (c0, n) in slabs:
                    banks = []
                    for k in range(4):
                        bk, bkb = psring.next()
                        P.mm([(bk[:, 0:n], ws[:, k, c, :], hT[:, c, c0:c0 + n]) for c in range(NCH)], [wrb[s]] + hbufs, [bkb])
                        banks.append((bk, bkb))
                    sl = slice(c0, c0 + n)
                    P.op(ACT, "activation", [banks[0][1]], [S_["qf"][1]], S_["qf"][0][:, sl], banks[0][0][:, 0:n], AF.Copy)
                    P.op(ACT, "activation", [banks[1][1]], [S_["sg"][1]], S_["sg"][0][:, sl], banks[1][0][:, 0:n], AF.Sigmoid)
                    P.op(DVE, "tensor_copy", [banks[2][1]], [S_["vbf"][1]], S_["vbf"][0][:, sl], banks[2][0][:, 0:n])
                    P.op(ACT, "activation", [banks[3][1]], [S_["sgl"][1]], S_["sgl"][0][:, sl], banks[3][0][:, 0:n], AF.Silu)
                if p == 0:
                    P.op(DVE, "memset", [], [S_["Sw"][1]], S_["Sw"][0], 0.0)
                else:
                    P.dma(SP, S_["Sw"][0], sscr[a, h], [], [S_["Sw"][1]])

            def cview(t_, n_chunks, csz, base=0):
                return t_[:, base:base + n_chunks * csz].rearrange("p (c t) -> p c t", t=csz)

            def stage_E(h):
                S_ = sets[h % 2]
                g = lambda nm: S_[nm][0]
                B = lambda nm: S_[nm][1]
                hi = a * 16 + h
                N_ = slice(0, nt)
                P.op(DVE, "tensor_scalar", [B("sg"), cb], [B("sg")], g("sg")[:, N_], g("sg")[:, N_],
                     oml_sb[:, hi:hi + 1], lb_sb[:, hi:hi + 1], ALU.mult, ALU.add)
                P.op(DVE, "tensor_scalar", [B("sg")], [B("kk")], g("kk")[:, N_], g("sg")[:, N_], -1.0, 1.0, ALU.mult, ALU.add)
                P.op(ACT, "activation", [B("sg")], [B("sg")], g("sg")[:, N_], g("sg")[:, N_], AF.Ln)
                P.op(DVE, "tensor_tensor_scan", [B("sg"), cb], [B("bb")], g("bb")[:, N_], rmask[:, N_], g("sg")[:, N_], 0.0, ALU.mult, ALU.add)
                HH = HC // 2
                bvc = cview(g("bb"), npc, HC, 0)
                dlv = cview(g("dl"), npc, HC, 0)
                b31 = bvc[:, :, HH - 1:HH].to_broadcast([128, npc, HH])
                P.op(DVE, "tensor_tensor", [B("bb")], [B("dl")], dlv[:, :, 0:HH], b31, bvc[:, :, 0:HH], ALU.subtract)
                P.op(DVE, "tensor_tensor", [B("bb")], [B("dl")], dlv[:, :, HH:HC], bvc[:, :, HH:HC], b31, ALU.subtract)
                P.op(ACT, "activation", [B("dl")], [B("dl")], g("dl")[:, 0:TP], g("dl")[:, 0:TP], AF.Exp)
                P.op(DVE, "tensor_tensor", [B("kk"), B("dl")], [B("pxk")], cview(g("pxk"), npc, HC, 0)[:, :, 0:HH],
                     cview(g("kk"), npc, HC, 0)[:, :, 0:HH], cview(g("dl"), npc, HC, 0)[:, :, 0:HH], ALU.mult)
                P.op(DVE, "tensor_tensor", [B("qf"), B("dl")], [B("pxq")], cview(g("pxq"), npc, HC, 0)[:, :, HH:HC],
                     cview(g("qf"), npc, HC, 0)[:, :, HH:HC], cview(g("dl"), npc, HC, 0)[:, :, HH:HC], ALU.mult)
                parts = [(2 * npc, HH, 0)] + ([(NSEQ_S, LS, TP)] if has_s else [])
                for (ncx, csz, base) in parts:
                    bv = cview(g("bb"), ncx, csz, base)
                    mid = csz // 2 - 1
                    P.op(DVE, "tensor_tensor", [B("bb")], [B("dd")], cview(g("dd"), ncx, csz, base), bv,
                         bv[:, :, mid:mid + 1].to_broadcast([128, ncx, csz]), ALU.subtract)
                parts = [(npc, HC, 0)] + ([(NSEQ_S, LS, TP)] if has_s else [])
                for (ncx, csz, base) in parts:
                    bv = cview(g("bb"), ncx, csz, base)
                    P.op(DVE, "tensor_tensor", [B("bb")], [B("dl")], cview(g("dl"), ncx, csz, base),
                         bv[:, :, csz - 1:csz].to_broadcast([128, ncx, csz]), bv, ALU.subtract)
                P.op(ACT, "activation", [B("dd")], [B("dd")], g("dd")[:, N_], g("dd")[:, N_], AF.Exp)
                P.op(DVE, "tensor_tensor", [B("qf"), B("dd")], [B("qt")], g("qt")[:, N_], g("qf")[:, N_], g("dd")[:, N_], ALU.mult)
                P.op(DVE, "reciprocal", [B("dd")], [B("dd")], g("dd")[:, N_], g("dd")[:, N_])
                P.op(DVE, "tensor_tensor", [B("kk"), B("dd")], [B("kt")], g("kt")[:, N_], g("kk")[:, N_], g("dd")[:, N_], ALU.mult)
                P.op(ACT, "activation", [B("bb")], [B("bb")], g("bb")[:, N_], g("bb")[:, N_], AF.Exp)
                P.op(DVE, "tensor_tensor", [B("qf"), B("bb")], [B("qh")], g("qh")[:, N_], g("qf")[:, N_], g("bb")[:, N_], ALU.mult)
                P.op(ACT, "activation", [B("dl")], [B("dl")], g("dl")[:, N_], g("dl")[:, N_], AF.Exp)
                P.op(DVE, "tensor_tensor", [B("kk"), B("dl")], [B("kh")], g("kh")[:, N_], g("kk")[:, N_], g("dl")[:, N_], ALU.mult)

            def stage_T(h):
                S_ = sets[h % 2]
                g = lambda nm: S_[nm][0]
                B = lambda nm: S_[nm][1]
                bk, bkb = psring.next()
                P.mm([(bk[0:HC, ci * HC:(ci + 1) * HC], g("kt")[:, ci * HC:(ci + 1) * HC], g("qt")[:, ci * HC:(ci + 1) * HC], True, True)
                      for ci in range(npc)], [B("kt"), B("qt")], [bkb])
                bkx, bkxb = psring.next()
                P.mm([(bkx[0:HC, ci * HC:(ci + 1) * HC], g("pxk")[:, ci * HC:(ci + 1) * HC], g("pxq")[:, ci * HC:(ci + 1) * HC], True, True)
                      for ci in range(npc)], [B("pxk"), B("pxq")], [bkxb])
                At = g("At")[0:HC, :].rearrange("p (c t) -> p c t", t=HC)
                tmpA = g("dd")[0:HC, 0:TP].rearrange("p (c t) -> p c t", t=HC)
                P.op(DVE, "tensor_tensor", [bkb, cb], [B("dd")], tmpA, bk[0:HC, :].rearrange("p (c t) -> p c t", t=HC),
                     cmd64[:, :].unsqueeze(1).to_broadcast([HC, npc, HC]), ALU.mult)
                P.op(DVE, "tensor_tensor", [B("dd"), bkxb], [B("At")], At, tmpA,
                     bkx[0:HC, :].rearrange("p (c t) -> p c t", t=HC), ALU.add)
                bk2, bk2b = psring.next()
                k16 = bk2.bitcast(BF16)
                P.mm([(k16[0:HC, ci * 128:(ci + 1) * 128], g("kh")[:, ci * HC:(ci + 1) * HC], ident16[:]) for ci in range(npc)],
                     [B("kh"), cb], [bk2b], transpose=True)
                P.op(ACT, "activation", [bk2b], [B("khT")], g("khT")[0:HC, :], k16[0:HC, :], AF.Copy)
                bk3, bk3b = psring.next()
                v16 = bk3.bitcast(BF16)
                P.mm([(v16[0:HC, ci * 128:(ci + 1) * 128], g("vbf")[:, ci * HC:(ci + 1) * HC], ident16[:]) for ci in range(npc)],
                     [B("vbf"), cb], [bk3b], transpose=True)
                P.op(DVE, "tensor_copy", [bk3b], [B("vT")], g("vT")[0:HC, :], v16[0:HC, :])
                if has_s:
                    bk4, bk4b = psring.next()
                    P.mm([(bk4[0:LS, i * LS:(i + 1) * LS], g("kt")[:, TP + i * LS:TP + (i + 1) * LS], g("qt")[:, TP + i * LS:TP + (i + 1) * LS], True, True)
                          for i in range(NSEQ_S)], [B("kt"), B("qt")], [bk4b])
                    P.op(DVE, "tensor_tensor", [bk4b, cb], [B("Ats")], g("Ats")[0:LS, :].rearrange("p (c t) -> p c t", t=LS),
                         bk4[0:LS, 0:NS].rearrange("p (c t) -> p c t", t=LS),
                         cm64[0:LS, 0:LS].unsqueeze(1).to_broadcast([LS, NSEQ_S, LS]), ALU.mult)
                    bk5, bk5b = psring.next()
                    s16 = bk5.bitcast(BF16)
                    P.mm([(s16[0:LS, i * 128:(i + 1) * 128], g("kh")[:, TP + i * LS:TP + (i + 1) * LS], ident16[:]) for i in range(NSEQ_S)]
                         + [(s16[0:LS, 512 + i * 128:512 + (i + 1) * 128], g("vbf")[:, TP + i * LS:TP + (i + 1) * LS], ident16[:]) for i in range(NSEQ_S)],
                         [B("kh"), B("vbf"), cb], [bk5b], transpose=True)
                    P.op(ACT, "activation", [bk5b], [B("khTs")], g("khTs")[0:LS, :], s16[0:LS, 0:512], AF.Copy)
                    P.op(ACT, "activation", [bk5b], [B("vTs")], g("vTs")[0:LS, :], s16[0:LS, 512:1024], AF.Copy)

            def stage_U(h):
                S_ = sets[h % 2]
                g = lambda nm: S_[nm][0]
                B = lambda nm: S_[nm][1]
                khT = g("khT")[0:HC, :].rearrange("p (c k) -> p c k", k=128)
                vT = g("vT")[0:HC, :].rearrange("p (c k) -> p c k", k=128)
                Sbf = g("Sbf").rearrange("p (c k) -> p c k", k=128)
                S = g("Sw")
                for half in range(npc // 4):
                    bk, bkb = psring.next()
                    P.mm([(bk[:, k * 128:(k + 1) * 128], khT[:, half * 4 + k, :], vT[:, half * 4 + k, :], True, True) for k in range(4)],
                         [B("khT"), B("vT")], [bkb])
                    for k in range(4):
                        ci = half * 4 + k
                        P.op(DVE, "tensor_copy", [B("Sw")], [B("Sbf")], Sbf[:, ci, :], S)
                        P.op(DVE, "scalar_tensor_tensor", [B("bb"), bkb], [B("Sw")], S, S,
                             g("bb")[:, ci * HC + HC - 1:ci * HC + HC], bk[:, k * 128:(k + 1) * 128], ALU.mult, ALU.add)
                if last:
                    P.dma(SP, hg_p[a, h], S, [B("Sw")], [])
                else:
                    P.dma(SP, sscr[a, h], S, [B("Sw")], [])
                if has_s:
                    Ss = g("Ss").rearrange("p (i k) -> p i k", k=128)
                    P.dma(SP, Ss, st_in[a, :, h].rearrange("i k v -> k i v"), [], [B("Ss")])
                    P.op(DVE, "tensor_copy", [B("Ss")], [B("Sbs")], g("Sbs"), g("Ss"))
                    bk, bkb = psring.next()
                    khTs = g("khTs")[0:LS, :].rearrange("p (i k) -> p i k", k=128)
                    vTs = g("vTs")[0:LS, :].rearrange("p (i k) -> p i k", k=128)
                    P.mm([(bk[:, i * 128:(i + 1) * 128], khTs[:, i, :], vTs[:, i, :], True, True) for i in range(NSEQ_S)],
                         [B("khTs"), B("vTs")], [bkb])
                    dec = g("bb")[:, TP:TP + NS].rearrange("p (i t) -> p i t", t=LS)[:, :, LS - 1:LS]
                    P.op(DVE, "tensor_tensor", [B("bb")], [B("Ss")], Ss, Ss, dec.to_broadcast([128, NSEQ_S, 128]), ALU.mult)
                    P.op(DVE, "tensor_tensor", [bkb], [B("Ss")], Ss, Ss, bk[:, :].rearrange("p (i k) -> p i k", k=128), ALU.add)
                    P.dma(SP, hg_s[a, :, h].rearrange("i k v -> k i v"), Ss, [B("Ss")], [])

            def stage_O(h):
                S_ = sets[h % 2]
                g = lambda nm: S_[nm][0]
                B = lambda nm: S_[nm][1]
                At = g("At")[0:HC, :].rearrange("p (c t) -> p c t", t=HC)
                vT = g("vT")[0:HC, :].rearrange("p (c k) -> p c k", k=128)
                Sbf = g("Sbf").rearrange("p (c k) -> p c k", k=128)
                obank = []
                bk, bkb = psring.next()
                mms = []
                for ci in range(npc):
                    o_ = bk[:, ci * HC:(ci + 1) * HC]
                    mms.append((o_, vT[:, ci, :], At[:, ci, :], True, False))
                    mms.append((o_, Sbf[:, ci, :], g("qh")[:, ci * HC:(ci + 1) * HC], False, True))
                P.mm(mms, [B("vT"), B("At"), B("Sbf"), B("qh")], [bkb])
                obank.append((bk, bkb, 0, TP))
                if has_s:
                    bk2, bk2b = psring.next()
                    Ats = g("Ats")[0:LS, :].rearrange("p (c t) -> p c t", t=LS)
                    vTs = g("vTs")[0:LS, :].rearrange("p (i k) -> p i k", k=128)
                    Sbs = g("Sbs").rearrange("p (i k) -> p i k", k=128)
                    mms = []
                    for i in range(NSEQ_S):
                        o_ = bk2[:, i * LS:(i + 1) * LS]
                        mms.append((o_, vTs[:, i, :], Ats[:, i, :], True, False))
                        mms.append((o_, Sbs[:, i, :], g("qh")[:, TP + i * LS:TP + (i + 1) * LS], False, True))
                    P.mm(mms, [B("vTs"), B("Ats"), B("Sbs"), B("qh")], [bk2b])
                    obank.append((bk2, bk2b, TP, NS))
                hi = a * 16 + h
                for (bk_, bkb_, c0, n) in obank:
                    sl = slice(c0, c0 + n)
                    P.op(ACT, "activation", [bkb_], [B("dd")], g("dd")[:, sl], bk_[:, 0:n], AF.Square)
                    bn, bnb = psring.next()
                    P.mm([(bn[:, 0:n], ones32[:], g("dd")[:, sl], True, True)], [B("dd"), cb], [bnb])
                    P.op(ACT, "activation", [bnb], [B("dl")], g("dl")[:, sl], bn[:, 0:n], AF.Sqrt, bias=EPS, scale=1.0 / 128)
                    P.op(DVE, "reciprocal", [B("dl")], [B("dl")], g("dl")[:, sl], g("dl")[:, sl])
                    P.op(DVE, "scalar_tensor_tensor", [bkb_, B("dl"), cb], [B("dd")], g("dd")[:, sl], bk_[:, 0:n],
                         hgn_sb[:, hi:hi + 1], g("dl")[:, sl], ALU.mult, ALU.mult)
                    P.op(DVE, "tensor_tensor", [B("dd"), B("sgl")], [obb[h]], ob[:, h, sl], g("dd")[:, sl],
                         g("sgl")[:, sl], ALU.mult)

            stage_P(0)
            stage_E(0)
            for h in range(16):
                if h + 1 < 16:
                    stage_P(h + 1)
                stage_T(h)
                stage_U(h)
                stage_O(h)
                if h + 1 < 16:
                    stage_E(h + 1)
            out_proj(w_hout[a], slabs, nt)
            post_norm(l, 3, False, slabs, nt, ym, ymb)

        def swa(l, p, slabs, nt):
            b = l // 2
            last = (p == NP - 1)
            has_s = (p == 0)
            pre_norm(l, 2, slabs, nt)
            carve = carver()
            k32 = carve(4 * (WIN + NS)).rearrange("p (j n) -> p j n", n=WIN + NS); k32b = Buf()
            vl32 = carve(512); vl32b = Buf()
            vsst_r = Ring([(carve(128), Buf()) for _ in range(2)])
            krp = carve(4 * (WIN + TP) // 2, BF16).rearrange("p (j n) -> p j n", n=WIN + TP); krpb = [Buf() for _ in range(4)]
            vtm = carve(5 * 512 // 2, BF16).rearrange("p (t n) -> p t n", n=512); vtmb = [Buf() for _ in range(5)]
            qsets = []
            for _ in range(2):
                qsets.append((carve(4 * NTMAX // 2, BF16).rearrange("p (c n) -> p c n", n=NTMAX), [Buf() for _ in range(4)]))
            vbf = carve(NTMAX // 2, BF16); vbfb = Buf()
            qf_r = Ring([(carve(NTMAX), Buf()) for _ in range(2)])
            t2_r = Ring([(carve(NTMAX), Buf()) for _ in range(2)])
            r32_r = Ring([(carve(NTMAX), Buf()) for _ in range(2)])
            pe_r = Ring([(carve(256), Buf()) for _ in range(3)])
            pm_r = Ring([(carve(256), Buf()) for _ in range(3)])
            pn_r = Ring([(carve(256), Buf()) for _ in range(3)])
            pT_r = Ring([(carve(128, BF16), Buf()) for _ in range(3)])
            sm_r = Ring([(carve(8), Buf()) for _ in range(4)])
            stg = carve(512); stgb = Buf()
            if has_s:
                qs = carve(4 * 64 // 2, BF16).rearrange("p (j i c t) -> p j i c t", j=4, i=4, c=4); qsb = Buf()
                kcs = carve(16 * 132 // 2, BF16).rearrange("p (j i n) -> p j i n", j=4, i=4); kcsb = Buf()
                vcs = carve(4 * 512 // 2, BF16).rearrange("p (i n) -> p i n", n=512); vcsb = Buf()
                vns = carve(4 * 512 // 2, BF16).rearrange("p (i n) -> p i n", n=512); vnsb = Buf()
                kc32 = carve(512); kc32b = Buf()

            P.op(DVE, "tensor_copy", [kvpb[b]], krpb, krp[:, :, 0:WIN], kprev[b][:, :, :])
            P.op(DVE, "tensor_copy", [kvpb[b]], [vtmb[0]], vtm[:, 0, :], vprev[b][:, :])
            P.dma(SP, cosT[:, 0:TP], cosT_d[:, p * TP:(p + 1) * TP], [], [ropeb])
            P.dma(SP, sinT[:, 0:TP], sinT_d[:, p * TP:(p + 1) * TP], [], [ropeb])
            if has_s:
                P.dma(SP, cosT[:, TP:TP + NS], cosT_d[:, cfg.seq:cfg.seq + NS], [], [ropeb])
                P.dma(SP, sinT[:, TP:TP + NS], sinT_d[:, cfg.seq:cfg.seq + NS], [], [ropeb])

            wv = w_ain[b].rearrange("(c p) n -> p c n", p=128)

            def rope(bank, bankb, n, c0, outs):
                qf, qfb = qf_r.next()
                P.op(ACT, "activation", [bankb], [qfb], qf[:, 0:n], bank[:, 0:n], AF.Copy)
                if "sR" in DBG_SKIP:
                    for (en, out_ap, bufs, cs, v3) in outs:
                        src = qf[:, cs]
                        if v3:
                            src = src.rearrange("p (i t) -> p i t", t=LS)
                        P.op(DVE, "tensor_copy", [qfb], bufs, out_ap, src)
                    return
                br, brb = psring.next()
                P.mm([(br[:, 0:n], rotm[:], qf[:, 0:n], True, True)], [qfb, cb], [brb])
                t2, t2b = t2_r.next()
                P.op(DVE, "tensor_tensor", [brb, ropeb], [t2b], t2[:, 0:n], br[:, 0:n], sinT[:, c0:c0 + n], ALU.mult)
                P.op(DVE, "tensor_tensor", [qfb, ropeb], [qfb], qf[:, 0:n], qf[:, 0:n], cosT[:, c0:c0 + n], ALU.mult)
                r32, r32b = r32_r.next()
                P.op(DVE, "tensor_tensor", [qfb, t2b], [r32b], r32[:, 0:n], qf[:, 0:n], t2[:, 0:n], ALU.add)
                for (en, out_ap, bufs, cs, v3) in outs:
                    src = r32[:, cs]
                    if v3:
                        src = src.rearrange("p (i t) -> p i t", t=LS)
                    if en == "act":
                        P.op(ACT, "activation", [r32b], bufs, out_ap, src, AF.Copy)
                    else:
                        P.op(DVE, "tensor_copy", [r32b], bufs, out_ap, src)

            def stage_P(jp):
                qr, qrb = qsets[jp % 2]
                s1 = wload([(0, 512, 16, wv[:, :, jp * 512:(jp + 1) * 512])])
                ws1 = wr[s1][:, :].rearrange("p (c n) -> p c n", n=512)
                for (c0, n) in slabs:
                    for c in range(4):
                        bk, bkb = psring.next()
                        P.mm([(bk[:, 0:n], ws1[:, k, c * 128:(c + 1) * 128], hT[:, k, c0:c0 + n]) for k in range(NCH)],
                             [wrb[s1]] + hbufs, [bkb])
                        rope(bk, bkb, n, c0, [("act", qr[:, c, c0:c0 + n], [qrb[c]], slice(0, n), False)])
                s2 = wload([(0, 128, 16, wv[:, :, 2048 + jp * 128:2048 + (jp + 1) * 128]),
                            (2048, 128, 16, wv[:, :, 2560 + jp * 128:2560 + (jp + 1) * 128])])
                ws2 = wr[s2][:, 0:4096].rearrange("p (g c n) -> p g c n", g=2, c=16)
                for (c0, n) in slabs:
                    bk, bkb = psring.next()
                    P.mm([(bk[:, 0:n], ws2[:, 0, k, :], hT[:, k, c0:c0 + n]) for k in range(NCH)], [wrb[s2]] + hbufs, [bkb])
                    if "sK" in DBG_SKIP:
                        pass
                    elif c0 == 0:
                        rope(bk, bkb, n, c0, [("act", krp[:, jp, WIN:WIN + TP], [krpb[jp]], slice(0, n), False),
                                              ("dve", k32[:, jp, 0:WIN], [k32b], slice(TP - WIN, TP), False)])
                    else:
                        rope(bk, bkb, n, c0, [("act", kcs[:, jp, :, 128:132], [kcsb], slice(0, n), True),
                                              ("dve", k32[:, jp, WIN:WIN + NS], [k32b], slice(0, n), False)])
                    if c0 == 0:
                        for tt in range(4):
                            bt, btb = psring.next()
                            P.mm([(bt[:, 0:128], hT[:, k, tt * 128:(tt + 1) * 128], ws2[:, 1, k, :]) for k in range(NCH)],
                                 [wrb[s2]] + hbufs, [btb])
                            if tt == 3 and last:
                                P.op(ACT, "activation", [btb], [vl32b], vl32[:, jp * 128:(jp + 1) * 128], bt[:, 0:128], AF.Copy)
                                P.op(DVE, "tensor_copy", [vl32b], [vtmb[1 + tt]], vtm[:, 1 + tt, jp * 128:(jp + 1) * 128],
                                     vl32[:, jp * 128:(jp + 1) * 128])
                            else:
                                P.op(ACT, "activation", [btb], [vtmb[1 + tt]], vtm[:, 1 + tt, jp * 128:(jp + 1) * 128], bt[:, 0:128], AF.Copy)
                    else:
                        for i in range(NSEQ_S):
                            bt, btb = psring.next()
                            P.mm([(bt[0:LS, 0:128], hT[:, k, TP + i * LS:TP + (i + 1) * LS], ws2[:, 1, k, :]) for k in range(NCH)],
                                 [wrb[s2]] + hbufs, [btb])
                            vs_, vsb_ = vsst_r.next()
                            P.op(ACT, "activation", [btb], [vsb_], vs_[0:LS, :], bt[0:LS, 0:128], AF.Copy)
                            P.op(DVE, "tensor_copy", [vsb_], [vnsb], vns[0:LS, i, jp * 128:(jp + 1) * 128], vs_[0:LS, :])
                            P.dma(SP, vw_s[b, i, WIN - LS:WIN, jp * 128:(jp + 1) * 128], vs_[0:LS, :], [vsb_], [])

            def softmax_pv(S_ap, sbuf_b, rows, ncols, nsink_col, sink_col, mask_ap):
                sm, smb = sm_r.next()
                P.op(DVE, "reduce_max", [sbuf_b], [smb], sm[0:rows, 0:1], S_ap, AX.X)
                P.op(DVE, "tensor_scalar", [smb, cb], [smb], sm[0:rows, 1:2], sm[0:rows, 0:1], -0.125, nsink_col, ALU.mult, ALU.min)
                pe_, peb = pe_r.next()
                P.op(ACT, "activation", [sbuf_b, smb], [peb], pe_[0:rows, 0:ncols], S_ap, AF.Exp, bias=sm[0:rows, 1:2], scale=0.125)
                P.op(ACT, "activation", [smb, cb], [smb], sm[0:rows, 2:3], sink_col, AF.Exp, bias=sm[0:rows, 1:2], scale=1.0)
                pm, pmb = pm_r.next()
                P.op(DVE, "tensor_tensor", [peb, cb], [pmb], pm[0:rows, 0:ncols], pe_[0:rows, 0:ncols], mask_ap, ALU.mult)
                P.op(DVE, "reduce_sum", [pmb], [smb], sm[0:rows, 3:4], pm[0:rows, 0:ncols], AX.X)
                P.op(DVE, "tensor_tensor", [smb], [smb], sm[0:rows, 4:5], sm[0:rows, 3:4], sm[0:rows, 2:3], ALU.add)
                P.op(DVE, "reciprocal", [smb], [smb], sm[0:rows, 5:6], sm[0:rows, 4:5])
                pn, pnb = pn_r.next()
                P.op(ACT, "activation", [pmb, smb], [pnb], pn[0:rows, 0:ncols], pm[0:rows, 0:ncols], AF.Copy, scale=sm[0:rows, 5:6])
                return pn, pnb

            def stage_A(jp):
                qr, qrb = qsets[jp % 2]
                for c in range(4):
                    bo, bob = accring.next()
                    for offi in range(2):
                        kv = 2 * jp + offi
                        hq = 4 * kv + c
                        psl = slice(offi * 64, offi * 64 + 64)
                        for qb in range(TP // 128):
                            bs, bsb = psring.next()
                            P.mm([(bs[:, 0:256], qr[psl, c, qb * 128:(qb + 1) * 128], krp[psl, jp, qb * 128:qb * 128 + 256], True, True)],
                                 [qrb[c], krpb[jp]], [bsb])
                            mask = mfirst if (p == 0 and qb == 0 and "nomf" not in DBG_SKIP) else mband
                            pn, pnb = softmax_pv(bs[:, 0:256], bsb, 128, 256, nsink_sb[:, b * 32 + hq:b * 32 + hq + 1],
                                                 sink_sb[:, b * 32 + hq:b * 32 + hq + 1], mask[:, :])
                            bt, btb = psring.next()
                            P.mm([(bt[:, kb * 128:(kb + 1) * 128], pn[:, kb * 128:(kb + 1) * 128], ident32[:]) for kb in range(2)],
                                 [pnb, cb], [btb], transpose=True)
                            pT, pTb = pT_r.next()
                            P.op(DVE, "tensor_copy", [btb], [pTb], pT[:, 0:256], bt[:, 0:256])
                            P.mm([(bo[psl, qb * 128:(qb + 1) * 128], vtm[:, qb + kb, jp * 128 + offi * 64:jp * 128 + offi * 64 + 64],
                                   pT[:, kb * 128:(kb + 1) * 128]) for kb in range(2)],
                                 [vtmb[qb], vtmb[qb + 1], pTb], [bob])
                    P.op(ACT, "activation", [bob], [obb[jp * 4 + c]], ob[:, jp * 4 + c, 0:TP], bo[:, 0:TP], AF.Copy)

            def sample_attn():
                pool_sync(P)
                for i in range(NSEQ_S):
                    P.dma(SP, kc32[:, :], ck_in[b, i], [], [kc32b])
                    bt, btb = psring.next()
                    P.mm([(bt[:, j * 128:(j + 1) * 128], kc32[:, j * 128:(j + 1) * 128], ident32[:]) for j in range(4)],
                         [kc32b, cb], [btb], transpose=True)
                    P.op(DVE, "tensor_copy", [btb], [kcsb], kcs[:, :, i, 0:128], bt[:, :].rearrange("p (j n) -> p j n", n=128))
                    P.dma(POOL, vcs[:, i, :], cv_in[b, i], [], [vcsb])
                for jp in range(4):
                    for offi in range(2):
                        kv = 2 * jp + offi
                        psl = slice(offi * 64, offi * 64 + 64)
                        for i in range(NSEQ_S):
                            bs, bsb = psring.next()
                            P.mm([(bs[0:16, 0:132], qs[psl, jp, i, :, :], kcs[psl, jp, i, :], True, True)], [qsb, kcsb], [bsb])
                            pn, pnb = softmax_pv(bs[0:16, 0:132], bsb, 16, 132, nsinks16[:, b * 8 + kv:b * 8 + kv + 1],
                                                 sinks16[:, b * 8 + kv:b * 8 + kv + 1], msamp[:, :])
                            bt, btb = psring.next()
                            P.mm([(bt[:, 0:16], pn[0:16, 0:128], ident32[0:16, 0:16]),
                                  (bt[0:LS, 16:32], pn[0:16, 128:132], ident32[0:16, 0:16])], [pnb, cb], [btb], transpose=True)
                            pT, pTb = pT_r.next()
                            P.op(DVE, "tensor_copy", [btb], [pTb], pT[:, 0:16], bt[:, 0:16])
                            P.op(DVE, "tensor_copy", [btb], [pTb], pT[0:LS, 16:32], bt[0:LS, 16:32])
                            bo, bob = psring.next()
                            P.mm([(bo[psl, 0:16], vcs[:, i, kv * 64:(kv + 1) * 64], pT[:, 0:16]),
                                  (bo[psl, 0:16], vns[0:LS, i, kv * 64:(kv + 1) * 64], pT[0:LS, 16:32])],
                                 [vcsb, vnsb, pTb], [bob])
                            P.op(ACT, "activation", [bob], obb[jp * 4:jp * 4 + 4],
                                 ob[psl, jp * 4:jp * 4 + 4, TP + i * LS:TP + (i + 1) * LS],
                                 bo[psl, 0:16].rearrange("p (c t) -> p c t", t=LS), AF.Copy)
                for i in range(NSEQ_S):
                    P.dma(SP, kw_s[b, i, 0:WIN - LS], ck_in[b, i, LS:WIN], [], [])
                    P.dma(SP, vw_s[b, i, 0:WIN - LS], cv_in[b, i, LS:WIN], [], [])
                    for (src32, srcb, dst) in ((k32, k32b, kw_s),):
                        bt, btb = psring.next()
                        P.mm([(bt[0:LS, j * 128:(j + 1) * 128], src32[:, j, WIN + i * LS:WIN + (i + 1) * LS], ident32[:]) for j in range(4)],
                             [srcb, cb], [btb], transpose=True)
                        P.op(ACT, "activation", [btb], [stgb], stg[0:LS, :], bt[0:LS, :], AF.Copy)
                        P.dma(SP, dst[b, i, WIN - LS:WIN], stg[0:LS, :], [stgb], [])

            def fill_qs(jp):
                qr, qrb = qsets[jp % 2]
                P.op(DVE, "tensor_copy", qrb, [qsb], qs[:, jp].rearrange("p i c t -> p c i t"),
                     qr[:, :, TP:TP + NS].rearrange("p c (i t) -> p c i t", t=LS))

            for i in range(5):
                if i < 4 and "sP" not in DBG_SKIP:
                    stage_P(i)
                    if has_s and "sQ" not in DBG_SKIP:
                        fill_qs(i)
                if i >= 1 and "sA" not in DBG_SKIP:
                    stage_A(i - 1)
            if has_s and "sS" not in DBG_SKIP:
                sample_attn()
            if last and "sC" not in DBG_SKIP:
                P.dma(SP, vw_p[b], vl32[:, :], [vl32b], [])
                for (src32, srcb, dst) in ((k32, k32b, kw_p),):
                    bt, btb = psring.next()
                    P.mm([(bt[:, j * 128:(j + 1) * 128], src32[:, j, 0:WIN], ident32[:]) for j in range(4)],
                         [srcb, cb], [btb], transpose=True)
                    P.op(ACT, "activation", [btb], [stgb], stg[:, :], bt[:, :], AF.Copy)
                    P.dma(SP, dst[b], stg[:, :], [stgb], [])
            if not last:
                P.op(DVE, "tensor_copy", krpb, [kvpb[b]], kprev[b][:, :, :], krp[:, :, TP:TP + WIN])
                P.op(DVE, "tensor_copy", [vtmb[4]], [kvpb[b]], vprev[b][:, :], vtm[:, 4, :])
            out_proj(w_aout[b], slabs, nt)
            post_norm(l, 3, False, slabs, nt, ym, ymb)

        def load_x(p, slabs):
            for (c0, n) in slabs:
                for t0 in range(0, n, 128):
                    r = min(128, n - t0)
                    xs_, xsb = xstring.next()
                    src = xp[p * TP + t0:p * TP + t0 + r, :] if c0 == 0 else xs[0:r, :]
                    P.dma(SP, xs_[0:r, :], src, [], [xsb])
                    for q in range(4):
                        bk, bkb = psring.next()
                        P.mm([(bk[:, k * 128:k * 128 + r], xs_[0:r, (4 * q + k) * 128:(4 * q + k + 1) * 128], ident32[0:r, 0:r]) for k in range(4)],
                             [xsb, cb], [bkb], transpose=True)
                        P.op(ACT, "activation", [bkb], xb[4 * q:4 * q + 4], xT[:, 4 * q:4 * q + 4, c0 + t0:c0 + t0 + r],
                             bk[:, :].rearrange("p (k t) -> p k t", t=128)[:, :, 0:r], AF.Copy)

        def store_x(p, slabs):
            for (c0, n) in slabs:
                for t0 in range(0, n, 128):
                    r = min(128, n - t0)
                    xs_, xsb = xstring.next()
                    for q in range(4):
                        bk, bkb = psring.next()
                        P.mm([(bk[0:r, k * 128:(k + 1) * 128], xT[:, 4 * q + k, c0 + t0:c0 + t0 + r], ident32[:]) for k in range(4)],
                             xb[4 * q:4 * q + 4] + [cb], [bkb], transpose=True)
                        P.op(ACT if q % 2 else DVE, "activation" if q % 2 else "tensor_copy", [bkb], [xsb],
                             xs_[0:r, q * 512:(q + 1) * 512], bk[0:r, :], *([AF.Copy] if q % 2 else []))
                    dst = yp[p * TP + t0:p * TP + t0 + r, :] if c0 == 0 else ys[0:r, :]
                    P.dma(SP, dst, xs_[0:r, :], [xsb], [])

        for p in range(NP):
            slabs = slabs_of(p)
            nt = sum(n for _, n in slabs)
            P.barrier()
            load_x(p, slabs)
            P.barrier()
            for l in range(L):
                if "ffn" not in DBG_SKIP:
                    ffn(l, 0, slabs, nt)
                P.barrier()
                if l % 2 == 0:
                    if "hgrn" not in DBG_SKIP:
                        hgrn(l, p, slabs, nt)
                else:
                    if "swa" not in DBG_SKIP:
                        swa(l, p, slabs, nt)
                P.barrier()
                if "ffn" not in DBG_SKIP:
                    ffn(l, 1, slabs, nt)
            P.barrier()
            store_x(p, slabs)
        P.barrier()
    return nc


def _attn_perm():
    idx = np.empty(2048, np.int64)
    for jp in range(4):
        for c in range(4):
            for off in range(2):
                h = 4 * (2 * jp + off) + c
                base = jp * 512 + c * 128 + off * 64
                idx[base:base + 64] = h * 64 + np.arange(64)
    return idx


_CACHE = {}


def run(cfg, inputs):
    ncores = int(os.environ.get('K_NCORES', '8'))
    key = (cfg.depth, cfg.seq, cfg.dff)
    if key not in _CACHE:
        _CACHE[key] = build(cfg)
    nc = _CACHE[key]
    f = lambda a: np.ascontiguousarray(np.asarray(a, dtype=np.float32))
    L, NA, NB = cfg.depth, cfg.na, cfg.nb
    perm = _attn_perm()
    gains = f(np.asarray(inputs["norm_gains"]).reshape(L, 6, NCH, 128).transpose(3, 0, 1, 2).reshape(128, L * 6 * NCH))
    lbl = f(np.asarray(inputs["hgrn_lb_logits"]).reshape(NA, 16, 128).transpose(2, 0, 1).reshape(128, NA * 16))
    hgn = f(np.asarray(inputs["hgrn_norm_gain"]).reshape(NA, 16, 128).transpose(2, 0, 1).reshape(128, NA * 16))
    if NB:
        wa = np.asarray(inputs["w_attn_in"])
        w_ain = f(np.concatenate([wa[:, :, :2048][:, :, perm], wa[:, :, 2048:]], axis=2))
        w_aout = f(np.asarray(inputs["w_attn_out"])[:, perm, :])
        sinks = f(inputs["attn_sinks"])
        ck = np.asarray(inputs["cache_k_win"]).reshape(NB, cfg.dec_batch, WIN, 512)
        cv = np.asarray(inputs["cache_v_win"]).reshape(NB, cfg.dec_batch, WIN, 512)
    else:
        w_ain = np.zeros((1, D, 3072), np.float32); w_aout = np.zeros((1, D, D), np.float32)
        sinks = np.zeros((1, 32), np.float32)
        ck = np.zeros((1, cfg.dec_batch, WIN, 512), np.float32); cv = ck
    consts = host_consts(cfg)
    shared = {
        "gains": gains,
        "w_fin": f(np.asarray(inputs["w_ffn_in"]).reshape(L, 2, NCH, 128, 2, cfg.nff // 2, 256)
                   .transpose(0, 1, 5, 3, 4, 2, 6).reshape(L, 2, cfg.nff // 2, 128, 8192)),
        "w_fout": f(np.asarray(inputs["w_ffn_out"]).reshape(L, 2, cfg.nff, 128, NCH, 128)
                    .transpose(0, 1, 4, 3, 2, 5).reshape(L, 2, NCH, 128, cfg.nff * 128)),
        "w_hin": f(inputs["w_hgrn_in"]), "lbl": lbl, "hgn": hgn, "w_hout": f(inputs["w_hgrn_out"]),
        "w_ain": w_ain, "sinks_b": sinks, "w_aout": w_aout,
        "sinks16_d": f(np.repeat(sinks.reshape(max(NB, 1), 8, 4).transpose(2, 0, 1).reshape(4, max(NB, 1) * 8), 4, axis=0)),
    }
    for k, v in consts.items():
        shared["c_" + k] = f(v)
    xp_all = np.asarray(inputs["x_prompt"]); xs_all = np.asarray(inputs["x_sample"])
    st_all = np.asarray(inputs["state_hgrn"])
    in_maps = []
    for c in range(ncores):
        m = dict(shared)
        m["xp"] = f(xp_all[c % cfg.batch])
        sl = slice(c * NSEQ_S, (c + 1) * NSEQ_S)
        m["xs"] = f(xs_all[sl].reshape(NS, D))
        m["st_in"] = f(st_all[:, sl])
        m["ck_in"] = f(ck[:, sl]); m["cv_in"] = f(cv[:, sl])
        in_maps.append(m)
    res = run_bass_kernel_spmd(nc, in_maps, core_ids=list(range(ncores)))
    R = res.results
    yp = np.stack([R[b_ % ncores]["yp"] for b_ in range(cfg.batch)])
    ys = np.concatenate([R[c]["ys"].reshape(NSEQ_S, LS, D) for c in range(ncores)], axis=0)
    hgp = np.stack([R[b_ % ncores]["hg_p"] for b_ in range(cfg.batch)], axis=1)
    hgs = np.concatenate([R[c]["hg_s"] for c in range(ncores)], axis=1)
    out = [yp, ys, hgp, hgs]
    kp = np.stack([R[b_ % ncores]["kw_p"][:NB] for b_ in range(cfg.batch)], axis=1).reshape(NB, cfg.batch, WIN, 8, 64)
    vp = np.stack([R[b_ % ncores]["vw_p"][:NB] for b_ in range(cfg.batch)], axis=1).reshape(NB, cfg.batch, WIN, 8, 64)
    ks = np.concatenate([R[c]["kw_s"][:NB] for c in range(ncores)], axis=1).reshape(NB, ncores * NSEQ_S, WIN, 8, 64)
    vs = np.concatenate([R[c]["vw_s"][:NB] for c in range(ncores)], axis=1).reshape(NB, ncores * NSEQ_S, WIN, 8, 64)
    out += [kp, vp, ks, vs]
    return tuple(np.ascontiguousarray(o, dtype=np.float32) for o in out)


def kernel(**inputs):
    return run(Cfg(), inputs)
```

```python
import contextlib
import numpy as np
import concourse.bass as bass
import concourse.mybir as mybir
from concourse.bass_utils import run_bass_kernel_spmd

F32 = mybir.dt.float32
BF16 = mybir.dt.bfloat16
AF = mybir.ActivationFunctionType
ALU = mybir.AluOpType
AX = mybir.AxisListType

D = 2048
NCH = 16
TP = 512
NSEQ_S = 4
LS = 4
NS = NSEQ_S * LS
NTMAX = TP + NS
WIN = 128
PAST = 16384
EPS = 1e-6
HC = 64
import os
DBG_SKIP = os.environ.get('K_SKIP', '').split(',')
WORKW = 17600


class Cfg:
    def __init__(self, depth=4, seq=2048, dff=5632, batch=4, dec_batch=32):
        self.depth, self.seq, self.dff, self.batch, self.dec_batch = depth, seq, dff, batch, dec_batch
        self.npass = seq // TP
        self.nff = dff // 128
        self.na = (depth + 1) // 2
        self.nb = depth // 2


class Buf:
    __slots__ = ("w", "r")

    def __init__(self):
        self.w = None
        self.r = {}


class Eng:
    def __init__(self, h, sem, is_pe=False):
        self.h, self.sem, self.cnt, self.waited, self.is_pe = h, sem, 0, {}, is_pe

    def wait(self, dep):
        sem, val = dep
        if sem is self.sem and self.is_pe:
            return
        k = id(sem)
        if self.waited.get(k, 0) >= val:
            return
        self.waited[k] = val
        self.h.wait_ge(sem, val)


class Prog:
    def __init__(self, nc, es):
        self.nc, self.es = nc, es
        sm = lambda n: es.enter_context(nc.semaphore(n))
        self.pe = Eng(nc.tensor, sm("s_pe"), True)
        self.act = Eng(nc.scalar, sm("s_act"))
        self.dve = Eng(nc.vector, sm("s_dve"))
        self.pool = Eng(nc.gpsimd, sm("s_pool"))
        self.sp = Eng(nc.sync, sm("s_sp"))
        self.dsems = [[sm("s_d%d" % i), 0] for i in range(12)]
        self.dnext = 0
        self.psems = [[sm("s_q%d" % i), 0] for i in range(2)]
        self.pnext = 0

    def _deps(self, eng, reads, writes):
        for b in reads:
            if b.w is not None:
                eng.wait(b.w)
        for b in writes:
            if b.w is not None:
                eng.wait(b.w)
            for d in b.r.values():
                eng.wait(d)

    def _mark(self, dep, reads, writes):
        for b in reads:
            b.r[id(dep[0])] = dep
        for b in writes:
            b.w = dep
            b.r = {}

    def op(self, eng, name, reads, writes, *a, **k):
        self._deps(eng, reads, writes)
        ins = getattr(eng.h, name)(*a, **k)
        eng.cnt += 1
        ins.then_inc(eng.sem, 1)
        self._mark((eng.sem, eng.cnt), reads, writes)

    def mm(self, mms, reads, writes, transpose=False):
        eng = self.pe
        self._deps(eng, reads, writes)
        n = len(mms)
        ins = None
        for i, m in enumerate(mms):
            if transpose:
                ins = eng.h.transpose(m[0], m[1], m[2])
            else:
                st = m[3] if len(m) > 3 else (i == 0)
                sp = m[4] if len(m) > 4 else (i == n - 1)
                ins = eng.h.matmul(m[0], m[1], m[2], start=st, stop=sp)
        eng.cnt += 1
        ins.then_inc(eng.sem, 1)
        self._mark((eng.sem, eng.cnt), reads, writes)

    def dma(self, eng, out, in_, reads, writes, semslot=None, **k):
        if semslot is None and eng is self.pool:
            semslot = self.psems[self.pnext]
            self.pnext = (self.pnext + 1) % len(self.psems)
        elif semslot is None:
            semslot = self.dsems[self.dnext]
            self.dnext = (self.dnext + 1) % len(self.dsems)
        sem = semslot[0]
        if semslot[1] > 0:
            eng.wait((sem, semslot[1]))
        self._deps(eng, reads, writes)
        eng.h.dma_start(out=out, in_=in_, **k).then_inc(sem, 16)
        semslot[1] += 16
        self._mark((sem, semslot[1]), reads, writes)

    def barrier(self, waiters=None):
        engs = [self.pe, self.act, self.dve, self.pool, self.sp]
        for e in (waiters or [self.pe, self.act, self.dve, self.sp]):
            for f in engs:
                if f is not e and f.cnt > 0:
                    e.wait((f.sem, f.cnt))
            for s in self.dsems + self.psems:
                if s[1] > 0:
                    e.wait((s[0], s[1]))


def pool_sync(P):
    P.barrier(waiters=[P.pool])


class Ring:
    def __init__(self, items):
        self.items, self.i = items, 0

    def next(self):
        it = self.items[self.i]
        self.i = (self.i + 1) % len(self.items)
        return it


def host_consts(cfg):
    c = {}
    c["ident32"] = np.eye(128, dtype=np.float32)
    c["ones32"] = np.ones((128, 128), np.float32)
    R = np.zeros((128, 128), np.float32)
    for blk in range(2):
        for j in range(32):
            R[blk * 64 + j + 32, blk * 64 + j] = -1.0
            R[blk * 64 + j, blk * 64 + j + 32] = 1.0
    c["rotm"] = R
    half = 32
    inv = np.power(np.float32(10000.0), -(np.arange(half, dtype=np.float32) * np.float32(2.0 / 64))).astype(np.float32)
    pos = np.concatenate([np.arange(cfg.seq, dtype=np.float32),
                          (PAST + np.arange(LS)).astype(np.float32)])
    ang = (pos[None, :] * inv[:, None]).astype(np.float32)
    cos = np.cos(ang.astype(np.float64)).astype(np.float32)
    sin = np.sin(ang.astype(np.float64)).astype(np.float32)
    cosT = np.tile(cos, (4, 1))
    sinT = np.tile(sin, (4, 1))
    c["cosT"] = np.concatenate([cosT[:, :cfg.seq]] + [cosT[:, cfg.seq:]] * NSEQ_S, axis=1).copy()
    c["sinT"] = np.concatenate([sinT[:, :cfg.seq]] + [sinT[:, cfg.seq:]] * NSEQ_S, axis=1).copy()
    rm = np.ones((128, NTMAX), np.float32)
    rm[:, 0:TP:HC] = 0.0
    rm[:, TP::LS] = 0.0
    c["rmask"] = rm
    c["cm64"] = np.triu(np.ones((HC, HC), np.float32))
    cmd = np.triu(np.ones((HC, HC), np.float32)); cmd[0:HC // 2, HC // 2:] = 0.0
    c["cmd64"] = cmd
    t = np.arange(128)[:, None]
    s = np.arange(256)[None, :]
    band = ((s >= t) & (s <= t + 128)).astype(np.float32)
    first = band * (s >= 128)
    c["mband"] = band
    c["mfirst"] = first.astype(np.float32)
    tt = (np.arange(16) % 4)[:, None]
    j = np.arange(132)[None, :]
    c["msamp"] = ((j >= tt) & (j <= 128 + tt)).astype(np.float32)
    return c


CONST_SHAPES = {
    "ident32": [128, 128], "ones32": [128, 128], "rotm": [128, 128],
    "rmask": [128, NTMAX], "cm64": [HC, HC], "cmd64": [HC, HC], "mband": [128, 256], "mfirst": [128, 256],
    "msamp": [16, 132],
}


def build(cfg):
    nc = bass.Bass("TRN2", target_bir_lowering=False)
    NP, NFF, NA, NB, DFF = cfg.npass, cfg.nff, cfg.na, cfg.nb, cfg.dff
    L = cfg.depth

    def din(name, shape):
        return nc.dram_tensor(name, list(shape), F32, kind="ExternalInput").ap()

    def dout(name, shape):
        return nc.dram_tensor(name, list(shape), F32, kind="ExternalOutput").ap()

    xp = din("xp", [cfg.seq, D])
    xs = din("xs", [NS, D])
    st_in = din("st_in", [NA, NSEQ_S, 16, 128, 128])
    ck_in = din("ck_in", [max(NB, 1), NSEQ_S, WIN, 512])
    cv_in = din("cv_in", [max(NB, 1), NSEQ_S, WIN, 512])
    gains = din("gains", [128, L * 6 * NCH])
    w_fin = din("w_fin", [L, 2, NFF // 2, 128, 8192])
    w_fout = din("w_fout", [L, 2, NCH, 128, NFF * 128])
    w_hin = din("w_hin", [NA, D, 8192])
    lbl = din("lbl", [128, NA * 16])
    hgn = din("hgn", [128, NA * 16])
    w_hout = din("w_hout", [NA, D, D])
    w_ain = din("w_ain", [max(NB, 1), D, 3072])
    sinks_b = din("sinks_b", [max(NB, 1), 32])
    w_aout = din("w_aout", [max(NB, 1), D, D])
    sinks16_d = din("sinks16_d", [16, max(NB, 1) * 8])
    cst = {k: din("c_" + k, v) for k, v in CONST_SHAPES.items()}
    cosT_d = din("c_cosT", [128, cfg.seq + NS])
    sinT_d = din("c_sinT", [128, cfg.seq + NS])

    yp = dout("yp", [cfg.seq, D])
    ys = dout("ys", [NS, D])
    hg_p = dout("hg_p", [NA, 16, 128, 128])
    hg_s = dout("hg_s", [NA, NSEQ_S, 16, 128, 128])
    kw_p = dout("kw_p", [max(NB, 1), WIN, 512])
    vw_p = dout("vw_p", [max(NB, 1), WIN, 512])
    kw_s = dout("kw_s", [max(NB, 1), NSEQ_S, WIN, 512])
    vw_s = dout("vw_s", [max(NB, 1), NSEQ_S, WIN, 512])

    with contextlib.ExitStack() as es:
        P = Prog(nc, es)
        PE, ACT, DVE, POOL, SP = P.pe, P.act, P.dve, P.pool, P.sp

        def sb(name, shape, dt=F32):
            return es.enter_context(nc.sbuf_tensor(name, list(shape), dt))

        xT = sb("xT", [128, NCH, NTMAX])
        xb = [Buf() for _ in range(NCH)]
        yT = sb("yT", [128, NCH, NTMAX])
        yb = [Buf() for _ in range(NCH)]
        hT = yT[:, 0:NCH // 2, :].bitcast(BF16).rearrange("p a (b n) -> p (a b) n", b=2)
        hbufs = yb[0:NCH // 2]
        work = sb("work", [128, WORKW])
        ob = yT[:, NCH // 2:NCH, :].bitcast(BF16).rearrange("p a (b n) -> p (a b) n", b=2)
        obb = [yb[NCH // 2 + c // 2] for c in range(NCH)]
        ym = work[:, 0:NCH * NTMAX].rearrange("p (c n) -> p c n", n=NTMAX)
        ymb = [Buf() for _ in range(NCH)]
        rstd = sb("rstd", [128, NTMAX])
        rstdb = Buf()
        wr = [sb("wr%d" % i, [128, 8192], BF16) for i in range(2)]
        wrb = [Buf() for _ in range(2)]
        wsem = [[es.enter_context(nc.semaphore("s_w%d" % i)), 0] for i in range(2)]
        wring = Ring(list(range(2)))
        sqr = [sb("sq%d" % i, [128, NTMAX]) for i in range(2)]
        sqring = Ring([(sqr[i], Buf()) for i in range(2)])
        tmpr = [sb("tmp%d" % i, [128, 512]) for i in range(3)]
        tmpring = Ring([(tmpr[i], Buf()) for i in range(3)])
        g_sb = sb("g_sb", [128, L * 6 * NCH])
        gh_sb = sb("gh_sb", [128, L * 6 * NCH])
        gb = Buf()
        ident32 = sb("ident32", [128, 128]); ident16 = sb("ident16", [128, 128], BF16)
        ones32 = sb("ones32", [128, 128]); rotm = sb("rotm", [128, 128]); ones16 = sb("ones16", [128, 128], BF16)
        rmask = sb("rmask", [128, NTMAX]); cm64 = sb("cm64", [HC, HC]); cmd64 = sb("cmd64", [HC, HC])
        mband = sb("mband", [128, 256]); mfirst = sb("mfirst", [128, 256]); msamp = sb("msamp", [16, 132])
        cb = Buf()
        lb_sb = sb("lb_sb", [128, NA * 16]); oml_sb = sb("oml_sb", [128, NA * 16]); hgn_sb = sb("hgn_sb", [128, NA * 16])
        sscr = nc.dram_tensor("sscr", [NA, 16, 128, 128], F32).ap()
        if NB:
            kprev = [sb("kprev%d" % b, [128, 4, WIN], BF16) for b in range(NB)]
            vprev = [sb("vprev%d" % b, [128, 512], BF16) for b in range(NB)]
            kvpb = [Buf() for _ in range(NB)]
            sink_sb = sb("sink_sb", [128, NB * 32]); nsink_sb = sb("nsink_sb", [128, NB * 32])
            sinks16 = sb("sinks16", [16, NB * 8]); nsinks16 = sb("nsinks16", [16, NB * 8])
            cosT = sb("cosT", [128, NTMAX]); sinT = sb("sinT", [128, NTMAX])
            ropeb = Buf()
        yflat = yT[:, :, :].rearrange("p c n -> p (c n)")
        xstring = Ring([(yflat[:, i * D:(i + 1) * D], Buf()) for i in range(2)])

        ps = es.enter_context(nc.psum_tensor("ps", [128, 8, 512], F32))
        psring = Ring([(ps[:, i, :], Buf()) for i in range(6)])
        accring = Ring([(ps[:, i, :], Buf()) for i in range(6, 8)])

        def wcols(off, n):
            return slice(off, off + n)

        def ld(dst, src, bufs):
            P.dma(SP, dst, src, [], bufs)

        ld(g_sb[:], gains, [gb])
        for nm, t_ in (("ident32", ident32), ("ones32", ones32), ("rotm", rotm), ("rmask", rmask),
                       ("cm64", cm64), ("cmd64", cmd64), ("mband", mband), ("mfirst", mfirst), ("msamp", msamp)):
            ld(t_[:], cst[nm], [cb])
        ld(lb_sb[:], lbl, [cb])
        ld(hgn_sb[:], hgn, [cb])
        P.op(DVE, "tensor_copy", [cb], [cb], ident16[:], ident32[:])
        P.op(DVE, "tensor_copy", [cb], [cb], ones16[:], ones32[:])
        P.op(DVE, "tensor_scalar", [gb], [gb], gh_sb[:], g_sb[:], 0.5, None, ALU.mult)
        P.op(DVE, "memset", [], [cb], oml_sb[:], 1.0)
        if NA > 1:
            assert NA == 2
            P.op(DVE, "tensor_tensor", [cb], [cb], oml_sb[:, 16:32], lb_sb[:, 16:32], lb_sb[:, 0:16], ALU.subtract)
            P.op(ACT, "activation", [cb], [cb], lb_sb[:, 16:32], oml_sb[:, 16:32], AF.Sigmoid)
            P.op(DVE, "tensor_scalar", [cb], [cb], oml_sb[:, 16:32], lb_sb[:, 16:32], -1.0, 1.0, ALU.mult, ALU.add)
        P.op(DVE, "memset", [cb], [cb], lb_sb[:, 0:16], 0.0)
        for b in range(NB):
            P.op(DVE, "memset", [], [kvpb[b]], kprev[b][:], 0.0)
            P.op(DVE, "memset", [], [kvpb[b]], vprev[b][:], 0.0)
            ld(sink_sb[:, b * 32:(b + 1) * 32], sinks_b[b:b + 1, :].partition_broadcast(128), [cb])
        if NB:
            ld(sinks16[:], sinks16_d, [cb])
            P.op(DVE, "tensor_scalar", [cb], [cb], nsink_sb[:], sink_sb[:], -1.0, None, ALU.mult)
            P.op(DVE, "tensor_scalar", [cb], [cb], nsinks16[:], sinks16[:], -1.0, None, ALU.mult)

        def gcol(tbl, l, n, c):
            i = (l * 6 + n) * NCH + c
            return tbl[:, i:i + 1]

        def wload(parts):
            s = wring.next()
            for (off, n, kc, src) in parts:
                dst = wr[s][:, off:off + kc * n].rearrange("p (k n) -> p k n", n=n)
                P.dma(POOL, dst, src, [], [wrb[s]], semslot=wsem[s])
            return s

        def wload_flat(src2d, ncols):
            s = wring.next()
            b = max(d for d in range(1, 2049) if ncols % d == 0)
            P.dma(POOL, wr[s][:, 0:ncols].rearrange("p (a b) -> p a b", b=b), src2d.rearrange("p (a b) -> p a b", b=b),
                  [], [wrb[s]], semslot=wsem[s])
            return s

        def slabs_of(p):
            return [(0, TP)] + ([(TP, NS)] if p == 0 else [])

        def compute_rstd(src, sbufs, slabs, nt):
            for (c0, n) in slabs:
                bank, bb = psring.next()
                for c in range(NCH):
                    sq, sqb = sqring.next()
                    sqv = sq[:, :].bitcast(BF16)
                    P.op(ACT, "activation", [sbufs[c]], [sqb], sqv[:, 0:n], src[:, c, c0:c0 + n], AF.Square)
                    P.mm([(bank[:, 0:n], ones16[:], sqv[:, 0:n], c == 0, c == NCH - 1)], [sqb, cb], [bb])
                t_, tb = tmpring.next()
                P.op(ACT, "activation", [bb], [tb], t_[:, 0:n], bank[:, 0:n], AF.Sqrt, bias=EPS, scale=1.0 / D)
                P.op(DVE, "reciprocal", [tb], [rstdb], rstd[:, c0:c0 + n], t_[:, 0:n])

        def pre_norm(l, n_, slabs, nt):
            compute_rstd(xT, xb, slabs, nt)
            for c in range(NCH):
                P.op(DVE, "scalar_tensor_tensor", [xb[c], rstdb, gb], [hbufs[c // 2]],
                     hT[:, c, 0:nt], xT[:, c, 0:nt], gcol(g_sb, l, n_, c), rstd[:, 0:nt], ALU.mult, ALU.mult)

        def post_norm(l, n_, half, slabs, nt, src=None, srcb=None):
            if src is None:
                src, srcb = yT, yb
            compute_rstd(src, srcb, slabs, nt)
            tbl = gh_sb if half else g_sb
            for c in range(NCH):
                P.op(DVE, "scalar_tensor_tensor", [rstdb, gb], [srcb[c]],
                     src[:, c, 0:nt], src[:, c, 0:nt], gcol(tbl, l, n_, c), rstd[:, 0:nt], ALU.mult, ALU.mult)
                P.op(POOL, "tensor_tensor", [srcb[c]], [xb[c]], xT[:, c, 0:nt], xT[:, c, 0:nt], src[:, c, 0:nt], ALU.add)

        actT = work[:, 0:NFF * NTMAX // 2].bitcast(BF16).rearrange("p (j n) -> p j n", n=NTMAX)
        ab = [Buf() for _ in range(NFF)]

        def ffn(l, i, slabs, nt):
            pre_norm(l, 0 if i == 0 else 4, slabs, nt)
            for fb in range(NFF // 2):
                s = wload_flat(w_fin[l, i, fb], 8192)
                ws = wr[s][:, :].rearrange("p (g c n) -> p g c n", g=2, c=16)
                for j in range(2):
                    J = 2 * fb + j
                    for (c0, n) in slabs:
                        bg, bgb = psring.next()
                        P.mm([(bg[:, 0:n], ws[:, 0, c, j * 128:(j + 1) * 128], hT[:, c, c0:c0 + n]) for c in range(NCH)],
                             [wrb[s]] + hbufs, [bgb])
                        bu, bub = psring.next()
                        P.mm([(bu[:, 0:n], ws[:, 1, c, j * 128:(j + 1) * 128], hT[:, c, c0:c0 + n]) for c in range(NCH)],
                             [wrb[s]] + hbufs, [bub])
                        t_, tb = tmpring.next()
                        P.op(ACT, "activation", [bgb], [tb], t_[:, 0:n], bg[:, 0:n], AF.Silu)
                        P.op(DVE, "tensor_tensor", [tb, bub], [ab[J]], actT[:, J, c0:c0 + n], t_[:, 0:n], bu[:, 0:n], ALU.mult)
            for dc in range(NCH):
                s = wload_flat(w_fout[l, i, dc], NFF * 128)
                ws = wr[s][:, 0:NFF * 128].rearrange("p (j n) -> p j n", n=128)
                for (c0, n) in slabs:
                    bk, bkb = psring.next()
                    P.mm([(bk[:, 0:n], ws[:, j, :], actT[:, j, c0:c0 + n]) for j in range(NFF)], [wrb[s]] + ab, [bkb])
                    P.op(ACT, "activation", [bkb], [yb[dc]], yT[:, dc, c0:c0 + n], bk[:, 0:n], AF.Copy)
            post_norm(l, 1 if i == 0 else 5, True, slabs, nt)

        def out_proj(wmat, slabs, nt):
            P.barrier()
            wv = wmat.rearrange("(c p) n -> p c n", p=128)
            for dcb in range(4):
                s = wload([(0, 512, 16, wv[:, :, dcb * 512:(dcb + 1) * 512])])
                ws = wr[s][:, :].rearrange("p (c n) -> p c n", n=512)
                for k in range(4):
                    dc = dcb * 4 + k
                    for (c0, n) in slabs:
                        bk, bkb = psring.next()
                        P.mm([(bk[:, 0:n], ws[:, c, k * 128:(k + 1) * 128], ob[:, c, c0:c0 + n]) for c in range(NCH)],
                             [wrb[s]] + yb[NCH // 2:], [bkb])
                        P.op(ACT, "activation", [bkb], [ymb[dc]], ym[:, dc, c0:c0 + n], bk[:, 0:n], AF.Copy)

        def carver():
            off = [0]

            def carve(nwords, dt=F32):
                v = work[:, off[0]:off[0] + nwords]
                off[0] += nwords
                assert off[0] <= WORKW, off[0]
                if dt == BF16:
                    v = v.bitcast(BF16)
                return v
            return carve

        def hgrn(l, p, slabs, nt):
            a = l // 2
            last = (p == NP - 1)
            has_s = (p == 0)
            pre_norm(l, 2, slabs, nt)
            carve = carver()
            sets = []
            for _ in range(2):
                d_ = {}
                for nm in ("qf", "sg", "kk", "bb", "dd", "dl", "sgl"):
                    d_[nm] = (carve(NTMAX), Buf())
                for nm in ("vbf", "qt", "kt", "qh", "kh", "pxk", "pxq"):
                    d_[nm] = (carve(NTMAX // 2, BF16), Buf())
                d_["At"] = (carve(256, BF16), Buf())
                d_["Ats"] = (carve(8, BF16), Buf())
                d_["khT"] = (carve(512, BF16), Buf())
                d_["vT"] = (carve(512, BF16), Buf())
                d_["khTs"] = (carve(256, BF16), Buf())
                d_["vTs"] = (carve(256, BF16), Buf())
                d_["Sbf"] = (carve(512, BF16), Buf())
                d_["Ss"] = (carve(512), Buf())
                d_["Sbs"] = (carve(256, BF16), Buf())
                d_["Sw"] = (carve(128), Buf())
                P.op(DVE, "memset", [], [d_["pxk"][1]], d_["pxk"][0][:, :], 0.0)
                P.op(DVE, "memset", [], [d_["pxq"][1]], d_["pxq"][0][:, :], 0.0)
                sets.append(d_)
            wv = w_hin[a].rearrange("(c p) n -> p c n", p=128)
            npc = TP // HC

            def stage_P(h):
                S_ = sets[h % 2]
                s = wload([(k * 2048, 128, 16, wv[:, :, k * 2048 + h * 128:k * 2048 + (h + 1) * 128]) for k in range(4)])
                ws = wr[s][:, :].rearrange("p (k c n) -> p k c n", k=4, c=16)
                for (c0, n) in slabs:
                    banks = []
                    for k in range(4):
                        bk, bkb = psring.next()
                        P.mm([(bk[:, 0:n], ws[:, k, c, :], hT[:, c, c0:c0 + n]) for c in range(NCH)], [wrb[s]] + hbufs, [bkb])
                        banks.append((bk, bkb))
                    sl = slice(c0, c0 + n)
                    P.op(ACT, "activation", [banks[0][1]], [S_["qf"][1]], S_["qf"][0][:, sl], banks[0][0][:, 0:n], AF.Copy)
                    P.op(ACT, "activation", [banks[1][1]], [S_["sg"][1]], S_["sg"][0][:, sl], banks[1][0][:, 0:n], AF.Sigmoid)
                    P.op(DVE, "tensor_copy", [banks[2][1]], [S_["vbf"][1]], S_["vbf"][0][:, sl], banks[2][0][:, 0:n])
                    P.op(ACT, "activation", [banks[3][1]], [S_["sgl"][1]], S_["sgl"][0][:, sl], banks[3][0][:, 0:n], AF.Silu)
                if p == 0:
                    P.op(DVE, "memset", [], [S_["Sw"][1]], S_["Sw"][0], 0.0)
                else:
                    P.dma(SP, S_["Sw"][0], sscr[a, h], [], [S_["Sw"][1]])

            def cview(t_, n_chunks, csz, base=0):
                return t_[:, base:base + n_chunks * csz].rearrange("p (c t) -> p c t", t=csz)

            def stage_E(h):
                S_ = sets[h % 2]
                g = lambda nm: S_[nm][0]
                B = lambda nm: S_[nm][1]
                hi = a * 16 + h
                N_ = slice(0, nt)
                P.op(DVE, "tensor_scalar", [B("sg"), cb], [B("sg")], g("sg")[:, N_], g("sg")[:, N_],
                     oml_sb[:, hi:hi + 1], lb_sb[:, hi:hi + 1], ALU.mult, ALU.add)
                P.op(DVE, "tensor_scalar", [B("sg")], [B("kk")], g("kk")[:, N_], g("sg")[:, N_], -1.0, 1.0, ALU.mult, ALU.add)
                P.op(ACT, "activation", [B("sg")], [B("sg")], g("sg")[:, N_], g("sg")[:, N_], AF.Ln)
                P.op(DVE, "tensor_tensor_scan", [B("sg"), cb], [B("bb")], g("bb")[:, N_], rmask[:, N_], g("sg")[:, N_], 0.0, ALU.mult, ALU.add)
                HH = HC // 2
                bvc = cview(g("bb"), npc, HC, 0)
                dlv = cview(g("dl"), npc, HC, 0)
                b31 = bvc[:, :, HH - 1:HH].to_broadcast([128, npc, HH])
                P.op(DVE, "tensor_tensor", [B("bb")], [B("dl")], dlv[:, :, 0:HH], b31, bvc[:, :, 0:HH], ALU.subtract)
                P.op(DVE, "tensor_tensor", [B("bb")], [B("dl")], dlv[:, :, HH:HC], bvc[:, :, HH:HC], b31, ALU.subtract)
                P.op(ACT, "activation", [B("dl")], [B("dl")], g("dl")[:, 0:TP], g("dl")[:, 0:TP], AF.Exp)
                P.op(DVE, "tensor_tensor", [B("kk"), B("dl")], [B("pxk")], cview(g("pxk"), npc, HC, 0)[:, :, 0:HH],
                     cview(g("kk"), npc, HC, 0)[:, :, 0:HH], cview(g("dl"), npc, HC, 0)[:, :, 0:HH], ALU.mult)
                P.op(DVE, "tensor_tensor", [B("qf"), B("dl")], [B("pxq")], cview(g("pxq"), npc, HC, 0)[:, :, HH:HC],
                     cview(g("qf"), npc, HC, 0)[:, :, HH:HC], cview(g("dl"), npc, HC, 0)[:, :, HH:HC], ALU.mult)
                parts = [(2 * npc, HH, 0)] + ([(NSEQ_S, LS, TP)] if has_s else [])
                for (ncx, csz, base) in parts:
                    bv = cview(g("bb"), ncx, csz, base)
                    mid = csz // 2 - 1
                    P.op(DVE, "tensor_tensor", [B("bb")], [B("dd")], cview(g("dd"), ncx, csz, base), bv,
                         bv[:, :, mid:mid + 1].to_broadcast([128, ncx, csz]), ALU.subtract)
                parts = [(npc, HC, 0)] + ([(NSEQ_S, LS, TP)] if has_s else [])
                for (ncx, csz, base) in parts:
                    bv = cview(g("bb"), ncx, csz, base)
                    P.op(DVE, "tensor_tensor", [B("bb")], [B("dl")], cview(g("dl"), ncx, csz, base),
                         bv[:, :, csz - 1:csz].to_broadcast([128, ncx, csz]), bv, ALU.subtract)
                P.op(ACT, "activation", [B("dd")], [B("dd")], g("dd")[:, N_], g("dd")[:, N_], AF.Exp)
                P.op(DVE, "tensor_tensor", [B("qf"), B("dd")], [B("qt")], g("qt")[:, N_], g("qf")[:, N_], g("dd")[:, N_], ALU.mult)
                P.op(DVE, "reciprocal", [B("dd")], [B("dd")], g("dd")[:, N_], g("dd")[:, N_])
                P.op(DVE, "tensor_tensor", [B("kk"), B("dd")], [B("kt")], g("kt")[:, N_], g("kk")[:, N_], g("dd")[:, N_], ALU.mult)
                P.op(ACT, "activation", [B("bb")], [B("bb")], g("bb")[:, N_], g("bb")[:, N_], AF.Exp)
                P.op(DVE, "tensor_tensor", [B("qf"), B("bb")], [B("qh")], g("qh")[:, N_], g("qf")[:, N_], g("bb")[:, N_], ALU.mult)
                P.op(ACT, "activation", [B("dl")], [B("dl")], g("dl")[:, N_], g("dl")[:, N_], AF.Exp)
                P.op(DVE, "tensor_tensor", [B("kk"), B("dl")], [B("kh")], g("kh")[:, N_], g("kk")[:, N_], g("dl")[:, N_], ALU.mult)

            def stage_T(h):
                S_ = sets[h % 2]
                g = lambda nm: S_[nm][0]
                B = lambda nm: S_[nm][1]
                bk, bkb = psring.next()
                P.mm([(bk[0:HC, ci * HC:(ci + 1) * HC], g("kt")[:, ci * HC:(ci + 1) * HC], g("qt")[:, ci * HC:(ci + 1) * HC], True, True)
                      for ci in range(npc)], [B("kt"), B("qt")], [bkb])
                bkx, bkxb = psring.next()
                P.mm([(bkx[0:HC, ci * HC:(ci + 1) * HC], g("pxk")[:, ci * HC:(ci + 1) * HC], g("pxq")[:, ci * HC:(ci + 1) * HC], True, True)
                      for ci in range(npc)], [B("pxk"), B("pxq")], [bkxb])
                At = g("At")[0:HC, :].rearrange("p (c t) -> p c t", t=HC)
                tmpA = g("dd")[0:HC, 0:TP].rearrange("p (c t) -> p c t", t=HC)
                P.op(DVE, "tensor_tensor", [bkb, cb], [B("dd")], tmpA, bk[0:HC, :].rearrange("p (c t) -> p c t", t=HC),
                     cmd64[:, :].unsqueeze(1).to_broadcast([HC, npc, HC]), ALU.mult)
                P.op(DVE, "tensor_tensor", [B("dd"), bkxb], [B("At")], At, tmpA,
                     bkx[0:HC, :].rearrange("p (c t) -> p c t", t=HC), ALU.add)
                bk2, bk2b = psring.next()
                k16 = bk2.bitcast(BF16)
                P.mm([(k16[0:HC, ci * 128:(ci + 1) * 128], g("kh")[:, ci * HC:(ci + 1) * HC], ident16[:]) for ci in range(npc)],
                     [B("kh"), cb], [bk2b], transpose=True)
                P.op(ACT, "activation", [bk2b], [B("khT")], g("khT")[0:HC, :], k16[0:HC, :], AF.Copy)
                bk3, bk3b = psring.next()
                v16 = bk3.bitcast(BF16)
                P.mm([(v16[0:HC, ci * 128:(ci + 1) * 128], g("vbf")[:, ci * HC:(ci + 1) * HC], ident16[:]) for ci in range(npc)],
                     [B("vbf"), cb], [bk3b], transpose=True)
                P.op(DVE, "tensor_copy", [bk3b], [B("vT")], g("vT")[0:HC, :], v16[0:HC, :])
                if has_s:
                    bk4, bk4b = psring.next()
                    P.mm([(bk4[0:LS, i * LS:(i + 1) * LS], g("kt")[:, TP + i * LS:TP + (i + 1) * LS], g("qt")[:, TP + i * LS:TP + (i + 1) * LS], True, True)
                          for i in range(NSEQ_S)], [B("kt"), B("qt")], [bk4b])
                    P.op(DVE, "tensor_tensor", [bk4b, cb], [B("Ats")], g("Ats")[0:LS, :].rearrange("p (c t) -> p c t", t=LS),
                         bk4[0:LS, 0:NS].rearrange("p (c t) -> p c t", t=LS),
                         cm64[0:LS, 0:LS].unsqueeze(1).to_broadcast([LS, NSEQ_S, LS]), ALU.mult)
                    bk5, bk5b = psring.next()
                    s16 = bk5.bitcast(BF16)
                    P.mm([(s16[0:LS, i * 128:(i + 1) * 128], g("kh")[:, TP + i * LS:TP + (i + 1) * LS], ident16[:]) for i in range(NSEQ_S)]
                         + [(s16[0:LS, 512 + i * 128:512 + (i + 1) * 128], g("vbf")[:, TP + i * LS:TP + (i + 1) * LS], ident16[:]) for i in range(NSEQ_S)],
                         [B("kh"), B("vbf"), cb], [bk5b], transpose=True)
                    P.op(ACT, "activation", [bk5b], [B("khTs")], g("khTs")[0:LS, :], s16[0:LS, 0:512], AF.Copy)
                    P.op(ACT, "activation", [bk5b], [B("vTs")], g("vTs")[0:LS, :], s16[0:LS, 512:1024], AF.Copy)

            def stage_U(h):
                S_ = sets[h % 2]
                g = lambda nm: S_[nm][0]
                B = lambda nm: S_[nm][1]
                khT = g("khT")[0:HC, :].rearrange("p (c k) -> p c k", k=128)
                vT = g("vT")[0:HC, :].rearrange("p (c k) -> p c k", k=128)
                Sbf = g("Sbf").rearrange("p (c k) -> p c k", k=128)
                S = g("Sw")
                for half in range(npc // 4):
                    bk, bkb = psring.next()
                    P.mm([(bk[:, k * 128:(k + 1) * 128], khT[:, half * 4 + k, :], vT[:, half * 4 + k, :], True, True) for k in range(4)],
                         [B("khT"), B("vT")], [bkb])
                    for k in range(4):
                        ci = half * 4 + k
                        P.op(DVE, "tensor_copy", [B("Sw")], [B("Sbf")], Sbf[:, ci, :], S)
                        P.op(DVE, "scalar_tensor_tensor", [B("bb"), bkb], [B("Sw")], S, S,
                             g("bb")[:, ci * HC + HC - 1:ci * HC + HC], bk[:, k * 128:(k + 1) * 128], ALU.mult, ALU.add)
                if last:
                    P.dma(SP, hg_p[a, h], S, [B("Sw")], [])
                else:
                    P.dma(SP, sscr[a, h], S, [B("Sw")], [])
                if has_s:
                    Ss = g("Ss").rearrange("p (i k) -> p i k", k=128)
                    P.dma(SP, Ss, st_in[a, :, h].rearrange("i k v -> k i v"), [], [B("Ss")])
                    P.op(DVE, "tensor_copy", [B("Ss")], [B("Sbs")], g("Sbs"), g("Ss"))
                    bk, bkb = psring.next()
                    khTs = g("khTs")[0:LS, :].rearrange("p (i k) -> p i k", k=128)
                    vTs = g("vTs")[0:LS, :].rearrange("p (i k) -> p i k", k=128)
                    P.mm([(bk[:, i * 128:(i + 1) * 128], khTs[:, i, :], vTs[:, i, :], True, True) for i in range(NSEQ_S)],
                         [B("khTs"), B("vTs")], [bkb])
                    dec = g("bb")[:, TP:TP + NS].rearrange("p (i t) -> p i t", t=LS)[:, :, LS - 1:LS]
                    P.op(DVE, "tensor_tensor", [B("bb")], [B("Ss")], Ss, Ss, dec.to_broadcast([128, NSEQ_S, 128]), ALU.mult)
                    P.op(DVE, "tensor_tensor", [bkb], [B("Ss")], Ss, Ss, bk[:, :].rearrange("p (i k) -> p i k", k=128), ALU.add)
                    P.dma(SP, hg_s[a, :, h].rearrange("i k v -> k i v"), Ss, [B("Ss")], [])

            def stage_O(h):
                S_ = sets[h % 2]
                g = lambda nm: S_[nm][0]
                B = lambda nm: S_[nm][1]
                At = g("At")[0:HC, :].rearrange("p (c t) -> p c t", t=HC)
                vT = g("vT")[0:HC, :].rearrange("p (c k) -> p c k", k=128)
                Sbf = g("Sbf").rearrange("p (c k) -> p c k", k=128)
                obank = []
                bk, bkb = psring.next()
                mms = []
                for ci in range(npc):
                    o_ = bk[:, ci * HC:(ci + 1) * HC]
                    mms.append((o_, vT[:, ci, :], At[:, ci, :], True, False))
                    mms.append((o_, Sbf[:, ci, :], g("qh")[:, ci * HC:(ci + 1) * HC], False, True))
                P.mm(mms, [B("vT"), B("At"), B("Sbf"), B("qh")], [bkb])
                obank.append((bk, bkb, 0, TP))
                if has_s:
                    bk2, bk2b = psring.next()
                    Ats = g("Ats")[0:LS, :].rearrange("p (c t) -> p c t", t=LS)
                    vTs = g("vTs")[0:LS, :].rearrange("p (i k) -> p i k", k=128)
                    Sbs = g("Sbs").rearrange("p (i k) -> p i k", k=128)
                    mms = []
                    for i in range(NSEQ_S):
                        o_ = bk2[:, i * LS:(i + 1) * LS]
                        mms.append((o_, vTs[:, i, :], Ats[:, i, :], True, False))
                        mms.append((o_, Sbs[:, i, :], g("qh")[:, TP + i * LS:TP + (i + 1) * LS], False, True))
                    P.mm(mms, [B("vTs"), B("Ats"), B("Sbs"), B("qh")], [bk2b])
                    obank.append((bk2, bk2b, TP, NS))
                hi = a * 16 + h
                for (bk_, bkb_, c0, n) in obank:
                    sl = slice(c0, c0 + n)
                    P.op(ACT, "activation", [bkb_], [B("dd")], g("dd")[:, sl], bk_[:, 0:n], AF.Square)
                    bn, bnb = psring.next()
                    P.mm([(bn[:, 0:n], ones32[:], g("dd")[:, sl], True, True)], [B("dd"), cb], [bnb])
                    P.op(ACT, "activation", [bnb], [B("dl")], g("dl")[:, sl], bn[:, 0:n], AF.Sqrt, bias=EPS, scale=1.0 / 128)
                    P.op(DVE, "reciprocal", [B("dl")], [B("dl")], g("dl")[:, sl], g("dl")[:, sl])
                    P.op(DVE, "scalar_tensor_tensor", [bkb_, B("dl"), cb], [B("dd")], g("dd")[:, sl], bk_[:, 0:n],
                         hgn_sb[:, hi:hi + 1], g("dl")[:, sl], ALU.mult, ALU.mult)
                    P.op(DVE, "tensor_tensor", [B("dd"), B("sgl")], [obb[h]], ob[:, h, sl], g("dd")[:, sl],
                         g("sgl")[:, sl], ALU.mult)

            stage_P(0)
            stage_E(0)
            for h in range(16):
                if h + 1 < 16:
                    stage_P(h + 1)
                stage_T(h)
                stage_U(h)
                stage_O(h)
                if h + 1 < 16:
                    stage_E(h + 1)
            out_proj(w_hout[a], slabs, nt)
            post_norm(l, 3, False, slabs, nt, ym, ymb)

        def swa(l, p, slabs, nt):
            b = l // 2
            last = (p == NP - 1)
            has_s = (p == 0)
            pre_norm(l, 2, slabs, nt)
            carve = carver()
            k32 = carve(4 * (WIN + NS)).rearrange("p (j n) -> p j n", n=WIN + NS); k32b = Buf()
            vl32 = carve(512); vl32b = Buf()
            vsst_r = Ring([(carve(128), Buf()) for _ in range(2)])
            krp = carve(4 * (WIN + TP) // 2, BF16).rearrange("p (j n) -> p j n", n=WIN + TP); krpb = [Buf() for _ in range(4)]
            vtm = carve(5 * 512 // 2, BF16).rearrange("p (t n) -> p t n", n=512); vtmb = [Buf() for _ in range(5)]
            qsets = []
            for _ in range(2):
                qsets.append((carve(4 * NTMAX // 2, BF16).rearrange("p (c n) -> p c n", n=NTMAX), [Buf() for _ in range(4)]))
            vbf = carve(NTMAX // 2, BF16); vbfb = Buf()
            qf_r = Ring([(carve(NTMAX), Buf()) for _ in range(2)])
            t2_r = Ring([(carve(NTMAX), Buf()) for _ in range(2)])
            r32_r = Ring([(carve(NTMAX), Buf()) for _ in range(2)])
            pe_r = Ring([(carve(256), Buf()) for _ in range(3)])
            pm_r = Ring([(carve(256), Buf()) for _ in range(3)])
            pn_r = Ring([(carve(256), Buf()) for _ in range(3)])
            pT_r = Ring([(carve(128, BF16), Buf()) for _ in range(3)])
            sm_r = Ring([(carve(8), Buf()) for _ in range(4)])
            stg = carve(512); stgb = Buf()
            if has_s:
                qs = carve(4 * 64 // 2, BF16).rearrange("p (j i c t) -> p j i c t", j=4, i=4, c=4); qsb = Buf()
                kcs = carve(16 * 132 // 2, BF16).rearrange("p (j i n) -> p j i n", j=4, i=4); kcsb = Buf()
                vcs = carve(4 * 512 // 2, BF16).rearrange("p (i n) -> p i n", n=512); vcsb = Buf()
                vns = carve(4 * 512 // 2, BF16).rearrange("p (i n) -> p i n", n=512); vnsb = Buf()
                kc32 = carve(512); kc32b = Buf()

            P.op(DVE, "tensor_copy", [kvpb[b]], krpb, krp[:, :, 0:WIN], kprev[b][:, :, :])
            P.op(DVE, "tensor_copy", [kvpb[b]], [vtmb[0]], vtm[:, 0, :], vprev[b][:, :])
            P.dma(SP, cosT[:, 0:TP], cosT_d[:, p * TP:(p + 1) * TP], [], [ropeb])
            P.dma(SP, sinT[:, 0:TP], sinT_d[:, p * TP:(p + 1) * TP], [], [ropeb])
            if has_s:
                P.dma(SP, cosT[:, TP:TP + NS], cosT_d[:, cfg.seq:cfg.seq + NS], [], [ropeb])
                P.dma(SP, sinT[:, TP:TP + NS], sinT_d[:, cfg.seq:cfg.seq + NS], [], [ropeb])

            wv = w_ain[b].rearrange("(c p) n -> p c n", p=128)

            def rope(bank, bankb, n, c0, outs):
                qf, qfb = qf_r.next()
                P.op(ACT, "activation", [bankb], [qfb], qf[:, 0:n], bank[:, 0:n], AF.Copy)
                if "sR" in DBG_SKIP:
                    for (en, out_ap, bufs, cs, v3) in outs:
                        src = qf[:, cs]
                        if v3:
                            src = src.rearrange("p (i t) -> p i t", t=LS)
                        P.op(DVE, "tensor_copy", [qfb], bufs, out_ap, src)
                    return
                br, brb = psring.next()
                P.mm([(br[:, 0:n], rotm[:], qf[:, 0:n], True, True)], [qfb, cb], [brb])
                t2, t2b = t2_r.next()
                P.op(DVE, "tensor_tensor", [brb, ropeb], [t2b], t2[:, 0:n], br[:, 0:n], sinT[:, c0:c0 + n], ALU.mult)
                P.op(DVE, "tensor_tensor", [qfb, ropeb], [qfb], qf[:, 0:n], qf[:, 0:n], cosT[:, c0:c0 + n], ALU.mult)
                r32, r32b = r32_r.next()
                P.op(DVE, "tensor_tensor", [qfb, t2b], [r32b], r32[:, 0:n], qf[:, 0:n], t2[:, 0:n], ALU.add)
                for (en, out_ap, bufs, cs, v3) in outs:
                    src = r32[:, cs]
                    if v3:
                        src = src.rearrange("p (i t) -> p i t", t=LS)
                    if en == "act":
                        P.op(ACT, "activation", [r32b], bufs, out_ap, src, AF.Copy)
                    else:
                        P.op(DVE, "tensor_copy", [r32b], bufs, out_ap, src)

            def stage_P(jp):
                qr, qrb = qsets[jp % 2]
                s1 = wload([(0, 512, 16, wv[:, :, jp * 512:(jp + 1) * 512])])
                ws1 = wr[s1][:, :].rearrange("p (c n) -> p c n", n=512)
                for (c0, n) in slabs:
                    for c in range(4):
                        bk, bkb = psring.next()
                        P.mm([(bk[:, 0:n], ws1[:, k, c * 128:(c + 1) * 128], hT[:, k, c0:c0 + n]) for k in range(NCH)],
                             [wrb[s1]] + hbufs, [bkb])
                        rope(bk, bkb, n, c0, [("act", qr[:, c, c0:c0 + n], [qrb[c]], slice(0, n), False)])
                s2 = wload([(0, 128, 16, wv[:, :, 2048 + jp * 128:2048 + (jp + 1) * 128]),
                            (2048, 128, 16, wv[:, :, 2560 + jp * 128:2560 + (jp + 1) * 128])])
                ws2 = wr[s2][:, 0:4096].rearrange("p (g c n) -> p g c n", g=2, c=16)
                for (c0, n) in slabs:
                    bk, bkb = psring.next()
                    P.mm([(bk[:, 0:n], ws2[:, 0, k, :], hT[:, k, c0:c0 + n]) for k in range(NCH)], [wrb[s2]] + hbufs, [bkb])
                    if "sK" in DBG_SKIP:
                        pass
                    elif c0 == 0:
                        rope(bk, bkb, n, c0, [("act", krp[:, jp, WIN:WIN + TP], [krpb[jp]], slice(0, n), False),
                                              ("dve", k32[:, jp, 0:WIN], [k32b], slice(TP - WIN, TP), False)])
                    else:
                        rope(bk, bkb, n, c0, [("act", kcs[:, jp, :, 128:132], [kcsb], slice(0, n), True),
                                              ("dve", k32[:, jp, WIN:WIN + NS], [k32b], slice(0, n), False)])
                    if c0 == 0:
                        for tt in range(4):
                            bt, btb = psring.next()
                            P.mm([(bt[:, 0:128], hT[:, k, tt * 128:(tt + 1) * 128], ws2[:, 1, k, :]) for k in range(NCH)],
                                 [wrb[s2]] + hbufs, [btb])
                            if tt == 3 and last:
                                P.op(ACT, "activation", [btb], [vl32b], vl32[:, jp * 128:(jp + 1) * 128], bt[:, 0:128], AF.Copy)
                                P.op(DVE, "tensor_copy", [vl32b], [vtmb[1 + tt]], vtm[:, 1 + tt, jp * 128:(jp + 1) * 128],
                                     vl32[:, jp * 128:(jp + 1) * 128])
                            else:
                                P.op(ACT, "activation", [btb], [vtmb[1 + tt]], vtm[:, 1 + tt, jp * 128:(jp + 1) * 128], bt[:, 0:128], AF.Copy)
                    else:
                        for i in range(NSEQ_S):
                            bt, btb = psring.next()
                            P.mm([(bt[0:LS, 0:128], hT[:, k, TP + i * LS:TP + (i + 1) * LS], ws2[:, 1, k, :]) for k in range(NCH)],
                                 [wrb[s2]] + hbufs, [btb])
                            vs_, vsb_ = vsst_r.next()
                            P.op(ACT, "activation", [btb], [vsb_], vs_[0:LS, :], bt[0:LS, 0:128], AF.Copy)
                            P.op(DVE, "tensor_copy", [vsb_], [vnsb], vns[0:LS, i, jp * 128:(jp + 1) * 128], vs_[0:LS, :])
                            P.dma(SP, vw_s[b, i, WIN - LS:WIN, jp * 128:(jp + 1) * 128], vs_[0:LS, :], [vsb_], [])

            def softmax_pv(S_ap, sbuf_b, rows, ncols, nsink_col, sink_col, mask_ap):
                sm, smb = sm_r.next()
                P.op(DVE, "reduce_max", [sbuf_b], [smb], sm[0:rows, 0:1], S_ap, AX.X)
                P.op(DVE, "tensor_scalar", [smb, cb], [smb], sm[0:rows, 1:2], sm[0:rows, 0:1], -0.125, nsink_col, ALU.mult, ALU.min)
                pe_, peb = pe_r.next()
                P.op(ACT, "activation", [sbuf_b, smb], [peb], pe_[0:rows, 0:ncols], S_ap, AF.Exp, bias=sm[0:rows, 1:2], scale=0.125)
                P.op(ACT, "activation", [smb, cb], [smb], sm[0:rows, 2:3], sink_col, AF.Exp, bias=sm[0:rows, 1:2], scale=1.0)
                pm, pmb = pm_r.next()
                P.op(DVE, "tensor_tensor", [peb, cb], [pmb], pm[0:rows, 0:ncols], pe_[0:rows, 0:ncols], mask_ap, ALU.mult)
                P.op(DVE, "reduce_sum", [pmb], [smb], sm[0:rows, 3:4], pm[0:rows, 0:ncols], AX.X)
                P.op(DVE, "tensor_tensor", [smb], [smb], sm[0:rows, 4:5], sm[0:rows, 3:4], sm[0:rows, 2:3], ALU.add)
                P.op(DVE, "reciprocal", [smb], [smb], sm[0:rows, 5:6], sm[0:rows, 4:5])
                pn, pnb = pn_r.next()
                P.op(ACT, "activation", [pmb, smb], [pnb], pn[0:rows, 0:ncols], pm[0:rows, 0:ncols], AF.Copy, scale=sm[0:rows, 5:6])
                return pn, pnb

            def stage_A(jp):
                qr, qrb = qsets[jp % 2]
                for c in range(4):
                    bo, bob = accring.next()
                    for offi in range(2):
                        kv = 2 * jp + offi
                        hq = 4 * kv + c
                        psl = slice(offi * 64, offi * 64 + 64)
                        for qb in range(TP // 128):
                            bs, bsb = psring.next()
                            P.mm([(bs[:, 0:256], qr[psl, c, qb * 128:(qb + 1) * 128], krp[psl, jp, qb * 128:qb * 128 + 256], True, True)],
                                 [qrb[c], krpb[jp]], [bsb])
                            mask = mfirst if (p == 0 and qb == 0 and "nomf" not in DBG_SKIP) else mband
                            pn, pnb = softmax_pv(bs[:, 0:256], bsb, 128, 256, nsink_sb[:, b * 32 + hq:b * 32 + hq + 1],
                                                 sink_sb[:, b * 32 + hq:b * 32 + hq + 1], mask[:, :])
                            bt, btb = psring.next()
                            P.mm([(bt[:, kb * 128:(kb + 1) * 128], pn[:, kb * 128:(kb + 1) * 128], ident32[:]) for kb in range(2)],
                                 [pnb, cb], [btb], transpose=True)
                            pT, pTb = pT_r.next()
                            P.op(DVE, "tensor_copy", [btb], [pTb], pT[:, 0:256], bt[:, 0:256])
                            P.mm([(bo[psl, qb * 128:(qb + 1) * 128], vtm[:, qb + kb, jp * 128 + offi * 64:jp * 128 + offi * 64 + 64],
                                   pT[:, kb * 128:(kb + 1) * 128]) for kb in range(2)],
                                 [vtmb[qb], vtmb[qb + 1], pTb], [bob])
                    P.op(ACT, "activation", [bob], [obb[jp * 4 + c]], ob[:, jp * 4 + c, 0:TP], bo[:, 0:TP], AF.Copy)

            def sample_attn():
                pool_sync(P)
                for i in range(NSEQ_S):
                    P.dma(SP, kc32[:, :], ck_in[b, i], [], [kc32b])
                    bt, btb = psring.next()
                    P.mm([(bt[:, j * 128:(j + 1) * 128], kc32[:, j * 128:(j + 1) * 128], ident32[:]) for j in range(4)],
                         [kc32b, cb], [btb], transpose=True)
                    P.op(DVE, "tensor_copy", [btb], [kcsb], kcs[:, :, i, 0:128], bt[:, :].rearrange("p (j n) -> p j n", n=128))
                    P.dma(POOL, vcs[:, i, :], cv_in[b, i], [], [vcsb])
                for jp in range(4):
                    for offi in range(2):
                        kv = 2 * jp + offi
                        psl = slice(offi * 64, offi * 64 + 64)
                        for i in range(NSEQ_S):
                            bs, bsb = psring.next()
                            P.mm([(bs[0:16, 0:132], qs[psl, jp, i, :, :], kcs[psl, jp, i, :], True, True)], [qsb, kcsb], [bsb])
                            pn, pnb = softmax_pv(bs[0:16, 0:132], bsb, 16, 132, nsinks16[:, b * 8 + kv:b * 8 + kv + 1],
                                                 sinks16[:, b * 8 + kv:b * 8 + kv + 1], msamp[:, :])
                            bt, btb = psring.next()
                            P.mm([(bt[:, 0:16], pn[0:16, 0:128], ident32[0:16, 0:16]),
                                  (bt[0:LS, 16:32], pn[0:16, 128:132], ident32[0:16, 0:16])], [pnb, cb], [btb], transpose=True)
                            pT, pTb = pT_r.next()
                            P.op(DVE, "tensor_copy", [btb], [pTb], pT[:, 0:16], bt[:, 0:16])
                            P.op(DVE, "tensor_copy", [btb], [pTb], pT[0:LS, 16:32], bt[0:LS, 16:32])
                            bo, bob = psring.next()
                            P.mm([(bo[psl, 0:16], vcs[:, i, kv * 64:(kv + 1) * 64], pT[:, 0:16]),
                                  (bo[psl, 0:16], vns[0:LS, i, kv * 64:(kv + 1) * 64], pT[0:LS, 16:32])],
                                 [vcsb, vnsb, pTb], [bob])
                            P.op(ACT, "activation", [bob], obb[jp * 4:jp * 4 + 4],
                                 ob[psl, jp * 4:jp * 4 + 4, TP + i * LS:TP + (i + 1) * LS],
                                 bo[psl, 0:16].rearrange("p (c t) -> p c t", t=LS), AF.Copy)
                for i in range(NSEQ_S):
                    P.dma(SP, kw_s[b, i, 0:WIN - LS], ck_in[b, i, LS:WIN], [], [])
                    P.dma(SP, vw_s[b, i, 0:WIN - LS], cv_in[b, i, LS:WIN], [], [])
                    for (src32, srcb, dst) in ((k32, k32b, kw_s),):
                        bt, btb = psring.next()
                        P.mm([(bt[0:LS, j * 128:(j + 1) * 128], src32[:, j, WIN + i * LS:WIN + (i + 1) * LS], ident32[:]) for j in range(4)],
                             [srcb, cb], [btb], transpose=True)
                        P.op(ACT, "activation", [btb], [stgb], stg[0:LS, :], bt[0:LS, :], AF.Copy)
                        P.dma(SP, dst[b, i, WIN - LS:WIN], stg[0:LS, :], [stgb], [])

            def fill_qs(jp):
                qr, qrb = qsets[jp % 2]
                P.op(DVE, "tensor_copy", qrb, [qsb], qs[:, jp].rearrange("p i c t -> p c i t"),
                     qr[:, :, TP:TP + NS].rearrange("p c (i t) -> p c i t", t=LS))

            for i in range(5):
                if i < 4 and "sP" not in DBG_SKIP:
                    stage_P(i)
                    if has_s and "sQ" not in DBG_SKIP:
                        fill_qs(i)
                if i >= 1 and "sA" not in DBG_SKIP:
                    stage_A(i - 1)
            if has_s and "sS" not in DBG_SKIP:
                sample_attn()
            if last and "sC" not in DBG_SKIP:
                P.dma(SP, vw_p[b], vl32[:, :], [vl32b], [])
                for (src32, srcb, dst) in ((k32, k32b, kw_p),):
                    bt, btb = psring.next()
                    P.mm([(bt[:, j * 128:(j + 1) * 128], src32[:, j, 0:WIN], ident32[:]) for j in range(4)],
                         [srcb, cb], [btb], transpose=True)
                    P.op(ACT, "activation", [btb], [stgb], stg[:, :], bt[:, :], AF.Copy)
                    P.dma(SP, dst[b], stg[:, :], [stgb], [])
            if not last:
                P.op(DVE, "tensor_copy", krpb, [kvpb[b]], kprev[b][:, :, :], krp[:, :, TP:TP + WIN])
                P.op(DVE, "tensor_copy", [vtmb[4]], [kvpb[b]], vprev[b][:, :], vtm[:, 4, :])
            out_proj(w_aout[b], slabs, nt)
            post_norm(l, 3, False, slabs, nt, ym, ymb)

        def load_x(p, slabs):
            for (c0, n) in slabs:
                for t0 in range(0, n, 128):
                    r = min(128, n - t0)
                    xs_, xsb = xstring.next()
                    src = xp[p * TP + t0:p * TP + t0 + r, :] if c0 == 0 else xs[0:r, :]
                    P.dma(SP, xs_[0:r, :], src, [], [xsb])
                    for q in range(4):
                        bk, bkb = psring.next()
                        P.mm([(bk[:, k * 128:k * 128 + r], xs_[0:r, (4 * q + k) * 128:(4 * q + k + 1) * 128], ident32[0:r, 0:r]) for k in range(4)],
                             [xsb, cb], [bkb], transpose=True)
                        P.op(ACT, "activation", [bkb], xb[4 * q:4 * q + 4], xT[:, 4 * q:4 * q + 4, c0 + t0:c0 + t0 + r],
                             bk[:, :].rearrange("p (k t) -> p k t", t=128)[:, :, 0:r], AF.Copy)

        def store_x(p, slabs):
            for (c0, n) in slabs:
                for t0 in range(0, n, 128):
                    r = min(128, n - t0)
                    xs_, xsb = xstring.next()
                    for q in range(4):
                        bk, bkb = psring.next()
                        P.mm([(bk[0:r, k * 128:(k + 1) * 128], xT[:, 4 * q + k, c0 + t0:c0 + t0 + r], ident32[:]) for k in range(4)],
                             xb[4 * q:4 * q + 4] + [cb], [bkb], transpose=True)
                        P.op(ACT if q % 2 else DVE, "activation" if q % 2 else "tensor_copy", [bkb], [xsb],
                             xs_[0:r, q * 512:(q + 1) * 512], bk[0:r, :], *([AF.Copy] if q % 2 else []))
                    dst = yp[p * TP + t0:p * TP + t0 + r, :] if c0 == 0 else ys[0:r, :]
                    P.dma(SP, dst, xs_[0:r, :], [xsb], [])

        for p in range(NP):
            slabs = slabs_of(p)
            nt = sum(n for _, n in slabs)
            P.barrier()
            load_x(p, slabs)
            P.barrier()
            for l in range(L):
                if "ffn" not in DBG_SKIP:
                    ffn(l, 0, slabs, nt)
                P.barrier()
                if l % 2 == 0:
                    if "hgrn" not in DBG_SKIP:
                        hgrn(l, p, slabs, nt)
                else:
                    if "swa" not in DBG_SKIP:
                        swa(l, p, slabs, nt)
                P.barrier()
                if "ffn" not in DBG_SKIP:
                    ffn(l, 1, slabs, nt)
            P.barrier()
            store_x(p, slabs)
        P.barrier()
    return nc


def _attn_perm():
    idx = np.empty(2048, np.int64)
    for jp in range(4):
        for c in range(4):
            for off in range(2):
                h = 4 * (2 * jp + off) + c
                base = jp * 512 + c * 128 + off * 64
                idx[base:base + 64] = h * 64 + np.arange(64)
    return idx


_CACHE = {}


def run(cfg, inputs):
    ncores = int(os.environ.get('K_NCORES', '8'))
    key = (cfg.depth, cfg.seq, cfg.dff)
    if key not in _CACHE:
        _CACHE[key] = build(cfg)
    nc = _CACHE[key]
    f = lambda a: np.ascontiguousarray(np.asarray(a, dtype=np.float32))
    L, NA, NB = cfg.depth, cfg.na, cfg.nb
    perm = _attn_perm()
    gains = f(np.asarray(inputs["norm_gains"]).reshape(L, 6, NCH, 128).transpose(3, 0, 1, 2).reshape(128, L * 6 * NCH))
    lbl = f(np.asarray(inputs["hgrn_lb_logits"]).reshape(NA, 16, 128).transpose(2, 0, 1).reshape(128, NA * 16))
    hgn = f(np.asarray(inputs["hgrn_norm_gain"]).reshape(NA, 16, 128).transpose(2, 0, 1).reshape(128, NA * 16))
    if NB:
        wa = np.asarray(inputs["w_attn_in"])
        w_ain = f(np.concatenate([wa[:, :, :2048][:, :, perm], wa[:, :, 2048:]], axis=2))
        w_aout = f(np.asarray(inputs["w_attn_out"])[:, perm, :])
        sinks = f(inputs["attn_sinks"])
        ck = np.asarray(inputs["cache_k_win"]).reshape(NB, cfg.dec_batch, WIN, 512)
        cv = np.asarray(inputs["cache_v_win"]).reshape(NB, cfg.dec_batch, WIN, 512)
    else:
        w_ain = np.zeros((1, D, 3072), np.float32); w_aout = np.zeros((1, D, D), np.float32)
        sinks = np.zeros((1, 32), np.float32)
        ck = np.zeros((1, cfg.dec_batch, WIN, 512), np.float32); cv = ck
    consts = host_consts(cfg)
    shared = {
        "gains": gains,
        "w_fin": f(np.asarray(inputs["w_ffn_in"]).reshape(L, 2, NCH, 128, 2, cfg.nff // 2, 256)
                   .transpose(0, 1, 5, 3, 4, 2, 6).reshape(L, 2, cfg.nff // 2, 128, 8192)),
        "w_fout": f(np.asarray(inputs["w_ffn_out"]).reshape(L, 2, cfg.nff, 128, NCH, 128)
                    .transpose(0, 1, 4, 3, 2, 5).reshape(L, 2, NCH, 128, cfg.nff * 128)),
        "w_hin": f(inputs["w_hgrn_in"]), "lbl": lbl, "hgn": hgn, "w_hout": f(inputs["w_hgrn_out"]),
        "w_ain": w_ain, "sinks_b": sinks, "w_aout": w_aout,
        "sinks16_d": f(np.repeat(sinks.reshape(max(NB, 1), 8, 4).transpose(2, 0, 1).reshape(4, max(NB, 1) * 8), 4, axis=0)),
    }
    for k, v in consts.items():
        shared["c_" + k] = f(v)
    xp_all = np.asarray(inputs["x_prompt"]); xs_all = np.asarray(inputs["x_sample"])
    st_all = np.asarray(inputs["state_hgrn"])
    in_maps = []
    for c in range(ncores):
        m = dict(shared)
        m["xp"] = f(xp_all[c % cfg.batch])
        sl = slice(c * NSEQ_S, (c + 1) * NSEQ_S)
        m["xs"] = f(xs_all[sl].reshape(NS, D))
        m["st_in"] = f(st_all[:, sl])
        m["ck_in"] = f(ck[:, sl]); m["cv_in"] = f(cv[:, sl])
        in_maps.append(m)
    res = run_bass_kernel_spmd(nc, in_maps, core_ids=list(range(ncores)))
    R = res.results
    yp = np.stack([R[b_ % ncores]["yp"] for b_ in range(cfg.batch)])
    ys = np.concatenate([R[c]["ys"].reshape(NSEQ_S, LS, D) for c in range(ncores)], axis=0)
    hgp = np.stack([R[b_ % ncores]["hg_p"] for b_ in range(cfg.batch)], axis=1)
    hgs = np.concatenate([R[c]["hg_s"] for c in range(ncores)], axis=1)
    out = [yp, ys, hgp, hgs]
    kp = np.stack([R[b_ % ncores]["kw_p"][:NB] for b_ in range(cfg.batch)], axis=1).reshape(NB, cfg.batch, WIN, 8, 64)
    vp = np.stack([R[b_ % ncores]["vw_p"][:NB] for b_ in range(cfg.batch)], axis=1).reshape(NB, cfg.batch, WIN, 8, 64)
    ks = np.concatenate([R[c]["kw_s"][:NB] for c in range(ncores)], axis=1).reshape(NB, ncores * NSEQ_S, WIN, 8, 64)
    vs = np.concatenate([R[c]["vw_s"][:NB] for c in range(ncores)], axis=1).reshape(NB, ncores * NSEQ_S, WIN, 8, 64)
    out += [kp, vp, ks, vs]
    return tuple(np.ascontiguousarray(o, dtype=np.float32) for o in out)


def kernel(**inputs):
    return run(Cfg(), inputs)
```
